# Optimizing a Trainium2 kernel written in Bass

```python
import math
import jax, jax.numpy as jnp
from jax import lax
import numpy as np

D_MODEL = 1024
BATCH = 8
SEQ = 8192
DEPTH = 2

CTX_LEN = 256
GRID_W = 64
ROPE_BASE = 10000.0
QBLOCK = 128

DEEPNORM_ALPHA = (2 * DEPTH) ** 0.25
DEEPNORM_BETA = (8 * DEPTH) ** -0.25

CHUNK = 128
GMLP_GROUPS = 4
GMLP_GROUP_CH = 128
GMLP_WIDTH = GMLP_GROUPS * GMLP_GROUP_CH

MLA_HEADS = 8
MLA_NOPE = 64
MLA_ROPE = 32
MLA_V = 64
MLA_Q_RANK = 256
MLA_KV_RANK = 128
MLA_WIDTH = MLA_HEADS * MLA_V
EVEN_SPLITS = [GMLP_WIDTH, 2 * GMLP_WIDTH, 2 * GMLP_WIDTH + MLA_Q_RANK,
               2 * GMLP_WIDTH + MLA_Q_RANK + MLA_KV_RANK]
EVEN_IN = 2 * GMLP_WIDTH + MLA_Q_RANK + MLA_KV_RANK + MLA_ROPE

DIFF_HEADS = 8
DIFF_HEAD_DIM = D_MODEL // DIFF_HEADS // 2
DIFF_WIDTH = DIFF_HEADS * 2 * DIFF_HEAD_DIM
ODD_IN = 3 * DIFF_WIDTH

N_EXPERTS = 16
EXPERT_FF = 1024
CAPACITY_FACTOR = 2

kernel_name = "hybrid_gmlp_mla_diffattn_ecmoe_dit"


def layer_norm(x, g, b, eps=1e-5):
    xf = x.astype(jnp.float32)
    mu = jnp.mean(xf, -1, keepdims=True)
    var = jnp.mean(jnp.square(xf - mu), -1, keepdims=True)
    return ((xf - mu) * lax.rsqrt(var + eps) * g + b).astype(x.dtype)


def rms_norm(x, g, eps=1e-6):
    xf = x.astype(jnp.float32)
    return (xf * lax.rsqrt(jnp.mean(jnp.square(xf), -1, keepdims=True) + eps) * g).astype(x.dtype)


def axial_rope_tables(n_tokens, dim, dtype):
    n_rows = n_tokens // GRID_W
    row = jnp.repeat(jnp.arange(n_rows, dtype=jnp.float32), GRID_W)
    col = jnp.tile(jnp.arange(GRID_W, dtype=jnp.float32), n_rows)
    n_freq = dim // 4
    inv_freq = ROPE_BASE ** (-jnp.arange(n_freq, dtype=jnp.float32) / n_freq)
    ang = jnp.concatenate([row[:, None] * inv_freq, col[:, None] * inv_freq], -1)
    return jnp.cos(ang).astype(dtype), jnp.sin(ang).astype(dtype)


def apply_rope(x, cos, sin):
    x1 = x[..., 0::2]
    x2 = x[..., 1::2]
    cc = cos[None, :, None, :]
    ss = sin[None, :, None, :]
    return jnp.stack([x1 * cc - x2 * ss, x1 * ss + x2 * cc], -1).reshape(x.shape)


def attention_blocked(qs, ks, v, coeffs):
    bn, n, h, d = qs[0].shape
    scale = d ** -0.5
    nblk = n // QBLOCK
    qb = tuple(q.reshape(bn, nblk, QBLOCK, h, d).swapaxes(0, 1) for q in qs)

    def block(qblk):
        p = 0.0
        for coef, q, k in zip(coeffs, qblk, ks):
            s = jnp.einsum('bqhd,bkhd->bhqk', q, k).astype(jnp.float32) * scale
            p = p + coef * jax.nn.softmax(s, axis=-1)
        return jnp.einsum('bhqk,bkhe->bqhe', p.astype(v.dtype), v)

    out = lax.map(block, qb)
    return out.swapaxes(0, 1).reshape(bn, n, h, v.shape[-1])


def modulation(cond, w_mod, b_mod):
    m = jax.nn.silu(cond) @ w_mod + b_mod
    return jnp.split(m[..., None, :], 6, axis=-1)


def chunk_gmlp(u, v, ln_g, ln_b, ws, bs):
    bn, n, w = v.shape
    vn = layer_norm(v, ln_g, ln_b).reshape(bn, n // CHUNK, CHUNK, GMLP_GROUPS, GMLP_GROUP_CH)
    mixed = jnp.einsum('gpq,bnqgc->bnpgc', ws, vn) + bs.T[None, None, :, :, None]
    return u * mixed.reshape(bn, n, w)


def even_project(h, p, rope):
    bn, n, _ = h.shape
    u, v, cq, ckv, kr = jnp.split(h @ p['w_in'], EVEN_SPLITS, axis=-1)
    a = chunk_gmlp(jax.nn.gelu(u, approximate=False), jax.nn.gelu(v, approximate=False),
                   p['gmlp_ln_g'], p['gmlp_ln_b'], p['gmlp_ws'], p['gmlp_bs'])
    q = (rms_norm(cq, p['mla_q_norm']) @ p['mla_w_uq']).reshape(bn, n, MLA_HEADS, MLA_NOPE + MLA_ROPE)
    kv = (rms_norm(ckv, p['mla_kv_norm']) @ p['mla_w_ukv']).reshape(bn, n, MLA_HEADS, MLA_NOPE + MLA_V)
    q_nope, q_rope = q[..., :MLA_NOPE], q[..., MLA_NOPE:]
    k_nope, val = kv[..., :MLA_NOPE], kv[..., MLA_NOPE:]
    kr = kr[:, :, None, :]
    if rope is not None:
        q_rope = apply_rope(q_rope, *rope)
        kr = apply_rope(kr, *rope)
    q = jnp.concatenate([q_nope, q_rope], -1)
    k = jnp.concatenate([k_nope, jnp.broadcast_to(kr, (bn, n, MLA_HEADS, MLA_ROPE))], -1)
    return a, q, k, val


def even_mixer(h_lat, h_ctx, p, need_ctx):
    bn, n, _ = h_lat.shape
    rope = axial_rope_tables(n, MLA_ROPE, h_lat.dtype)
    a_l, q_l, k_l, v_l = even_project(h_lat, p, rope)
    a_c, q_c, k_c, v_c = even_project(h_ctx, p, None)
    o_l = attention_blocked((q_l,), (jnp.concatenate([k_l, k_c], 1),),
                            jnp.concatenate([v_l, v_c], 1), (1.0,))
    out_l = jnp.concatenate([a_l, o_l.reshape(bn, n, MLA_WIDTH)], -1) @ p['w_out']
    out_c = None
    if need_ctx:
        o_c = attention_blocked((q_c,), (k_c,), v_c, (1.0,))
        out_c = jnp.concatenate([a_c, o_c.reshape(bn, h_ctx.shape[1], MLA_WIDTH)], -1) @ p['w_out']
    return out_l, out_c


def odd_mixer(h_lat, h_ctx, p, layer_idx, need_ctx):
    lam_init = 0.8 - 0.6 * math.exp(-0.3 * layer_idx)
    lam = (jnp.exp(jnp.sum(p['lambda_q1'].astype(jnp.float32) * p['lambda_k1']))
           - jnp.exp(jnp.sum(p['lambda_q2'].astype(jnp.float32) * p['lambda_k2'])) + lam_init)
    bn, n, _ = h_lat.shape
    m = h_ctx.shape[1]
    rope = axial_rope_tables(n, DIFF_HEAD_DIM, h_lat.dtype)
    q, k, v = jnp.split(h_lat @ p['w_in'], 3, axis=-1)
    q = apply_rope(q.reshape(bn, n, 2 * DIFF_HEADS, DIFF_HEAD_DIM), *rope).reshape(bn, n, DIFF_HEADS, 2, DIFF_HEAD_DIM)
    k = apply_rope(k.reshape(bn, n, 2 * DIFF_HEADS, DIFF_HEAD_DIM), *rope).reshape(bn, n, DIFF_HEADS, 2, DIFF_HEAD_DIM)
    v = v.reshape(bn, n, DIFF_HEADS, 2 * DIFF_HEAD_DIM)
    k_c, v_c = jnp.split(h_ctx @ p['w_in'][:, DIFF_WIDTH:], 2, axis=-1)
    k_c = k_c.reshape(bn, m, DIFF_HEADS, 2, DIFF_HEAD_DIM)
    v_c = v_c.reshape(bn, m, DIFF_HEADS, 2 * DIFF_HEAD_DIM)
    kk = jnp.concatenate([k, k_c], 1)
    vv = jnp.concatenate([v, v_c], 1)
    o = attention_blocked((q[..., 0, :], q[..., 1, :]), (kk[..., 0, :], kk[..., 1, :]), vv, (1.0, -lam))
    out_l = (rms_norm(o, p['subln_g']) * (1.0 - lam_init)).reshape(bn, n, DIFF_WIDTH) @ p['w_out']
    out_c = None
    if need_ctx:
        q_c = (h_ctx @ p['w_in'][:, :DIFF_WIDTH]).reshape(bn, m, DIFF_HEADS, 2, DIFF_HEAD_DIM)
        o_c = attention_blocked((q_c[..., 0, :], q_c[..., 1, :]), (k_c[..., 0, :], k_c[..., 1, :]), v_c, (1.0, -lam))
        out_c = (rms_norm(o_c, p['subln_g']) * (1.0 - lam_init)).reshape(bn, m, DIFF_WIDTH) @ p['w_out']
    return out_l, out_c


def expert_choice_ffn(h, router, w_gate, w_up, w_down):
    n, d = h.shape[1], h.shape[2]
    cap = CAPACITY_FACTOR * n // N_EXPERTS
    aff = jax.nn.softmax(jnp.einsum('bnd,de->bne', h, router).astype(jnp.float32), axis=-1)
    g, idx = lax.top_k(aff.transpose(0, 2, 1), cap)
    xs = jax.vmap(lambda hb, ib: hb[ib])(h, idx)
    hid = jax.nn.silu(jnp.einsum('becd,edf->becf', xs, w_gate)) * jnp.einsum('becd,edf->becf', xs, w_up)
    y = jnp.einsum('becf,efd->becd', hid, w_down) * g[..., None].astype(h.dtype)
    return jax.vmap(lambda yb, ib: jnp.zeros((n, d), yb.dtype).at[ib.reshape(-1)].add(yb.reshape(-1, d)))(y, idx)


def setup_inputs(seed: int = 0) -> dict:
    key = jax.random.key(seed)
    ks = iter(jax.random.split(key, 64))
    D = D_MODEL

    def nrm(shape, scale=1.0):
        return jax.random.normal(next(ks), shape, jnp.float32) * scale

    def gain(n):
        return 1.0 + nrm((n,), 0.02)

    inp = {}
    inp['x'] = nrm((BATCH, SEQ, D))
    inp['c'] = nrm((BATCH, D))
    inp['ctx'] = nrm((BATCH, CTX_LEN, D))
    inp['c_ctx'] = nrm((D,))
    inp['w_mod_0'] = nrm((D, 6 * D), 0.25 * D ** -0.5)
    inp['b_mod_0'] = nrm((6 * D,), 0.01)
    inp['w_in_0'] = nrm((D, EVEN_IN), D ** -0.5)
    inp['gmlp_ln_g_0'] = gain(GMLP_WIDTH)
    inp['gmlp_ln_b_0'] = nrm((GMLP_WIDTH,), 0.02)
    inp['gmlp_ws_0'] = nrm((GMLP_GROUPS, CHUNK, CHUNK), CHUNK ** -0.5)
    inp['gmlp_bs_0'] = 1.0 + nrm((GMLP_GROUPS, CHUNK), 0.02)
    inp['mla_q_norm_0'] = gain(MLA_Q_RANK)
    inp['mla_w_uq_0'] = nrm((MLA_Q_RANK, MLA_HEADS * (MLA_NOPE + MLA_ROPE)), MLA_Q_RANK ** -0.5)
    inp['mla_kv_norm_0'] = gain(MLA_KV_RANK)
    inp['mla_w_ukv_0'] = nrm((MLA_KV_RANK, MLA_HEADS * (MLA_NOPE + MLA_V)), MLA_KV_RANK ** -0.5)
    inp['w_out_0'] = nrm((GMLP_WIDTH + MLA_WIDTH, D), DEEPNORM_BETA * (GMLP_WIDTH + MLA_WIDTH) ** -0.5)
    inp['ln_mix_g_0'] = gain(D)
    inp['ln_mix_b_0'] = nrm((D,), 0.02)
    inp['router_0'] = nrm((D, N_EXPERTS), D ** -0.5)
    inp['w_gate_0'] = nrm((N_EXPERTS, D, EXPERT_FF), D ** -0.5)
    inp['w_up_0'] = nrm((N_EXPERTS, D, EXPERT_FF), D ** -0.5)
    inp['w_down_0'] = nrm((N_EXPERTS, EXPERT_FF, D), DEEPNORM_BETA * EXPERT_FF ** -0.5)
    inp['ln_ffn_g_0'] = gain(D)
    inp['ln_ffn_b_0'] = nrm((D,), 0.02)
    inp['w_mod_1'] = nrm((D, 6 * D), 0.25 * D ** -0.5)
    inp['b_mod_1'] = nrm((6 * D,), 0.01)
    inp['w_in_1'] = nrm((D, ODD_IN), D ** -0.5)
    inp['lambda_q1_1'] = nrm((DIFF_HEAD_DIM,), 0.1)
    inp['lambda_k1_1'] = nrm((DIFF_HEAD_DIM,), 0.1)
    inp['lambda_q2_1'] = nrm((DIFF_HEAD_DIM,), 0.1)
    inp['lambda_k2_1'] = nrm((DIFF_HEAD_DIM,), 0.1)
    inp['subln_g_1'] = gain(2 * DIFF_HEAD_DIM)
    inp['w_out_1'] = nrm((DIFF_WIDTH, D), DEEPNORM_BETA * DIFF_WIDTH ** -0.5)
    inp['ln_mix_g_1'] = gain(D)
    inp['ln_mix_b_1'] = nrm((D,), 0.02)
    inp['router_1'] = nrm((D, N_EXPERTS), D ** -0.5)
    inp['w_gate_1'] = nrm((N_EXPERTS, D, EXPERT_FF), D ** -0.5)
    inp['w_up_1'] = nrm((N_EXPERTS, D, EXPERT_FF), D ** -0.5)
    inp['w_down_1'] = nrm((N_EXPERTS, EXPERT_FF, D), DEEPNORM_BETA * EXPERT_FF ** -0.5)
    inp['ln_ffn_g_1'] = gain(D)
    inp['ln_ffn_b_1'] = nrm((D,), 0.02)
    return inp


def reference(x, c, ctx, c_ctx,
              w_mod_0, b_mod_0, w_in_0, gmlp_ln_g_0, gmlp_ln_b_0, gmlp_ws_0, gmlp_bs_0,
              mla_q_norm_0, mla_w_uq_0, mla_kv_norm_0, mla_w_ukv_0, w_out_0,
              ln_mix_g_0, ln_mix_b_0, router_0, w_gate_0, w_up_0, w_down_0, ln_ffn_g_0, ln_ffn_b_0,
              w_mod_1, b_mod_1, w_in_1, lambda_q1_1, lambda_k1_1, lambda_q2_1, lambda_k2_1, subln_g_1,
              w_out_1, ln_mix_g_1, ln_mix_b_1, router_1, w_gate_1, w_up_1, w_down_1, ln_ffn_g_1, ln_ffn_b_1):
    p0 = dict(w_mod=w_mod_0, b_mod=b_mod_0, w_in=w_in_0, gmlp_ln_g=gmlp_ln_g_0, gmlp_ln_b=gmlp_ln_b_0,
              gmlp_ws=gmlp_ws_0, gmlp_bs=gmlp_bs_0, mla_q_norm=mla_q_norm_0, mla_w_uq=mla_w_uq_0,
              mla_kv_norm=mla_kv_norm_0, mla_w_ukv=mla_w_ukv_0, w_out=w_out_0,
              ln_mix_g=ln_mix_g_0, ln_mix_b=ln_mix_b_0, router=router_0, w_gate=w_gate_0,
              w_up=w_up_0, w_down=w_down_0, ln_ffn_g=ln_ffn_g_0, ln_ffn_b=ln_ffn_b_0)
    p1 = dict(w_mod=w_mod_1, b_mod=b_mod_1, w_in=w_in_1, lambda_q1=lambda_q1_1, lambda_k1=lambda_k1_1,
              lambda_q2=lambda_q2_1, lambda_k2=lambda_k2_1, subln_g=subln_g_1, w_out=w_out_1,
              ln_mix_g=ln_mix_g_1, ln_mix_b=ln_mix_b_1, router=router_1, w_gate=w_gate_1,
              w_up=w_up_1, w_down=w_down_1, ln_ffn_g=ln_ffn_g_1, ln_ffn_b=ln_ffn_b_1)
    layers = (p0, p1)
    x_lat, x_ctx = x, ctx
    for l in range(DEPTH):
        p = layers[l]
        need_ctx = l < DEPTH - 1
        sh_a, sc_a, g_a, sh_f, sc_f, g_f = modulation(c, p['w_mod'], p['b_mod'])
        csh_a, csc_a, cg_a, csh_f, csc_f, cg_f = modulation(c_ctx, p['w_mod'], p['b_mod'])
        h_lat = x_lat * (1.0 + sc_a) + sh_a
        h_ctx = x_ctx * (1.0 + csc_a) + csh_a
        if l % 2 == 0:
            m_lat, m_ctx = even_mixer(h_lat, h_ctx, p, need_ctx)
        else:
            m_lat, m_ctx = odd_mixer(h_lat, h_ctx, p, l, need_ctx)
        x_lat = layer_norm(DEEPNORM_ALPHA * x_lat + (1.0 + g_a) * m_lat, p['ln_mix_g'], p['ln_mix_b'])
        f_lat = expert_choice_ffn(x_lat * (1.0 + sc_f) + sh_f, p['router'], p['w_gate'], p['w_up'], p['w_down'])
        x_lat = layer_norm(DEEPNORM_ALPHA * x_lat + (1.0 + g_f) * f_lat, p['ln_ffn_g'], p['ln_ffn_b'])
        if need_ctx:
            x_ctx = layer_norm(DEEPNORM_ALPHA * x_ctx + (1.0 + cg_a) * m_ctx, p['ln_mix_g'], p['ln_mix_b'])
            f_ctx = expert_choice_ffn(x_ctx * (1.0 + csc_f) + csh_f, p['router'], p['w_gate'], p['w_up'], p['w_down'])
            x_ctx = layer_norm(DEEPNORM_ALPHA * x_ctx + (1.0 + cg_f) * f_ctx, p['ln_ffn_g'], p['ln_ffn_b'])
    return x_lat
```

```python
import math
import numpy as np
import concourse.bass as bass
import concourse.mybir as mybir
from concourse.bass_utils import run_bass_kernel_spmd

F32 = mybir.dt.float32
BF16 = mybir.dt.bfloat16
I32 = mybir.dt.int32
U32 = mybir.dt.uint32
ALU = mybir.AluOpType
AF = mybir.ActivationFunctionType

D = 1024
NLAT = 8192
NCTX = 256
NALL = NLAT + NCTX
NPAD = NALL + 128
ALPHA = 4 ** 0.25
LAM_INIT = 0.8 - 0.6 * math.exp(-0.3)
CAP = 192
ECAP = 1024
CCAP = 32
AW = 50000


class Tok:
    __slots__ = ("w", "r", "name")

    def __init__(self, name=""):
        self.w = None
        self.r = {}
        self.name = name


class Tile:
    def __init__(self, ap, name=""):
        self.ap = ap
        self.tok = Tok(name)

    def __getitem__(self, k):
        return self.ap[k]


def _toks(xs):
    return [x.tok if isinstance(x, Tile) else x for x in xs]


class Sched:
    EPOCH = 24000
    NDMA = 48

    def __init__(self, nc):
        self.nc = nc
        self.eng = {"pe": nc.tensor, "act": nc.scalar, "dve": nc.vector, "pool": nc.gpsimd, "sp": nc.sync}
        self.cnt = {e: 0 for e in self.eng}
        self.sems = {e: [] for e in self.eng}
        self.known = {e: {} for e in self.eng}
        self.pending = {e: [] for e in self.eng}
        self.dma_sems = [nc.alloc_semaphore(f"dq{i}") for i in range(self.NDMA)]
        self.dma_val = [0] * self.NDMA
        self.dma_next = 0

    def _sem(self, e, seq):
        k = (seq - 1) // self.EPOCH
        while len(self.sems[e]) <= k:
            self.sems[e].append(self.nc.alloc_semaphore(f"c_{e}_{len(self.sems[e])}"))
        return self.sems[e][k], (seq - 1) % self.EPOCH + 1

    def _wait(self, e, ev):
        if ev is None:
            return
        kind, src, val = ev
        if kind == "eng":
            if src == e and e == "pe":
                return
            key = ("eng", src)
            if self.known[e].get(key, 0) >= val:
                return
            if src == e and val <= self.cnt[e] - 2:
                return
            sem, v = self._sem(src, val)
            self.eng[e].wait_ge(sem, v)
            self.known[e][key] = val
        else:
            key = ("dma", src)
            if self.known[e].get(key, 0) >= val:
                return
            self.eng[e].wait_ge(self.dma_sems[src], val)
            self.known[e][key] = val

    def _deps(self, e, reads, writes):
        for t in reads:
            self._wait(e, t.w)
        for t in writes:
            self._wait(e, t.w)
            for ev in list(t.r.values()):
                self._wait(e, ev)

    def op(self, e, fn, reads=(), writes=(), sig=True):
        reads = _toks(reads)
        writes = _toks(writes)
        self._deps(e, reads, writes)
        ins = fn(self.eng[e])
        if not sig:
            self.pending[e].append((reads, writes))
            return ins
        self.cnt[e] += 1
        seq = self.cnt[e]
        sem, v = self._sem(e, seq)
        ins.then_inc(sem, 1)
        me = ("eng", e, seq)
        groups = self.pending[e] + [(reads, writes)]
        self.pending[e] = []
        for rs, ws in groups:
            for t in rs:
                t.r[("eng", e)] = me
            for t in ws:
                t.w = me
                t.r = {}
        return ins

    def dma(self, q, fn, reads=(), writes=()):
        reads = _toks(reads)
        writes = _toks(writes)
        self._deps(q, reads, writes)
        k = self.dma_next
        self.dma_next = (k + 1) % self.NDMA
        if self.dma_val[k] > 0:
            self._wait(q, ("dma", k, self.dma_val[k]))
        ins = fn(self.eng[q])
        self.dma_val[k] += 16
        ins.then_inc(self.dma_sems[k], 16)
        me = ("dma", k, self.dma_val[k])
        for t in reads:
            t.r[("dma", k)] = me
        for t in writes:
            t.w = me
            t.r = {}
        return ins

    def barrier(self, engines=None):
        engines = engines or list(self.eng)
        for e in self.eng:
            assert not self.pending[e]
        for e in engines:
            for f in self.eng:
                if f != e and self.cnt[f] > 0:
                    self._wait(e, ("eng", f, self.cnt[f]))
            for k in range(self.NDMA):
                if self.dma_val[k] > 0:
                    self._wait(e, ("dma", k, self.dma_val[k]))


def _dsize(dt):
    return 2 if dt == BF16 else 4


class KB:
    def __init__(self, dbg=False, stop_after=None):
        self.dbg = dbg
        self.stop_after = stop_after
        nc = self.nc = bass.Bass("TRN2", target_bir_lowering=False)
        self.s = Sched(nc)
        self.arena = nc.alloc_sbuf_tensor("arena", [128, AW], F32)
        self.off = 0
        self.persist = 0
        self.psum = nc.alloc_psum_tensor("psum_all", [128, 4096], F32)
        self.banks = [Tile(self.psum[:, i * 512:(i + 1) * 512], f"bank{i}") for i in range(8)]
        self.ext = {}
        self.outs = {}

    def tile(self, shape, dt=F32, name=""):
        P = shape[0]
        n = 1
        for d in shape[1:]:
            n *= d
        words = (n * _dsize(dt) + 3) // 4
        words = (words + 7) // 8 * 8
        assert self.off + words <= AW, f"arena overflow {name} {self.off}+{words}"
        ap = self.arena[0:P, self.off:self.off + words]
        self.off += words
        if dt != F32:
            ap = ap.bitcast(dt)
        ap = ap[:, 0:n]
        if len(shape) == 3:
            ap = ap.rearrange("p (a b) -> p a b", a=shape[1])
        elif len(shape) == 4:
            ap = ap.rearrange("p (a b c) -> p a b c", a=shape[1], b=shape[2])
        return Tile(ap, name)

    def phase(self):
        self.s.barrier()
        self.off = self.persist
        for b in self.banks:
            b.tok = Tok(b.tok.name)

    def keep(self):
        self.persist = self.off

    def bank_bf(self, i):
        return self.banks[i].ap.bitcast(BF16)

    def bank2(self, i):
        return self.psum[:, i * 512:(i + 2) * 512]

    def inp(self, name, shape, dt=F32):
        t = self.nc.dram_tensor(name, list(shape), dt, kind="ExternalInput")
        self.ext[name] = (tuple(shape), dt)
        return t.ap()

    def scratch(self, name, shape, dt):
        if self.dbg:
            t = self.nc.dram_tensor(name, list(shape), dt, kind="ExternalOutput")
            self.outs[name] = (tuple(shape), dt)
        else:
            t = self.nc.dram_tensor(name, list(shape), dt)
        return t.ap()

    def dma(self, q, out, in_, reads=(), writes=(), **kw):
        return self.s.dma(q, lambda e: e.dma_start(out=out, in_=in_, **kw), reads, writes)

    def mm(self, out, lhsT, rhs, start, stop, reads, writes, sig=True):
        return self.s.op("pe", lambda e: e.matmul(out, lhsT=lhsT, rhs=rhs, start=start, stop=stop), reads, writes, sig)

    def tr(self, out, in_, ident, reads, writes, sig=True):
        return self.s.op("pe", lambda e: e.transpose(out=out, in_=in_, identity=ident), reads, writes, sig)

    def act(self, out, in_, func, reads, writes, bias=0.0, scale=1.0, accum=None):
        if accum is None:
            return self.s.op("act", lambda e: e.activation(out=out, in_=in_, func=func, bias=bias, scale=scale), reads, writes)
        return self.s.op("act", lambda e: e.activation(out=out, in_=in_, func=func, bias=bias, scale=scale, accum_out=accum), reads, writes)

    def v(self, fn, reads, writes):
        return self.s.op("dve", fn, reads, writes)

    def g(self, fn, reads, writes):
        return self.s.op("pool", fn, reads, writes)

    def tt(self, out, a, b, op, reads, writes, eng="dve"):
        return self.s.op(eng, lambda e: e.tensor_tensor(out=out, in0=a, in1=b, op=op), reads, writes)

    def ts(self, out, a, s1, s2, op0, op1, reads, writes, eng="dve", accum=None):
        if accum is not None:
            return self.s.op(eng, lambda e: e.tensor_scalar(out=out, in0=a, scalar1=s1, scalar2=s2, op0=op0, op1=op1, accum_out=accum), reads, writes)
        if s2 is None:
            return self.s.op(eng, lambda e: e.tensor_scalar(out=out, in0=a, scalar1=s1, scalar2=None, op0=op0), reads, writes)
        return self.s.op(eng, lambda e: e.tensor_scalar(out=out, in0=a, scalar1=s1, scalar2=s2, op0=op0, op1=op1), reads, writes)

    def stt(self, out, a, sc, b, op0, op1, reads, writes, eng="dve"):
        return self.s.op(eng, lambda e: e.scalar_tensor_tensor(out=out, in0=a, scalar=sc, in1=b, op0=op0, op1=op1), reads, writes)

    def rsq(self, out, in_, scale, eps, tmp, reads, writes):
        self.act(tmp, in_, AF.Sqrt, reads, writes, bias=eps, scale=scale)
        self.s.op("dve", lambda e: e.reciprocal(out=out, in_=tmp), _toks(writes), _toks(writes))

    def cp(self, out, in_, reads, writes, eng="dve"):
        if eng == "act":
            return self.s.op("act", lambda e: e.copy(out=out, in_=in_), reads, writes)
        return self.s.op(eng, lambda e: e.tensor_copy(out=out, in_=in_), reads, writes)

    def rsqrt(self, out, in_, scale, eps, reads, writes, tmp, neghalf):
        self.ts(tmp.ap if isinstance(tmp, Tile) else tmp, in_, scale, eps, ALU.mult, ALU.add, reads, [tmp])
        self.tt(out, tmp.ap if isinstance(tmp, Tile) else tmp, neghalf, ALU.pow, [tmp], writes, eng="pool")


class Rot:
    def __init__(self, tiles):
        self.t = tiles
        self.i = -1

    def next(self):
        self.i = (self.i + 1) % len(self.t)
        return self.t[self.i]


MCH0 = [(0, 128), (128, 256), (256, 384), (384, 512), (512, 640), (640, 768), (768, 896), (896, 960)]


def rows_of(E, L, i):
    if L == 0:
        if i < 64:
            return E["x"][i * 128:(i + 1) * 128, :]
        return E["ctx"][(i - 64) * 128:(i - 63) * 128, :]
    return E["X2"][i * 128:(i + 1) * 128, :]


def setup_consts(kb, E):
    C = {}
    C["ident_f"] = kb.tile([128, 128], F32, "ident_f")
    C["ident_b"] = kb.tile([128, 128], BF16, "ident_b")
    C["ones_b"] = kb.tile([128, 128], BF16, "ones_b")
    C["ones_f"] = kb.tile([128, 128], F32, "ones_f")
    C["neghalf"] = kb.tile([128, 512], F32, "neghalf")
    C["mc"] = kb.tile([128, 4], F32, "mc")
    C["bd"] = kb.tile([128, 128], F32, "bd")
    C["affT"] = kb.tile([128, 1024], F32, "affT")
    C["affc"] = kb.tile([16, 256], F32, "affc")
    C["modT"] = [kb.tile([128, 48, 2], F32, f"modT{l}") for l in range(2)]
    C["neglam"] = kb.tile([128, 1], F32, "neglam")
    kb.dma("sp", C["ident_f"].ap, E["ident"], [], [C["ident_f"]])
    kb.dma("sp", C["mc"].ap, E["mconst"], [], [C["mc"]])
    kb.dma("sp", C["bd"].ap, E["bdiag"], [], [C["bd"]])
    kb.cp(C["ident_b"].ap, C["ident_f"].ap, [C["ident_f"]], [C["ident_b"]])
    kb.v(lambda e: e.memset(C["ones_b"].ap, 1.0), [], [C["ones_b"]])
    kb.v(lambda e: e.memset(C["ones_f"].ap, 1.0), [], [C["ones_f"]])
    kb.v(lambda e: e.memset(C["neghalf"].ap, -0.5), [], [C["neghalf"]])
    kb.keep()
    return C


def phase_mod(kb, E, C):
    b = kb.banks
    cc = kb.tile([128, 8, 2], F32, "cc")
    sc = kb.tile([128, 8, 2], F32, "sc")
    kb.dma("sp", cc.ap, E["ccT"], [], [cc])
    kb.act(sc.ap, cc.ap, AF.Silu, [cc], [sc])
    wts = Rot([kb.tile([128, 8, 512], F32, f"wmod{i}") for i in range(2)])
    for l in range(2):
        brow = kb.tile([2, 6144], F32, "brow")
        bT = kb.tile([128, 48], F32, "bT")
        modsb = kb.tile([2, 6144], F32, "modsb")
        kb.dma("sp", brow[0:1, :], E[f"bmod{l}"], [], [brow])
        kb.dma("sp", brow[1:2, :], E[f"bmod{l}"], [], [brow])
        kb.dma("sp", bT.ap, E[f"bmodT{l}"], [], [bT])
        for j in range(12):
            wt = wts.next()
            kb.dma("sp", wt.ap, E[f"wmod{l}"][j], [], [wt])
            bk = b[j % 2]
            for k in range(8):
                kb.mm(bk[0:2, :], sc[:, k, :], wt[:, k, :], k == 0, k == 7, [sc, wt], [bk], sig=(k == 7))
            kb.tt(modsb[:, j * 512:(j + 1) * 512], bk[0:2, :], brow[:, j * 512:(j + 1) * 512], ALU.add, [bk, brow], [modsb])
            for q in range(4):
                c48 = j * 4 + q
                bk2 = b[2 + c48 % 2]
                for k in range(8):
                    kb.mm(bk2[:, 0:2], wt[:, k, q * 128:(q + 1) * 128], sc[:, k, :], k == 0, k == 7, [sc, wt], [bk2], sig=(k == 7))
                kb.ts(C["modT"][l][:, c48, :], bk2[:, 0:2], bT[:, c48:c48 + 1], None, ALU.add, None, [bk2, bT], [C["modT"][l]])
        kb.dma("sp", E["MOD"][l], modsb.ap, [modsb], [])


def bc_load(kb, E, l, st, ch, name, plus1=False):
    t = kb.tile([128, 1024], F32, name)
    src = E["MOD"][l][st, ch * 1024:(ch + 1) * 1024].partition_broadcast(128)
    kb.dma("sp", t.ap, src, [], [t])
    if plus1:
        kb.ts(t.ap, t.ap, 1.0, None, ALU.add, None, [t], [t], eng="pool")
    return t


def bc_row(kb, src_row, name):
    t = kb.tile([128, 1024], F32, name)
    kb.dma("sp", t.ap, src_row.partition_broadcast(128), [], [t])
    return t


def phase_A0(kb, E, C):
    b = kb.banks
    modT = C["modT"][0]
    ident_b, ones_b, ones_f, neghalf = C["ident_b"], C["ones_b"], C["ones_f"], C["neghalf"]
    W = [kb.tile([128, 8, 1472], BF16, f"W{st}") for st in range(2)]
    bcol = kb.tile([128, 8, 2], F32, "bcol")
    brow = [kb.tile([1, 512], BF16, f"brow{st}") for st in range(2)]
    Wq = kb.tile([128, 2, 1024], BF16, "Wq")
    Wk = kb.tile([128, 1024], BF16, "Wk")
    Wv = kb.tile([128, 512], BF16, "Wv")
    wsT = kb.tile([128, 512], BF16, "wsT")
    gln = kb.tile([128, 8], F32, "gln")
    Rt = kb.tile([128, 512], F32, "Rt")
    mark = kb.off
    wtmp = kb.tile([128, 8, 1472], F32, "wtmp")
    kb.dma("sp", wtmp.ap, E["win0"], [], [wtmp])
    Wun = kb.tile([128, 8, 1472], BF16, "Wun")
    for k in range(8):
        kb.cp(Wun[:, k, :], wtmp[:, k, :], [wtmp], [Wun], eng=("act" if k % 2 else "dve"))
    shc = kb.tile([128, 8, 2], BF16, "shc")
    kb.cp(shc.ap, modT[:, 0:8, :], [modT], [shc])
    A1 = kb.tile([128, 8, 2], F32, "A1")
    kb.ts(A1.ap, modT[:, 8:16, :], 1.0, None, ALU.add, None, [modT], [A1])
    for st in range(2):
        for k in range(8):
            kb.ts(W[st][:, k, :], wtmp[:, k, :], A1[:, k, st:st + 1], None, ALU.mult, None, [wtmp, A1], [W[st]],
                  eng=("pool" if k % 2 else "dve"))
    for mi, (a, bb) in enumerate(MCH0):
        for k in range(8):
            kb.mm(b[0][0:bb - a, mi * 2:mi * 2 + 2], Wun[:, k, a:bb], shc[:, k, :], k == 0, k == 7, [Wun, shc], [b[0]], sig=(k == 7))
    kb.cp(bcol[:, 0:7, :], b[0][:, 0:14].rearrange("p (a b) -> p a b", a=7), [b[0]], [bcol])
    kb.cp(bcol[0:64, 7, :], b[0][0:64, 14:16], [b[0]], [bcol])
    for st in range(2):
        for k in range(8):
            kb.mm(b[1][0:1, :], shc[:, k, st:st + 1], Wun[:, k, 960:1472], k == 0, k == 7, [Wun, shc], [b[1]], sig=(k == 7))
        kb.cp(brow[st].ap, b[1][0:1, :], [b[1]], [brow[st]])
    qn = kb.tile([128, 2], F32, "qn")
    kvn = kb.tile([128, 1], F32, "kvn")
    kb.dma("sp", qn.ap, E["qnorm"], [], [qn])
    kb.dma("sp", kvn.ap, E["kvnorm"], [], [kvn])
    wq_f = kb.tile([128, 2, 1024], F32, "wq_f")
    kb.dma("sp", wq_f.ap, E["wuqx"], [], [wq_f])
    for r in range(2):
        kb.ts(Wq[:, r, :], wq_f[:, r, :], qn[:, r:r + 1], None, ALU.mult, None, [wq_f, qn], [Wq])
    wk_f = kb.tile([128, 1024], F32, "wk_f")
    kb.dma("sp", wk_f.ap, E["wukx"], [], [wk_f])
    kb.ts(Wk.ap, wk_f.ap, kvn[:, 0:1], None, ALU.mult, None, [wk_f, kvn], [Wk])
    wv_f = kb.tile([128, 512], F32, "wv_f")
    kb.dma("sp", wv_f.ap, E["wuv"], [], [wv_f])
    kb.ts(Wv.ap, wv_f.ap, kvn[:, 0:1], None, ALU.mult, None, [wv_f, kvn], [Wv])
    wsT_f = kb.tile([128, 512], F32, "wsT_f")
    kb.dma("sp", wsT_f.ap, E["wsT"], [], [wsT_f])
    kb.cp(wsT.ap, wsT_f.ap, [wsT_f], [wsT])
    kb.dma("sp", gln.ap, E["gln"], [], [gln])
    bs_bc = kb.tile([128, 512], F32, "bs_bc")
    kb.dma("sp", bs_bc.ap, E["gbs"][0, :].partition_broadcast(128), [], [bs_bc])
    kb.mm(b[2].ap, ones_f.ap, wsT_f.ap, True, True, [ones_f, wsT_f], [b[2]])
    for g in range(4):
        gs = slice(g * 128, (g + 1) * 128)
        kb.stt(Rt[:, gs], b[2][:, gs], gln[:, 4 + g:5 + g], bs_bc[:, gs], ALU.mult, ALU.add, [b[2], gln, bs_bc], [Rt])
    kb.s.barrier()
    kb.off = mark
    xTs = Rot([kb.tile([128, 8, 512], BF16, f"xT{i}") for i in range(2)])
    xbs = Rot([kb.tile([128, 1024], BF16, f"xb{i}") for i in range(8)])
    guT = kb.tile([128, 4, 512], BF16, "guT")
    tq = kb.tile([128, 2, 512], F32, "tq")
    tkv = kb.tile([128, 512], F32, "tkv")
    sq = kb.tile([128, 3, 512], BF16, "sq")
    krx = kb.tile([64, 512], F32, "krx")
    css = Rot([kb.tile([64, 512], F32, f"cs{i}") for i in range(2)])
    tmpA = kb.tile([128, 512], F32, "tmpA")
    rstd_q = kb.tile([128, 512], F32, "rstd_q")
    rstd_kv = kb.tile([128, 512], F32, "rstd_kv")
    cqn = kb.tile([128, 2, 512], BF16, "cqn")
    ckvn = kb.tile([128, 512], BF16, "ckvn")
    t1 = kb.tile([64, 512], F32, "t1")
    t2 = kb.tile([32, 512], F32, "t2")
    krr = kb.tile([32, 512], BF16, "krr")
    kts = Rot([kb.tile([128, 512], BF16, f"kt{i}") for i in range(2)])
    qts = Rot([kb.tile([128, 512], BF16, f"qt{i}") for i in range(2)])
    vaugs = Rot([kb.tile([128, 8, 128], BF16, f"vaug{i}") for i in range(2)])
    for t in kts.t + qts.t:
        kb.v(lambda e: e.memset(t.ap, 0.0), [], [t])
    for t in vaugs.t:
        kb.v(lambda e: e.memset(t.ap, 1.0), [], [t])
    gv = kb.tile([128, 512], F32, "gv")
    st6 = kb.tile([128, 6], F32, "st6")
    mv = kb.tile([128, 2], F32, "mv")
    rs1 = kb.tile([128, 2], F32, "rs1")
    vhat = kb.tile([128, 512], BF16, "vhat")
    tmpg = kb.tile([128, 512], F32, "tmpg")
    aTs = Rot([kb.tile([128, 4, 128], BF16, f"aT{i}") for i in range(2)])
    AT0v = E["AT0"].rearrange("(g c) t -> c g t", c=128)
    V0v = E["V0"].rearrange("h p kt d -> p h kt d")

    def loadX(gi_):
        res = []
        for tl_ in range(4 if gi_ < 16 else 2):
            xb_ = xbs.next()
            kb.dma("pool", xb_.ap, rows_of(E, 0, gi_ * 4 + tl_), [], [xb_])
            res.append(xb_)
        return res

    def stageL(gi):
        ntl = 4 if gi < 16 else 2
        Wd = ntl * 128
        st = 0 if gi < 16 else 1
        col0 = gi * 512
        xT = xTs.next()
        if gi == 0:
            xbox[0] = loadX(0)
        xb_cur = xbox[0]
        if gi + 1 < 17:
            xbox[0] = loadX(gi + 1)
        for tl in range(ntl):
            i = gi * 4 + tl
            xb = xb_cur[tl]
            pb = kb.bank_bf(0)
            for k in range(8):
                kb.tr(pb[:, k * 128:(k + 1) * 128], xb[:, k * 128:(k + 1) * 128], ident_b.ap, [xb, ident_b], [b[0]], sig=(k == 7))
            kb.cp(xT[:, :, tl * 128:(tl + 1) * 128], pb.rearrange("p (a b) -> p a b", a=8), [b[0]], [xT], eng=("act" if tl % 2 else "dve"))
        return xT

    def stageP(gi, xT):
        ntl = 4 if gi < 16 else 2
        Wd = ntl * 128
        st = 0 if gi < 16 else 1
        col0 = gi * 512
        cs = css.next()
        kb.dma("sp", cs[:, 0:Wd], E["cs0"][:, col0:col0 + Wd], [], [cs])
        for mi, (a, bb) in enumerate(MCH0):
            M_ = bb - a
            bk = b[1 + mi % 2]
            for k in range(8):
                kb.mm(bk[0:M_, 0:Wd], W[st][:, k, a:bb], xT[:, k, 0:Wd], k == 0, k == 7, [W[st], xT], [bk], sig=(k == 7))
            bias = bcol[0:M_, mi, st:st + 1]
            if mi < 4:
                kb.act(guT[:, mi, 0:Wd], bk[:, 0:Wd], AF.Gelu, [bk, bcol], [guT], bias=bias)
            elif mi < 6:
                r = mi - 4
                kb.act(tq[:, r, 0:Wd], bk[:, 0:Wd], AF.Identity, [bk, bcol], [tq], bias=bias)
                kb.act(sq[:, r, 0:Wd], bk[:, 0:Wd], AF.Square, [bk, bcol], [sq], bias=bias)
            elif mi == 6:
                kb.act(tkv[:, 0:Wd], bk[:, 0:Wd], AF.Identity, [bk, bcol], [tkv], bias=bias)
                kb.act(sq[:, 2, 0:Wd], bk[:, 0:Wd], AF.Square, [bk, bcol], [sq], bias=bias)
            else:
                kb.act(krx[:, 0:Wd], bk[0:64, 0:Wd], AF.Identity, [bk, bcol], [krx], bias=bias)
        kb.mm(b[5][:, 0:Wd], ones_b.ap, sq[:, 0, 0:Wd], True, False, [ones_b, sq], [b[5]], sig=False)
        kb.mm(b[5][:, 0:Wd], ones_b.ap, sq[:, 1, 0:Wd], False, True, [ones_b, sq], [b[5]])
        kb.rsq(rstd_q[:, 0:Wd], b[5][:, 0:Wd], 1.0 / 256, 1e-6, tmpA[:, 0:Wd], [b[5]], [tmpA, rstd_q])
        kb.mm(b[5][:, 0:Wd], ones_b.ap, sq[:, 2, 0:Wd], True, True, [ones_b, sq], [b[5]])
        kb.rsq(rstd_kv[:, 0:Wd], b[5][:, 0:Wd], 1.0 / 128, 1e-6, tmpA[:, 0:Wd], [b[5]], [tmpA, rstd_kv])
        for r in range(2):
            kb.tt(cqn[:, r, 0:Wd], tq[:, r, 0:Wd], rstd_q[:, 0:Wd], ALU.mult, [tq, rstd_q], [cqn])
        kb.tt(ckvn[:, 0:Wd], tkv[:, 0:Wd], rstd_kv[:, 0:Wd], ALU.mult, [tkv, rstd_kv], [ckvn])
        kb.tt(t1[0:32, 0:Wd], krx[0:32, 0:Wd], cs[0:32, 0:Wd], ALU.mult, [krx, cs], [t1])
        kb.tt(t2[0:32, 0:Wd], krx[32:64, 0:Wd], cs[32:64, 0:Wd], ALU.mult, [krx, cs], [t2])
        kb.tt(krr[:, 0:Wd], t1[0:32, 0:Wd], t2[0:32, 0:Wd], ALU.add, [t1, t2], [krr])
        for h in range(8):
            bk = b[6 + h % 2]
            kb.mm(bk[:, 0:Wd], Wk[:, h * 128:(h + 1) * 128], ckvn[:, 0:Wd], True, True, [Wk, ckvn], [bk])
            kt = kts.next()
            kb.cp(kt[64:128, 0:Wd], bk[64:128, 0:Wd], [bk], [kt], eng="act")
            kb.cp(kt[0:32, 0:Wd], krr[:, 0:Wd], [krr], [kt], eng="pool")
            kb.dma("sp", E["KT"][h][:, col0:col0 + Wd], kt[:, 0:Wd], [kt], [])
        for h in range(8):
            bk = b[6 + h % 2]
            for r in range(2):
                kb.mm(bk[:, 0:Wd], Wq[:, r, h * 128:(h + 1) * 128], cqn[:, r, 0:Wd], r == 0, r == 1, [Wq, cqn], [bk], sig=(r == 1))
            qt = qts.next()
            kb.cp(qt[64:128, 0:Wd], bk[64:128, 0:Wd], [bk], [qt], eng="act")
            kb.tt(t1[0:32, 0:Wd], bk[0:32, 0:Wd], cs[0:32, 0:Wd], ALU.mult, [bk, cs], [t1])
            kb.tt(t2[0:32, 0:Wd], bk[32:64, 0:Wd], cs[32:64, 0:Wd], ALU.mult, [bk, cs], [t2])
            kb.tt(qt[0:32, 0:Wd], t1[0:32, 0:Wd], t2[0:32, 0:Wd], ALU.add, [t1, t2], [qt])
            kb.dma("sp", E["QT"][h][:, col0:col0 + Wd], qt[:, 0:Wd], [qt], [])
        for tl in range(ntl):
            i = gi * 4 + tl
            ts_ = slice(tl * 128, (tl + 1) * 128)
            kb.mm(b[3].ap, ckvn[:, ts_], Wv.ap, True, True, [ckvn, Wv], [b[3]])
            va = vaugs.next()
            kb.cp(va[:, :, 0:64], b[3].ap.rearrange("p (a b) -> p a b", a=8), [b[3]], [va], eng="act")
            kb.dma("sp", V0v[:, :, i, :], va.ap, [va], [])
            for k in range(8):
                kb.mm(b[4].ap, xT[:, k, ts_], W[st][:, k, 960:1472], k == 0, False, [xT, W[st]], [b[4]], sig=False)
            kb.mm(b[4].ap, ones_b[0:1, 0:128], brow[st].ap, False, True, [ones_b, brow[st]], [b[4]])
            kb.act(gv.ap, b[4].ap, AF.Gelu, [b[4]], [gv])
            kb.v(lambda e: e.bn_stats(out=st6.ap, in_=gv.ap), [gv], [st6])
            kb.v(lambda e: e.bn_aggr(out=mv.ap, in_=st6.ap), [st6], [mv])
            kb.ts(rs1[:, 0:1], mv[:, 1:2], 1e-5, None, ALU.add, None, [mv], [rs1])
            kb.tt(rs1[:, 1:2], rs1[:, 0:1], neghalf[:, 0:1], ALU.pow, [rs1, neghalf], [rs1], eng="pool")
            kb.ts(vhat.ap, gv.ap, mv[:, 0:1], rs1[:, 1:2], ALU.subtract, ALU.mult, [gv, mv, rs1], [vhat])
            for g in range(4):
                gs = slice(g * 128, (g + 1) * 128)
                kb.mm(b[5][:, gs], vhat[:, gs], wsT[:, gs], True, True, [vhat, wsT], [b[5]], sig=(g == 3))
            for g in range(4):
                gs = slice(g * 128, (g + 1) * 128)
                kb.stt(tmpg[:, gs], b[5][:, gs], gln[:, g:g + 1], Rt[:, gs], ALU.mult, ALU.add, [b[5], gln, Rt], [tmpg])
            aT = aTs.next()
            kb.tt(aT.ap, tmpg.ap.rearrange("p (a b) -> p a b", a=4), guT[:, :, ts_], ALU.mult, [tmpg, guT], [aT])
            kb.dma("sp", AT0v[:, :, i * 128:(i + 1) * 128], aT.ap, [aT], [])

    xbox = [None]
    xT_prev = None
    for gi in range(17):
        xT_cur = stageL(gi)
        if xT_prev is not None:
            stageP(gi - 1, xT_prev)
        xT_prev = xT_cur
    stageP(16, xT_prev)


def phase_attn(kb, E, C, L):
    b = kb.banks
    ones_b, ones_f = C["ones_b"], C["ones_f"]
    scale = (96 ** -0.5) if L == 0 else 0.125
    nqb = 17 if L == 0 else 16
    Vd = E["V0"] if L == 0 else E["V1"]
    OT = E["OT"]
    KTb = [kb.tile([128, NALL], BF16, f"KTb{i}") for i in range(2)]
    Vb = [kb.tile([128, 66, 128], BF16, f"Vb{i}") for i in range(2)]
    qts = Rot([kb.tile([128, 512], BF16, f"aq{i}") for i in range(2)])
    if L == 0:
        Ps = Rot([kb.tile([128, 512], BF16, f"P{j}") for j in range(3)])
        rl = [kb.tile([128, 512], F32, f"rl{i}") for i in range(2)]
        ots = Rot([kb.tile([64, 512], BF16, f"ot{i}") for i in range(3)])
    else:
        Ps = Rot([kb.tile([128, 2, 512], BF16, f"P12_{j}") for j in range(4)])
        accs = Rot([kb.tile([128, 512], F32, f"acc{j}") for j in range(2)])
        acc2s = Rot([kb.tile([128, 512], F32, f"acc2_{j}") for j in range(2)])
        rls = [kb.tile([128, 512], F32, f"rl{i}") for i in range(2)]
        tAs = [kb.tile([128, 512], F32, f"tA{i}") for i in range(2)]
        ox = kb.tile([128, 512], F32, "ox")
        osb = [kb.tile([128, 512], F32, f"osb{i}") for i in range(2)]
        l2s = kb.tile([128, 512], F32, "l2s")
        sqx = kb.tile([128, 512], BF16, "sqx")
        tmpR = kb.tile([128, 512], F32, "tmpR")
        rstd = kb.tile([128, 512], F32, "rstdo")
        ots = Rot([kb.tile([128, 512], BF16, f"ot{i}") for i in range(2)])
        subg = kb.tile([128, 1], F32, "subg")
        kb.dma("sp", subg.ap, E["subln"], [], [subg])
        kb.ts(subg.ap, subg.ap, 1.0 - LAM_INIT, None, ALU.mult, None, [subg], [subg])
        neglam = C["neglam"]

    def load_head(h, bi):
        kb.dma("sp", KTb[bi].ap, E["KT"][h], [], [KTb[bi]])
        kb.dma("sp", Vb[bi].ap, Vd[h], [], [Vb[bi]])

    def loadQ(h_, qb_):
        Wd_ = 512 if qb_ < 16 else 256
        qt_ = qts.next()
        kb.dma("sp", qt_[:, 0:Wd_], E["QT"][h_][:, qb_ * 512:qb_ * 512 + Wd_], [], [qt_])
        return qt_

    pending_epi = []
    load_head(0, 0)
    for h in range(8):
        bi = h % 2
        if h + 1 < 8:
            load_head(h + 1, (h + 1) % 2)
        K_ = KTb[bi]
        V_ = Vb[bi]
        for qb in range(nqb):
            Wd = 512 if qb < 16 else 256
            col0 = qb * 512
            ktl = list(range(66)) if qb < 16 else [64, 65]
            n = len(ktl)
            if h == 0 and qb == 0:
                qt_next = loadQ(0, 0)
            qt = qt_next
            if qb + 1 < nqb:
                qt_next = loadQ(h, qb + 1)
            elif h + 1 < 8:
                qt_next = loadQ(h + 1, 0)
            if L == 0:
                O = b[4 + qb % 2]

                def S_(j):
                    kt = ktl[j]
                    sb = b[j % 3]
                    kb.mm(sb[:, 0:Wd], K_[:, kt * 128:(kt + 1) * 128], qt[:, 0:Wd], True, True, [K_, qt], [sb])
                S_(0)
                if n > 1:
                    S_(1)
                for j in range(n):
                    if j + 2 < n:
                        S_(j + 2)
                    sb = b[j % 3]
                    P = Ps.next()
                    kb.act(P[:, 0:Wd], sb[:, 0:Wd], AF.Exp, [sb], [P], scale=scale)
                    kb.mm(O[:, 0:Wd], V_[:, ktl[j], :], P[:, 0:Wd], j == 0, j == n - 1, [V_, P], [O])
                r = rl[qb % 2]
                kb.v(lambda e: e.reciprocal(out=r[64:128, 0:Wd], in_=O[64:128, 0:Wd]), [O], [r])
                ot = ots.next()
                kb.tt(ot[0:64, 0:Wd], O[0:64, 0:Wd], r[64:128, 0:Wd], ALU.mult, [O, r], [ot])
                kb.dma("sp", OT[h * 64:(h + 1) * 64, col0:col0 + Wd], ot[:, 0:Wd], [ot], [])
            else:
                O12 = [b[4], b[5]]
                L2 = b[6]
                acc = accs.next()
                acc2 = acc2s.next()

                def S_(j):
                    kt = ktl[j]
                    for i in range(2):
                        sb = b[(j % 2) * 2 + i]
                        ps = slice(i * 64, (i + 1) * 64)
                        kb.mm(sb.ap, K_[ps, kt * 128:(kt + 1) * 128], qt[ps, :], True, True, [K_, qt], [sb])
                S_(0)
                for j in range(n):
                    if j + 1 < n:
                        S_(j + 1)
                    s0 = (j % 2) * 2
                    P = Ps.next()
                    kb.act(P.ap.rearrange("p a b -> p (a b)"), kb.bank2(s0), AF.Exp, [b[s0], b[s0 + 1]], [P], scale=scale)
                    for i in range(2):
                        kb.mm(O12[i].ap, V_[:, ktl[j], :], P[:, i, :], j == 0, j == n - 1, [V_, P], [O12[i]])
                    if j == 0:
                        kb.cp(acc.ap, P[:, 0, :], [P], [acc])
                        kb.cp(acc2.ap, P[:, 1, :], [P], [acc2], eng="pool")
                    else:
                        kb.tt(acc.ap, acc.ap, P[:, 0, :], ALU.add, [acc, P], [acc])
                        kb.tt(acc2.ap, acc2.ap, P[:, 1, :], ALU.add, [acc2, P], [acc2], eng="pool")
                    if j % 6 == 5 and pending_epi:
                        pending_epi.pop(0)()
                while pending_epi:
                    pending_epi.pop(0)()
                kb.cp(osb[0].ap, O12[0].ap, [O12[0]], [osb[0]], eng="act")
                kb.cp(osb[1].ap, O12[1].ap, [O12[1]], [osb[1]])
                def mk_epi(acc=acc, acc2=acc2, h=h, col0=col0, Wd=Wd):
                    def e0():
                        kb.mm(b[7].ap, ones_f.ap, acc.ap, True, True, [ones_f, acc], [b[7]])
                        kb.v(lambda e: e.reciprocal(out=rls[0].ap, in_=b[7].ap), [b[7]], [rls[0]])

                    def e1():
                        kb.mm(L2.ap, ones_f.ap, acc2.ap, True, True, [ones_f, acc2], [L2])
                        kb.v(lambda e: e.reciprocal(out=rls[1].ap, in_=L2.ap), [L2], [rls[1]])

                    def e2():
                        for i in range(2):
                            kb.tt(tAs[i].ap, osb[i].ap, rls[i].ap, ALU.mult, [osb[i], rls[i]], [tAs[i]], eng="pool")

                    def e3():
                        kb.stt(ox.ap, tAs[1].ap, neglam[:, 0:1], tAs[0].ap, ALU.mult, ALU.add, [tAs[0], tAs[1], neglam], [ox])
                        kb.tt(sqx.ap, ox.ap, ox.ap, ALU.mult, [ox], [sqx], eng="pool")

                    def e4():
                        kb.mm(b[7].ap, ones_b.ap, sqx.ap, True, True, [ones_b, sqx], [b[7]])
                        kb.act(tmpR.ap, b[7].ap, AF.Sqrt, [b[7]], [tmpR], bias=1e-6, scale=1.0 / 128)

                    def e5():
                        kb.v(lambda e: e.reciprocal(out=rstd.ap, in_=tmpR.ap), [tmpR], [rstd])

                    def e6():
                        ot = ots.next()
                        kb.stt(ot.ap, ox.ap, subg[:, 0:1], rstd.ap, ALU.mult, ALU.mult, [ox, subg, rstd], [ot])
                        kb.dma("sp", OT[h * 128:(h + 1) * 128, col0:col0 + Wd], ot.ap, [ot], [])
                    return [e0, e1, e2, e3, e4, e5, e6]
                while pending_epi:
                    pending_epi.pop(0)()
                pending_epi.extend(mk_epi())
                if h == 7 and qb == nqb - 1:
                    while pending_epi:
                        pending_epi.pop(0)()


def layer_norm_tile(kb, y, gam, bet, out, st12, mv, rs1, neghalf):
    kb.v(lambda e: e.bn_stats(out=st12[:, 0:6], in_=y[:, 0:512]), [y], [st12])
    kb.v(lambda e: e.bn_stats(out=st12[:, 6:12], in_=y[:, 512:1024]), [y], [st12])
    kb.v(lambda e: e.bn_aggr(out=mv.ap, in_=st12.ap), [st12], [mv])
    kb.ts(rs1[:, 0:1], mv[:, 1:2], 1e-5, None, ALU.add, None, [mv], [rs1])
    kb.tt(rs1[:, 1:2], rs1[:, 0:1], neghalf[:, 0:1], ALU.pow, [rs1, neghalf], [rs1], eng="pool")
    kb.stt(rs1[:, 0:1], mv[:, 0:1], -1.0, rs1[:, 1:2], ALU.mult, ALU.mult, [mv, rs1], [rs1])
    kb.act(y.ap, y.ap, AF.Identity, [y, rs1], [y], bias=rs1[:, 0:1], scale=rs1[:, 1:2])
    kb.tt(y.ap, y.ap, gam.ap, ALU.mult, [y, gam], [y], eng="pool")
    kb.tt(out.ap, y.ap, bet.ap, ALU.add, [y, bet], [out])


def lockstep(gens):
    gens = list(gens)
    while gens:
        for g in list(gens):
            try:
                next(g)
            except StopIteration:
                gens.remove(g)


def phase_C(kb, E, C, L):
    b = kb.banks
    ntiles = 66 if L == 0 else 64
    nst = 2 if L == 0 else 1
    ident_b, ident_f, neghalf = C["ident_b"], C["ident_f"], C["neghalf"]
    wout = kb.tile([128, 8, 1024], BF16, "wout")
    kb.dma("pool", wout.ap, E[f"wout{L}"], [], [wout])
    router_f = kb.tile([128, 8, 16], F32, "router_f")
    kb.dma("sp", router_f.ap, E[f"router{L}"], [], [router_f])
    router_b = kb.tile([128, 8, 16], BF16, "router_b")
    kb.cp(router_b.ap, router_f.ap, [router_f], [router_b])
    gate = [bc_load(kb, E, L, st, 2, f"gate{st}", True) for st in range(nst)]
    G1 = kb.tile([128, 1024], F32, "G1")
    B1 = kb.tile([128, 1024], F32, "B1")
    G2 = [kb.tile([128, 1024], F32, f"G2_{st}") for st in range(nst)]
    B2 = [kb.tile([128, 1024], F32, f"B2_{st}") for st in range(nst)]
    mark = kb.off
    gam = bc_row(kb, E[f"lnmix{L}"][0, :], "gam")
    bet = bc_row(kb, E[f"lnmix{L}"][1, :], "bet")
    kb.ts(G1.ap, gam.ap, ALPHA, None, ALU.mult, None, [gam], [G1])
    kb.ts(B1.ap, bet.ap, ALPHA, None, ALU.mult, None, [bet], [B1])
    for st in range(nst):
        Af = bc_load(kb, E, L, st, 4, f"Af{st}", True)
        shf = bc_load(kb, E, L, st, 3, f"shf{st}")
        kb.tt(G2[st].ap, gam.ap, Af.ap, ALU.mult, [gam, Af], [G2[st]])
        kb.tt(B2[st].ap, bet.ap, Af.ap, ALU.mult, [bet, Af], [B2[st]])
        kb.tt(B2[st].ap, B2[st].ap, shf.ap, ALU.add, [B2[st], shf], [B2[st]])
    kb.s.barrier()
    kb.off = mark
    affx = [kb.tile([128, 16, 8], F32, f"affx{s}") for s in range(8)]
    for t in affx:
        kb.v(lambda e: e.memset(t.ap, 0.0), [], [t])
    catTs = Rot([kb.tile([128, 8, 128], BF16, f"catT{i}") for i in range(4)])
    xts = Rot([kb.tile([128, 1024], F32, f"xt{i}") for i in range(4)])

    def mk(shape, dt, nm):
        return [Rot([kb.tile(shape, dt, f"{nm}{sl}_{i}") for i in range(2)]) for sl in range(2)]
    tmps, ysC, x1as, t2s = mk([128, 1024], F32, "tmpC"), mk([128, 1024], F32, "yC"), mk([128, 1024], F32, "x1a"), mk([128, 1024], F32, "t2C")
    hfs = mk([128, 1024], BF16, "hf")
    hfTs = mk([128, 8, 128], BF16, "hfT")
    st12s, mvs, rs1s, sms, exs = (mk([128, 12], F32, "st12"), mk([128, 2], F32, "mvC"), mk([128, 2], F32, "rs1C"),
                                  mk([128, 4], F32, "sm"), mk([128, 16], F32, "ex"))
    affc_t = kb.tile([128, 16], F32, "affc_t")
    AT0v = E["AT0"].rearrange("(g c) t -> c g t", c=128)
    OTv = E["OT"].rearrange("(k p) t -> p k t", p=128)

    def loadsC(i):
        cols = slice(i * 128, (i + 1) * 128)
        catT = catTs.next()
        if L == 0:
            kb.dma("sp", catT[:, 0:4, :], AT0v[:, :, cols], [], [catT])
            kb.dma("sp", catT[:, 4:8, :], OTv[:, 0:4, cols], [], [catT])
        else:
            kb.dma("sp", catT.ap, OTv[:, :, cols], [], [catT])
        xt = xts.next()
        kb.dma("sp", xt.ap, rows_of(E, L, i), [], [xt])
        return catT, xt

    def tileA(i, sl, ld, out):
        st = 0 if i < 64 else 1
        catT, xt = ld
        tmp, y, st12, mv, rs1 = tmps[sl].next(), ysC[sl].next(), st12s[sl].next(), mvs[sl].next(), rs1s[sl].next()
        mb = (b[0], b[1]) if sl == 0 else (b[6], b[7])
        for half in range(2):
            hs = slice(half * 512, (half + 1) * 512)
            for k in range(8):
                kb.mm(mb[half].ap, catT[:, k, :], wout[:, k, hs], k == 0, k == 7, [catT, wout], [mb[half]], sig=(k == 7))
            yield
            kb.tt(tmp[:, hs], mb[half].ap, gate[st][:, hs], ALU.mult, [mb[half], gate[st]], [tmp])
            yield
        kb.stt(y.ap, xt.ap, ALPHA, tmp.ap, ALU.mult, ALU.add, [xt, tmp], [y])
        yield
        kb.v(lambda e: e.bn_stats(out=st12[:, 0:6], in_=y[:, 0:512]), [y], [st12])
        yield
        kb.v(lambda e: e.bn_stats(out=st12[:, 6:12], in_=y[:, 512:1024]), [y], [st12])
        yield
        kb.v(lambda e: e.bn_aggr(out=mv.ap, in_=st12.ap), [st12], [mv])
        kb.ts(rs1[:, 0:1], mv[:, 1:2], 1e-5, None, ALU.add, None, [mv], [rs1])
        yield
        kb.tt(rs1[:, 1:2], rs1[:, 0:1], neghalf[:, 0:1], ALU.pow, [rs1, neghalf], [rs1], eng="pool")
        yield
        kb.stt(rs1[:, 0:1], mv[:, 0:1], -1.0, rs1[:, 1:2], ALU.mult, ALU.mult, [mv, rs1], [rs1])
        yield
        kb.act(y.ap, y.ap, AF.Identity, [y, rs1], [y], bias=rs1[:, 0:1], scale=rs1[:, 1:2])
        yield
        x1a, t2, hf = x1as[sl].next(), t2s[sl].next(), hfs[sl].next()
        kb.tt(tmp.ap, y.ap, G1.ap, ALU.mult, [y, G1], [tmp], eng="pool")
        kb.tt(t2.ap, y.ap, G2[st].ap, ALU.mult, [y, G2[st]], [t2])
        yield
        kb.tt(hf.ap, t2.ap, B2[st].ap, ALU.add, [t2, B2[st]], [hf])
        kb.dma("sp", E["HF"][i * 128:(i + 1) * 128, :], hf.ap, [hf], [])
        yield
        kb.tt(x1a.ap, tmp.ap, B1.ap, ALU.add, [tmp, B1], [x1a])
        kb.dma("sp", E["FACC"][i * 128:(i + 1) * 128, :], x1a.ap, [x1a], [])
        out.append(hf)
        yield

    def tileB(i, sl, hf):
        hfT, sm, ex = hfTs[sl].next(), sms[sl].next(), exs[sl].next()
        tb = 2 + sl
        pb = kb.bank_bf(tb)
        for k in range(8):
            kb.tr(pb[:, k * 128:(k + 1) * 128], hf[:, k * 128:(k + 1) * 128], ident_b.ap, [hf, ident_b], [b[tb]], sig=(k == 7))
        yield
        kb.cp(hfT.ap, pb.rearrange("p (a b) -> p a b", a=8), [b[tb]], [hfT], eng="act")
        yield
        lg = b[tb][:, 512 - 16:512]
        for k in range(8):
            kb.mm(lg, hfT[:, k, :], router_b[:, k, :], k == 0, k == 7, [hfT, router_b], [b[tb]], sig=(k == 7))
        yield
        kb.v(lambda e: e.reduce_max(out=sm[:, 0:1], in_=lg, axis=mybir.AxisListType.X), [b[tb]], [sm])
        yield
        kb.ts(sm[:, 1:2], sm[:, 0:1], -1.0, None, ALU.mult, None, [sm], [sm])
        yield
        kb.act(ex.ap, lg, AF.Exp, [b[tb], sm], [ex, sm], bias=sm[:, 1:2], accum=sm[:, 2:3])
        yield
        kb.v(lambda e: e.reciprocal(out=sm[:, 3:4], in_=sm[:, 2:3]), [sm], [sm])
        yield
        if i < 64:
            s_, j = i % 8, i // 8
            ax = affx[s_]
            kb.ts(ax[:, :, s_], ex.ap, sm[:, 3:4], None, ALU.mult, None, [ex, sm], [ax])
            yield
            bk = b[4 + j // 4]
            kb.mm(bk[:, (j % 4) * 128:(j % 4 + 1) * 128], ax.ap.rearrange("p a b -> p (a b)"), ident_f.ap, s_ == 0, s_ == 7,
                  [ax, ident_f], [bk])
        else:
            kb.ts(affc_t.ap, ex.ap, sm[:, 3:4], None, ALU.mult, None, [ex, sm], [affc_t])
            yield
            kb.tr(b[2][0:16, (i - 64) * 128:(i - 63) * 128], affc_t.ap, ident_f.ap, [affc_t, ident_f], [b[2]])
            if i == 65:
                kb.cp(C["affc"].ap, b[2][0:16, 0:256], [b[2]], [C["affc"]])
        yield

    npairs = ntiles // 2
    lds = [loadsC(0), loadsC(1)]
    prev = None
    for p in range(npairs):
        cur_ld = lds
        if p + 1 < npairs:
            lds = [loadsC(2 * p + 2), loadsC(2 * p + 3)]
        outs = [[], []]
        lockstep([tileA(2 * p, 0, cur_ld[0], outs[0]), tileA(2 * p + 1, 1, cur_ld[1], outs[1])])
        if prev is not None:
            lockstep([tileB(2 * (p - 1), 0, prev[0][0]), tileB(2 * (p - 1) + 1, 1, prev[1][0])])
        prev = outs
    lockstep([tileB(2 * (npairs - 1), 0, prev[0][0]), tileB(2 * (npairs - 1) + 1, 1, prev[1][0])])
    kb.cp(C["affT"][:, 0:512], b[4].ap, [b[4]], [C["affT"]])
    kb.cp(C["affT"][:, 512:1024], b[5].ap, [b[5]], [C["affT"]])


def phase_D(kb, E, C, L):
    b = kb.banks
    has_ctx = (L == 0)
    nst = 2 if L == 0 else 1
    ident_b, ident_f, mc, bd = C["ident_b"], C["ident_f"], C["mc"], C["bd"]
    affT = C["affT"]
    gatef = [bc_load(kb, E, L, st, 5, f"gatef{st}", True) for st in range(nst)]
    Wt = [[kb.tile([128, 8, 1024], BF16, f"w{nm}{i}") for nm in ("gate", "up", "down")] for i in range(2)]

    def load_w(e_, bi):
        for wi, nm in enumerate(("gate", "up", "down")):
            kb.dma("pool", Wt[bi][wi].ap, E[f"w{nm}{L}"][e_].rearrange("(k p) n -> p k n", p=128), [], [Wt[bi][wi]])

    load_w(0, 0)
    work = kb.tile([128, 1024], F32, "work")
    kb.cp(work.ap, affT.ap, [affT], [work])
    vals = kb.tile([128, CAP], F32, "vals")
    idxu = kb.tile([128, CAP], U32, "idxu")
    for it in range(CAP // 8):
        sl = slice(it * 8, (it + 1) * 8)
        kb.v(lambda e: e.max(out=vals[:, sl], in_=work.ap), [work], [vals])
        kb.v(lambda e: e.max_index(out=idxu[:, sl], in_max=vals[:, sl], in_values=work.ap), [work, vals], [idxu])
        kb.v(lambda e: e.match_replace(out=work.ap, in_to_replace=vals[:, sl], in_values=work.ap, imm_value=-1.0), [vals, work], [work])
    lo = kb.tile([128, 1], F32, "lo")
    hi = kb.tile([128, 1], F32, "hi")
    mid = kb.tile([128, 1], F32, "mid")
    cnt = kb.tile([128, 1], F32, "cnt")
    d1 = kb.tile([128, 1], F32, "d1")
    ge = kb.tile([128, 1], F32, "ge")
    junk = kb.tile([128, CAP], F32, "junk")
    kb.v(lambda e: e.memset(lo.ap, 0.0), [], [lo])
    kb.v(lambda e: e.memset(hi.ap, 1.0), [], [hi])
    for it in range(32):
        kb.tt(mid.ap, lo.ap, hi.ap, ALU.add, [lo, hi], [mid])
        kb.ts(mid.ap, mid.ap, 0.5, None, ALU.mult, None, [mid], [mid])
        kb.ts(junk.ap, vals.ap, mid[:, 0:1], 0.0, ALU.is_ge, ALU.add, [vals, mid], [junk, cnt], accum=cnt.ap)
        kb.mm(b[0][:, 0:1], bd.ap, cnt.ap, True, True, [bd, cnt], [b[0]])
        kb.ts(ge.ap, b[0][:, 0:1], ECAP - 0.5, None, ALU.is_ge, None, [b[0]], [ge])
        kb.tt(d1.ap, mid.ap, lo.ap, ALU.subtract, [mid, lo], [d1])
        kb.stt(lo.ap, d1.ap, ge[:, 0:1], lo.ap, ALU.mult, ALU.add, [d1, ge, lo], [lo])
        kb.tt(d1.ap, hi.ap, mid.ap, ALU.subtract, [hi, mid], [d1])
        kb.stt(hi.ap, d1.ap, ge[:, 0:1], mid.ap, ALU.mult, ALU.add, [d1, ge, mid], [hi])
    valid = kb.tile([128, CAP], F32, "valid")
    gvv = kb.tile([128, CAP], F32, "gvv")
    kb.ts(valid.ap, vals.ap, lo[:, 0:1], None, ALU.is_ge, None, [vals, lo], [valid])
    kb.tt(gvv.ap, vals.ap, valid.ap, ALU.mult, [vals, valid], [gvv])
    jbi = kb.tile([128, CAP], I32, "jbi")
    kb.v(lambda e: e.tensor_single_scalar(out=jbi.ap, in_=idxu.ap.bitcast(I32), scalar=7, op=ALU.arith_shift_right), [idxu], [jbi])
    idxf = kb.tile([128, CAP], F32, "idxf")
    jbf = kb.tile([128, CAP], F32, "jbf")
    tokf = kb.tile([128, CAP], F32, "tokf")
    kb.cp(idxf.ap, idxu.ap, [idxu], [idxf])
    kb.cp(jbf.ap, jbi.ap, [jbi], [jbf])
    kb.stt(tokf.ap, jbf.ap, 896.0, idxf.ap, ALU.mult, ALU.add, [jbf, idxf], [tokf])
    kb.ts(tokf.ap, tokf.ap, mc[:, 0:1], None, ALU.add, None, [tokf, mc], [tokf])
    padL = kb.tile([128, 128], F32, "padL")
    padR = kb.tile([128, 128], F32, "padR")
    kb.v(lambda e: e.memset(padL.ap, 0.0), [], [padL])
    kb.v(lambda e: e.memset(padR.ap, 0.0), [], [padR])
    mains, tails = [], []
    for ai, arr in enumerate((tokf, gvv, valid)):
        bk = b[1 + ai]
        kb.cp(padL[:, 0:64], arr[:, 128:192], [arr], [padL])
        kb.cp(padR[:, 64:128], arr[:, 128:192], [arr], [padR])
        kb.tr(bk[:, 0:128], arr[:, 0:128], ident_f.ap, [arr, ident_f], [bk])
        kb.tr(bk[:, 128:256], padL.ap, ident_f.ap, [padL, ident_f], [bk])
        kb.tr(bk[:, 256:384], padR.ap, ident_f.ap, [padR, ident_f], [bk])
        m_ = kb.tile([128, 128], F32, f"main{ai}")
        t_ = kb.tile([128, 16, 4], F32, f"tail{ai}")
        kb.cp(m_.ap, bk[:, 0:128], [bk], [m_])
        kb.cp(t_.ap, bk[:, 128:256].rearrange("p (e k two) -> p e k two", e=16, k=4)[:, :, :, 0], [bk], [t_])
        kb.tt(t_.ap, t_.ap, bk[:, 256:384].rearrange("p (e k two) -> p e k two", e=16, k=4)[:, :, :, 1], ALU.add, [bk, t_], [t_])
        mains.append(m_)
        tails.append(t_)
    IDXM = kb.tile([128, 128], I32, "IDXM")
    IDXT = kb.tile([128, 64], I32, "IDXT")
    for src, dst, n_ in ((mains, IDXM, 128), (tails, IDXT, 64)):
        tk = src[0].ap if n_ == 128 else src[0].ap.rearrange("p a b -> p (a b)")
        vl = src[2].ap if n_ == 128 else src[2].ap.rearrange("p a b -> p (a b)")
        tmpi = kb.tile([128, n_], F32, "tmpi")
        kb.ts(tmpi.ap, tk, mc[:, 1:2], None, ALU.subtract, None, [src[0], mc], [tmpi])
        kb.tt(tmpi.ap, tmpi.ap, vl, ALU.mult, [tmpi, src[2]], [tmpi])
        kb.ts(tmpi.ap, tmpi.ap, mc[:, 1:2], None, ALU.add, None, [tmpi, mc], [tmpi])
        kb.cp(dst.ap, tmpi.ap, [tmpi], [dst])
    GM = mains[1]
    GT = tails[1]
    if has_ctx:
        cwork = kb.tile([16, 256], F32, "cwork")
        kb.cp(cwork.ap, C["affc"].ap, [C["affc"]], [cwork])
        cvals = kb.tile([16, CCAP], F32, "cvals")
        cidx = kb.tile([16, CCAP], U32, "cidx")
        for it in range(CCAP // 8):
            sl = slice(it * 8, (it + 1) * 8)
            kb.v(lambda e: e.max(out=cvals[:, sl], in_=cwork.ap), [cwork], [cvals])
            kb.v(lambda e: e.max_index(out=cidx[:, sl], in_max=cvals[:, sl], in_values=cwork.ap), [cwork, cvals], [cidx])
            kb.v(lambda e: e.match_replace(out=cwork.ap, in_to_replace=cvals[:, sl], in_values=cwork.ap, imm_value=-1.0), [cvals, cwork], [cwork])
        cidf = kb.tile([16, CCAP], F32, "cidf")
        kb.cp(cidf.ap, cidx.ap, [cidx], [cidf])
        kb.ts(cidf.ap, cidf.ap, float(NLAT), None, ALU.add, None, [cidf], [cidf])
        kb.tr(b[4][0:32, 0:16], cidf.ap, ident_f[0:16, 0:16], [cidf, ident_f], [b[4]])
        kb.tr(b[4][0:32, 16:32], cvals.ap, ident_f[0:16, 0:16], [cvals, ident_f], [b[4]])
        CIDX = kb.tile([32, 16], I32, "CIDX")
        CG = kb.tile([32, 16], F32, "CG")
        kb.cp(CIDX.ap, b[4][0:32, 0:16], [b[4]], [CIDX])
        kb.cp(CG.ap, b[4][0:32, 16:32], [b[4]], [CG])
    xss = Rot([kb.tile([128, 1024], BF16, f"xs{i}") for i in range(8)])
    xsTs = Rot([kb.tile([128, 8, 512], BF16, f"xsT{i}") for i in range(2)])
    hidTs = Rot([kb.tile([128, 8, 512], BF16, f"hidT{i}") for i in range(2)])
    sgs = Rot([kb.tile([128, 512], F32, f"sg{i}") for i in range(2)])
    ysbs = Rot([kb.tile([128, 1024], F32, f"ysb{i}") for i in range(3)])

    work_items = []
    for e_ in range(16):
        chunks = [(IDXM[:, e_ * 8 + s_:e_ * 8 + s_ + 1], GM[:, e_ * 8 + s_:e_ * 8 + s_ + 1], 128, 0, IDXM, GM) for s_ in range(8)]
        chunks += [(IDXT[:, e_ * 4 + k:e_ * 4 + k + 1], GT[:, e_, k:k + 1], 128, 0, IDXT, GT) for k in range(4)]
        blocks = [chunks[0:4], chunks[4:8], chunks[8:12]]
        if has_ctx:
            blocks.append([(CIDX[:, e_:e_ + 1], CG[:, e_:e_ + 1], 32, 1, CIDX, CG)])
        for bi_, blk in enumerate(blocks):
            work_items.append((e_, bi_, blk))

    def gathers(blk):
        res = []
        R = blk[0][2]
        for (icol, gcol, R_, st, it_, gt_) in blk:
            xs = xss.next()
            kb.s.dma("pool", lambda q: q.indirect_dma_start(out=xs[0:R, :], out_offset=None, in_=E["HF"],
                                                             in_offset=bass.IndirectOffsetOnAxis(ap=icol, axis=0)),
                     _toks([it_]), _toks([xs]))
            res.append(xs)
        return res

    prev_sc = []
    cur_sc = []
    g_next = gathers(work_items[0][2])
    for wi_, (e_, bi_, blk) in enumerate(work_items):
        bi = e_ % 2
        if bi_ == 0:
            if e_ + 1 < 16:
                load_w(e_ + 1, (e_ + 1) % 2)
            prev_sc = cur_sc
            cur_sc = []
        wg, wu, wd = Wt[bi]
        R = blk[0][2]
        Wd = R * len(blk)
        xs_list = g_next
        xsT = xsTs.next()
        hidT = hidTs.next()
        for cl, xs in enumerate(xs_list):
            pb = kb.bank_bf(0)
            for k in range(8):
                kb.tr(pb[:, k * 128:k * 128 + R], xs[0:R, k * 128:(k + 1) * 128], ident_b[0:R, 0:R], [xs, ident_b], [b[0]], sig=(k == 7))
            kb.cp(xsT[:, :, cl * R:(cl + 1) * R], pb.rearrange("p (a b) -> p a b", a=8)[:, :, 0:R], [b[0]], [xsT],
                  eng=("act" if cl % 2 else "dve"))
        if wi_ + 1 < len(work_items):
            g_next = gathers(work_items[wi_ + 1][2])
        for ffc in range(8):
            fs = slice(ffc * 128, (ffc + 1) * 128)
            G_ = b[1 + ffc % 2]
            U_ = b[3 + ffc % 2]
            for k in range(8):
                kb.mm(G_[:, 0:Wd], wg[:, k, fs], xsT[:, k, 0:Wd], k == 0, k == 7, [wg, xsT], [G_], sig=(k == 7))
            for k in range(8):
                kb.mm(U_[:, 0:Wd], wu[:, k, fs], xsT[:, k, 0:Wd], k == 0, k == 7, [wu, xsT], [U_], sig=(k == 7))
            sg = sgs.next()
            kb.act(sg[:, 0:Wd], G_[:, 0:Wd], AF.Silu, [G_], [sg])
            kb.tt(hidT[:, ffc, 0:Wd], sg[:, 0:Wd], U_[:, 0:Wd], ALU.mult, [sg, U_], [hidT])
        for cl, (icol, gcol, R_, st, it_, gt_) in enumerate(blk):
            ysb = ysbs.next()
            for half in range(2):
                hs = slice(half * 512, (half + 1) * 512)
                Y_ = b[5 + half]
                for ffc in range(8):
                    kb.mm(Y_[0:R, :], hidT[:, ffc, cl * R:(cl + 1) * R], wd[:, ffc, hs], ffc == 0, ffc == 7, [hidT, wd], [Y_], sig=(ffc == 7))
                kb.stt(ysb[0:R, hs], Y_[0:R, :], gcol, gatef[st][0:R, hs], ALU.mult, ALU.mult, [Y_, gt_, gatef[st]], [ysb])
            tk = Tok("sc")
            kb.s.dma("pool", lambda q: q.indirect_dma_start(out=E["FACC"], out_offset=bass.IndirectOffsetOnAxis(ap=icol, axis=0),
                                                             in_=ysb[0:R, :], in_offset=None, compute_op=ALU.add),
                     _toks([ysb, it_]) + prev_sc, [tk])
            cur_sc.append(tk)


def phase_E(kb, E, C):
    neghalf = C["neghalf"]
    gam = bc_row(kb, E["lnffn1"][0, :], "gamE")
    bet = bc_row(kb, E["lnffn1"][1, :], "betE")
    ys = Rot([kb.tile([128, 1024], F32, f"yE{i}") for i in range(4)])
    outs = Rot([kb.tile([128, 1024], F32, f"oE{i}") for i in range(4)])
    st12 = kb.tile([128, 12], F32, "st12E")
    mv = kb.tile([128, 2], F32, "mvE")
    rs1 = kb.tile([128, 2], F32, "rs1E")
    def loadE(i):
        y = ys.next()
        kb.dma("sp", y.ap, E["FACC"][i * 128:(i + 1) * 128, :], [], [y])
        return y

    st12s = [kb.tile([128, 12], F32, f"st12E{i}") for i in range(2)]
    mvs = [kb.tile([128, 2], F32, f"mvE{i}") for i in range(2)]
    rs1s = [kb.tile([128, 2], F32, f"rs1E{i}") for i in range(2)]

    def tileE(i, sl, y):
        o = outs.next()
        st12_, mv_, rs1_ = st12s[sl], mvs[sl], rs1s[sl]
        kb.v(lambda e: e.bn_stats(out=st12_[:, 0:6], in_=y[:, 0:512]), [y], [st12_])
        yield
        kb.v(lambda e: e.bn_stats(out=st12_[:, 6:12], in_=y[:, 512:1024]), [y], [st12_])
        yield
        kb.v(lambda e: e.bn_aggr(out=mv_.ap, in_=st12_.ap), [st12_], [mv_])
        kb.ts(rs1_[:, 0:1], mv_[:, 1:2], 1e-5, None, ALU.add, None, [mv_], [rs1_])
        yield
        kb.tt(rs1_[:, 1:2], rs1_[:, 0:1], neghalf[:, 0:1], ALU.pow, [rs1_, neghalf], [rs1_], eng="pool")
        yield
        kb.stt(rs1_[:, 0:1], mv_[:, 0:1], -1.0, rs1_[:, 1:2], ALU.mult, ALU.mult, [mv_, rs1_], [rs1_])
        yield
        kb.act(y.ap, y.ap, AF.Identity, [y, rs1_], [y], bias=rs1_[:, 0:1], scale=rs1_[:, 1:2])
        yield
        kb.tt(y.ap, y.ap, gam.ap, ALU.mult, [y, gam], [y], eng="pool")
        yield
        kb.tt(o.ap, y.ap, bet.ap, ALU.add, [y, bet], [o])
        kb.dma("sp", E["out"][i * 128:(i + 1) * 128, :], o.ap, [o], [])
        yield

    lds = [loadE(0), loadE(1)]
    for p in range(32):
        cur = lds
        if p + 1 < 32:
            lds = [loadE(2 * p + 2), loadE(2 * p + 3)]
        lockstep([tileE(2 * p, 0, cur[0]), tileE(2 * p + 1, 1, cur[1])])


def phase_A1(kb, E, C):
    b = kb.banks
    modT = C["modT"][1]
    ident_b, ones_b, ones_f, neghalf = C["ident_b"], C["ones_b"], C["ones_f"], C["neghalf"]
    l4 = kb.tile([1, 256], F32, "l4")
    kb.dma("sp", l4.ap, E["lam4"], [], [l4])
    lp = kb.tile([1, 128], F32, "lp")
    lsum = kb.tile([1, 4], F32, "lsum")
    kb.tt(lp[:, 0:64], l4[:, 0:64], l4[:, 64:128], ALU.mult, [l4], [lp])
    kb.tt(lp[:, 64:128], l4[:, 128:192], l4[:, 192:256], ALU.mult, [l4], [lp])
    kb.v(lambda e: e.reduce_sum(out=lsum[:, 0:1], in_=lp[:, 0:64], axis=mybir.AxisListType.X), [lp], [lsum])
    kb.v(lambda e: e.reduce_sum(out=lsum[:, 1:2], in_=lp[:, 64:128], axis=mybir.AxisListType.X), [lp], [lsum])
    kb.act(lsum[:, 2:4], lsum[:, 0:2], AF.Exp, [lsum], [lsum])
    kb.tt(lsum[:, 0:1], lsum[:, 3:4], lsum[:, 2:3], ALU.subtract, [lsum], [lsum])
    kb.ts(lsum[:, 1:2], lsum[:, 0:1], -LAM_INIT, None, ALU.add, None, [lsum], [lsum])
    kb.mm(b[0][:, 0:1], ones_f[0:1, :], lsum[:, 1:2], True, True, [ones_f, lsum], [b[0]])
    kb.cp(C["neglam"].ap, b[0][:, 0:1], [b[0]], [C["neglam"]])
    shc = kb.tile([128, 8, 2], BF16, "shc1")
    kb.cp(shc.ap, modT[:, 0:8, :], [modT], [shc])
    A1 = kb.tile([128, 8, 2], F32, "A1_1")
    kb.ts(A1.ap, modT[:, 8:16, :], 1.0, None, ALU.add, None, [modT], [A1])
    W = [kb.tile([128, 8, 3072], BF16, f"W1_{st}") for st in range(2)]
    bcol = kb.tile([128, 16, 2], F32, "bcol1")
    brow = [kb.tile([1, 1024], BF16, f"brow1_{st}") for st in range(2)]
    mark = kb.off
    wtmp = kb.tile([128, 8, 1024], F32, "wtmp1")
    Wun = kb.tile([128, 8, 1024], BF16, "Wun1")
    for third in range(3):
        cs_ = slice(third * 1024, (third + 1) * 1024)
        kb.dma("sp", wtmp.ap, E["win1"][:, :, cs_], [], [wtmp])
        for k in range(8):
            kb.cp(Wun[:, k, :], wtmp[:, k, :], [wtmp], [Wun], eng=("act" if k % 2 else "dve"))
        for st in range(2):
            for k in range(8):
                kb.ts(W[st][:, k, cs_], wtmp[:, k, :], A1[:, k, st:st + 1], None, ALU.mult, None, [wtmp, A1], [W[st]],
                      eng=("pool" if k % 2 else "dve"))
        if third < 2:
            for m in range(8):
                mi = third * 8 + m
                for k in range(8):
                    kb.mm(b[0][:, mi * 2:mi * 2 + 2], Wun[:, k, m * 128:(m + 1) * 128], shc[:, k, :], k == 0, k == 7, [Wun, shc], [b[0]], sig=(k == 7))
        else:
            for st in range(2):
                for half in range(2):
                    for k in range(8):
                        kb.mm(b[1 + half][0:1, :], shc[:, k, st:st + 1], Wun[:, k, half * 512:(half + 1) * 512], k == 0, k == 7,
                              [Wun, shc], [b[1 + half]], sig=(k == 7))
                    kb.cp(brow[st][:, half * 512:(half + 1) * 512], b[1 + half][0:1, :], [b[1 + half]], [brow[st]])
    kb.cp(bcol.ap, b[0][:, 0:32].rearrange("p (a b) -> p a b", a=16), [b[0]], [bcol])
    kb.s.barrier()
    kb.off = mark
    gam = bc_row(kb, E["lnffn0"][0, :], "gamA1")
    bet = bc_row(kb, E["lnffn0"][1, :], "betA1")
    ys = Rot([kb.tile([128, 1024], F32, f"yA{i}") for i in range(3)])
    x2s = Rot([kb.tile([128, 1024], F32, f"x2_{i}") for i in range(2)])
    xbs = Rot([kb.tile([128, 1024], BF16, f"xbA1_{i}") for i in range(2)])
    st12 = kb.tile([128, 12], F32, "st12A")
    mv = kb.tile([128, 2], F32, "mvA")
    rs1 = kb.tile([128, 2], F32, "rs1A")
    xTs = Rot([kb.tile([128, 8, 512], BF16, f"xTA{i}") for i in range(2)])
    cosTs = Rot([kb.tile([128, 512], F32, f"cosT{i}") for i in range(2)])
    sinTs = Rot([kb.tile([128, 512], F32, f"sinT{i}") for i in range(2)])
    qbs = Rot([kb.tile([128, 512], BF16, f"qb{i}") for i in range(3)])
    tAs = Rot([kb.tile([128, 512], F32, f"tA1_{i}") for i in range(2)])
    tBs = Rot([kb.tile([128, 512], F32, f"tB1_{i}") for i in range(2)])
    outs = Rot([kb.tile([128, 512], BF16, f"qk{i}") for i in range(3)])
    vaugs = Rot([kb.tile([128, 8, 128], BF16, f"vaug1_{i}") for i in range(2)])
    psw_f = kb.tile([128, 128], F32, "psw_f")
    kb.dma("sp", psw_f.ap, E["psw"], [], [psw_f])
    psw = kb.tile([128, 128], BF16, "psw")
    kb.cp(psw.ap, psw_f.ap, [psw_f], [psw])
    V1v = E["V1"].rearrange("h p kt d -> p h kt d")

    def loadA(i):
        y = ys.next()
        kb.dma("sp", y.ap, E["FACC"][i * 128:(i + 1) * 128, :], [], [y])
        return y

    def stageL(gi):
        ntl = 4 if gi < 16 else 2
        xT = xTs.next()

        def one(tl):
            i = gi * 4 + tl
            if i == 0:
                ybox[0] = loadA(0)
            y = ybox[0]
            if i + 1 < 66:
                ybox[0] = loadA(i + 1)
            x2 = x2s.next()
            layer_norm_tile(kb, y, gam, bet, x2, st12, mv, rs1, neghalf)
            if i < 64:
                kb.dma("sp", E["X2"][i * 128:(i + 1) * 128, :], x2.ap, [x2], [])
            xb = xbs.next()
            kb.cp(xb.ap, x2.ap, [x2], [xb], eng="act")
            pb = kb.bank_bf(0)
            for k in range(8):
                kb.tr(pb[:, k * 128:(k + 1) * 128], xb[:, k * 128:(k + 1) * 128], ident_b.ap, [xb, ident_b], [b[0]], sig=(k == 7))
            kb.cp(xT[:, :, tl * 128:(tl + 1) * 128], pb.rearrange("p (a b) -> p a b", a=8), [b[0]], [xT], eng=("act" if tl % 2 else "dve"))
        return xT, [(lambda tl=tl: one(tl)) for tl in range(ntl)]

    def stageP(gi, xT, Lsteps):
        ntl = 4 if gi < 16 else 2
        Wd = ntl * 128
        st = 0 if gi < 16 else 1
        col0 = gi * 512
        cosT, sinT = cosTs.next(), sinTs.next()
        kb.dma("sp", cosT[:, 0:Wd], E["cos1"][:, col0:col0 + Wd], [], [cosT])
        kb.dma("sp", sinT[:, 0:Wd], E["sin1"][:, col0:col0 + Wd], [], [sinT])

        def finish(mi, qb_):
            br = b[5 + mi % 2]
            kb.mm(br[:, 0:Wd], psw.ap, qb_[:, 0:Wd], True, True, [psw, qb_], [br])
            tA_ = tAs.next()
            kb.tt(tA_[:, 0:Wd], qb_[:, 0:Wd], cosT[:, 0:Wd], ALU.mult, [qb_, cosT], [tA_], eng="pool")
            tB_ = tBs.next()
            kb.tt(tB_[:, 0:Wd], br[:, 0:Wd], sinT[:, 0:Wd], ALU.mult, [br, sinT], [tB_])
            o = outs.next()
            kb.tt(o[:, 0:Wd], tA_[:, 0:Wd], tB_[:, 0:Wd], ALU.add, [tA_, tB_], [o])
            dst_d = E["QT"] if mi < 8 else E["KT"]
            kb.dma("sp", dst_d[mi % 8][:, col0:col0 + Wd], o[:, 0:Wd], [o], [])

        pend = None
        pbanks = (b[1], b[2], b[7])
        for mi in range(16):
            bk = pbanks[mi % 3]
            for k in range(8):
                kb.mm(bk[:, 0:Wd], W[st][:, k, mi * 128:(mi + 1) * 128], xT[:, k, 0:Wd], k == 0, k == 7, [W[st], xT], [bk], sig=(k == 7))
            qb_ = qbs.next()
            kb.act(qb_[:, 0:Wd], bk[:, 0:Wd], AF.Identity, [bk, bcol], [qb_], bias=bcol[:, mi, st:st + 1])
            if pend is not None:
                finish(*pend)
            pend = (mi, qb_)
            if mi % 4 == 3 and Lsteps:
                Lsteps.pop(0)()
        finish(*pend)
        for tl in range(ntl):
            i = gi * 4 + tl
            ts_ = slice(tl * 128, (tl + 1) * 128)
            va = vaugs.next()
            for half in range(2):
                bk = b[3 + half]
                for k in range(8):
                    kb.mm(bk.ap, xT[:, k, ts_], W[st][:, k, 2048 + half * 512:2048 + (half + 1) * 512], k == 0, False, [xT, W[st]], [bk], sig=False)
                kb.mm(bk.ap, ones_b[0:1, 0:128], brow[st][:, half * 512:(half + 1) * 512], False, True, [ones_b, brow[st]], [bk])
                kb.cp(va[:, half * 4:(half + 1) * 4, :], bk.ap.rearrange("p (a b) -> p a b", a=4), [bk], [va], eng=("act" if half else "dve"))
            kb.dma("sp", V1v[:, :, i, :], va.ap, [va], [])
        while Lsteps:
            Lsteps.pop(0)()

    ybox = [None]
    xT_cur, steps = stageL(0)
    for f_ in steps:
        f_()
    for gi in range(17):
        if gi + 1 < 17:
            xT_next, steps = stageL(gi + 1)
        else:
            xT_next, steps = None, []
        stageP(gi, xT_cur, steps)
        xT_cur = xT_next


PHASES = ["mod", "A0", "B0", "C0", "D0", "A1", "B1", "C1", "D1", "E"]


def build(dbg=False, stop_after=None, only=None):
    kb = KB(dbg, stop_after)
    E = {}
    inp = kb.inp
    E["x"] = inp("x", [NLAT, D])
    E["ctx"] = inp("ctx", [NCTX, D])
    E["ccT"] = inp("ccT", [128, 8, 2])
    E["ident"] = inp("ident", [128, 128])
    E["mconst"] = inp("mconst", [128, 4])
    E["bdiag"] = inp("bdiag", [128, 128])
    E["cs0"] = inp("cs0", [64, NALL])
    E["cos1"] = inp("cos1", [128, NALL])
    E["sin1"] = inp("sin1", [128, NALL])
    for l in range(2):
        E[f"wmod{l}"] = inp(f"wmod{l}", [12, 128, 8, 512])
        E[f"bmod{l}"] = inp(f"bmod{l}", [1, 6144])
        E[f"bmodT{l}"] = inp(f"bmodT{l}", [128, 48])
        E[f"wout{l}"] = inp(f"wout{l}", [128, 8, 1024])
        E[f"router{l}"] = inp(f"router{l}", [128, 8, 16])
        E[f"lnmix{l}"] = inp(f"lnmix{l}", [2, 1024])
        E[f"lnffn{l}"] = inp(f"lnffn{l}", [2, 1024])
        for nm in ("gate", "up", "down"):
            E[f"w{nm}{l}"] = inp(f"w{nm}{l}", [16, 1024, 1024])
    E["win0"] = inp("win0", [128, 8, 1472])
    E["wuqx"] = inp("wuqx", [128, 2, 1024])
    E["wukx"] = inp("wukx", [128, 1024])
    E["wuv"] = inp("wuv", [128, 512])
    E["qnorm"] = inp("qnorm", [128, 2])
    E["kvnorm"] = inp("kvnorm", [128, 1])
    E["gln"] = inp("gln", [128, 8])
    E["wsT"] = inp("wsT", [128, 512])
    E["gbs"] = inp("gbs", [1, 512])
    E["win1"] = inp("win1", [128, 8, 3072])
    E["lam4"] = inp("lam4", [1, 256])
    E["subln"] = inp("subln", [128, 1])
    E["psw"] = inp("psw", [128, 128])
    sc = kb.scratch
    E["MOD"] = sc("MOD", [2, 2, 6144], F32)
    E["AT0"] = sc("AT0", [512, NALL], BF16)
    E["QT"] = sc("QT", [8, 128, NALL], BF16)
    E["KT"] = sc("KT", [8, 128, NALL], BF16)
    E["V0"] = sc("V0", [8, 128, 66, 128], BF16)
    E["V1"] = sc("V1", [8, 128, 66, 128], BF16)
    E["OT"] = sc("OT", [1024, NALL], BF16)
    E["FACC"] = sc("FACC", [NPAD, D], F32)
    E["HF"] = sc("HF", [NPAD, D], BF16)
    E["X2"] = sc("X2", [NLAT, D], F32)
    E["out"] = kb.nc.dram_tensor("out", [NLAT, D], F32, kind="ExternalOutput").ap()
    C = setup_consts(kb, E)
    fns = {
        "mod": lambda: phase_mod(kb, E, C),
        "A0": lambda: phase_A0(kb, E, C),
        "B0": lambda: phase_attn(kb, E, C, 0),
        "C0": lambda: phase_C(kb, E, C, 0),
        "D0": lambda: phase_D(kb, E, C, 0),
        "A1": lambda: phase_A1(kb, E, C),
        "B1": lambda: phase_attn(kb, E, C, 1),
        "C1": lambda: phase_C(kb, E, C, 1),
        "D1": lambda: phase_D(kb, E, C, 1),
        "E": lambda: phase_E(kb, E, C),
    }
    for ph in PHASES:
        if only is None or ph in only:
            fns[ph]()
            kb.phase()
        if ph == stop_after:
            break
    kb.s.barrier()
    return kb


def _rope_tables():
    t = np.arange(NLAT)
    row = (t // 64).astype(np.float32)
    col = (t % 64).astype(np.float32)

    def ang(dim):
        nf = dim // 4
        inv = (np.float32(10000.0) ** (-np.arange(nf, dtype=np.float32) / np.float32(nf))).astype(np.float32)
        return np.concatenate([row[:, None] * inv, col[:, None] * inv], -1).astype(np.float32)

    a0 = ang(32)
    c0, s0 = np.cos(a0).T, np.sin(a0).T
    cs0 = np.zeros((64, NALL), np.float32)
    cs0[0:32, NLAT:] = 1.0
    cs0[0:16, :NLAT] = c0
    cs0[16:32, :NLAT] = c0
    cs0[32:48, :NLAT] = -s0
    cs0[48:64, :NLAT] = s0
    a1 = ang(64)
    c1, s1 = np.cos(a1).T, np.sin(a1).T
    cos1 = np.ones((128, NALL), np.float32)
    sin1 = np.zeros((128, NALL), np.float32)
    for blk in range(4):
        cos1[blk * 32:(blk + 1) * 32, :NLAT] = c1
        sin1[blk * 32:(blk + 1) * 32, :NLAT] = (-s1 if blk % 2 == 0 else s1)
    return cs0, cos1, sin1


def _pk(w):
    K = w.shape[0] // 128
    return np.ascontiguousarray(w.reshape(K, 128, -1).transpose(1, 0, 2))


def prep_shared(I):
    f = lambda a: np.ascontiguousarray(np.asarray(a, dtype=np.float32))
    S = {}
    S["ident"] = np.eye(128, dtype=np.float32)
    p = np.arange(128)
    mc = np.zeros((128, 4), np.float32)
    mc[:, 0] = 128 * (p % 8)
    mc[:, 1] = NALL + p
    S["mconst"] = mc
    S["bdiag"] = (p[:, None] // 8 == p[None, :] // 8).astype(np.float32)
    S["cs0"], S["cos1"], S["sin1"] = _rope_tables()
    for l in range(2):
        wm = f(I[f"w_mod_{l}"])
        S[f"wmod{l}"] = np.ascontiguousarray(wm.reshape(8, 128, 12, 512).transpose(2, 1, 0, 3))
        bm = f(I[f"b_mod_{l}"])
        S[f"bmod{l}"] = bm.reshape(1, 6144)
        S[f"bmodT{l}"] = np.ascontiguousarray(bm.reshape(48, 128).T)
        S[f"wout{l}"] = _pk(f(I[f"w_out_{l}"]))
        S[f"router{l}"] = _pk(f(I[f"router_{l}"]))
        S[f"lnmix{l}"] = np.stack([f(I[f"ln_mix_g_{l}"]), f(I[f"ln_mix_b_{l}"])])
        S[f"lnffn{l}"] = np.stack([f(I[f"ln_ffn_g_{l}"]), f(I[f"ln_ffn_b_{l}"])])
        for nm in ("gate", "up", "down"):
            S[f"w{nm}{l}"] = f(I[f"w_{nm}_{l}"])
    w = f(I["w_in_0"])
    kr = w[:, 1408:1440]
    krE, krO = kr[:, 0::2], kr[:, 1::2]
    S["win0"] = _pk(np.concatenate([w[:, 0:512], w[:, 1024:1280], w[:, 1280:1408], krE, krO, krO, krE, w[:, 512:1024]], 1))
    wuq = f(I["mla_w_uq_0"])
    blocks = []
    for h in range(8):
        nope = wuq[:, h * 96:h * 96 + 64]
        rp = wuq[:, h * 96 + 64:h * 96 + 96]
        rE, rO = rp[:, 0::2], rp[:, 1::2]
        blocks.append(np.concatenate([rE, rO, rO, rE, nope], 1))
    S["wuqx"] = _pk(np.concatenate(blocks, 1))
    wukv = f(I["mla_w_ukv_0"])
    S["wukx"] = np.ascontiguousarray(np.concatenate(
        [np.concatenate([np.zeros((128, 64), np.float32), wukv[:, h * 128:h * 128 + 64]], 1) for h in range(8)], 1))
    S["wuv"] = np.ascontiguousarray(np.concatenate([wukv[:, h * 128 + 64:h * 128 + 128] for h in range(8)], 1))
    S["qnorm"] = np.ascontiguousarray(f(I["mla_q_norm_0"]).reshape(2, 128).T)
    S["kvnorm"] = f(I["mla_kv_norm_0"]).reshape(128, 1)
    S["gln"] = np.ascontiguousarray(np.concatenate([f(I["gmlp_ln_g_0"]).reshape(4, 128).T, f(I["gmlp_ln_b_0"]).reshape(4, 128).T], 1))
    S["wsT"] = np.ascontiguousarray(f(I["gmlp_ws_0"]).transpose(2, 0, 1).reshape(128, 512))
    S["gbs"] = f(I["gmlp_bs_0"]).reshape(1, 512)
    w1 = f(I["w_in_1"])
    cols = []
    for part in range(2):
        for j in range(16):
            blk = w1[:, part * 1024 + j * 64:part * 1024 + (j + 1) * 64]
            cols += [blk[:, 0::2], blk[:, 1::2]]
    cols.append(w1[:, 2048:3072])
    S["win1"] = _pk(np.concatenate(cols, 1))
    S["lam4"] = np.concatenate([f(I["lambda_q1_1"]), f(I["lambda_k1_1"]), f(I["lambda_q2_1"]), f(I["lambda_k2_1"])]).reshape(1, 256)
    S["subln"] = f(I["subln_g_1"]).reshape(128, 1)
    S["psw"] = np.eye(128, dtype=np.float32)[:, np.arange(128) ^ 32]
    return S


def prep_core(I, S, bidx):
    m = dict(S)
    m["x"] = np.ascontiguousarray(np.asarray(I["x"][bidx], dtype=np.float32))
    m["ctx"] = np.ascontiguousarray(np.asarray(I["ctx"][bidx], dtype=np.float32))
    cc = np.stack([np.asarray(I["c"][bidx], np.float32), np.asarray(I["c_ctx"], np.float32)], -1)
    m["ccT"] = np.ascontiguousarray(cc.reshape(8, 128, 2).transpose(1, 0, 2))
    return m


_KB_CACHE = {}


def kernel(**inputs):
    if "kb" not in _KB_CACHE:
        _KB_CACHE["kb"] = build()
    kb = _KB_CACHE["kb"]
    S = prep_shared(inputs)
    in_maps = [prep_core(inputs, S, bidx) for bidx in range(8)]
    res = run_bass_kernel_spmd(kb.nc, in_maps, core_ids=list(range(8)))
    return np.stack([np.asarray(r["out"], dtype=np.float32) for r in res.results], 0)
```

```python
import math
import numpy as np
import concourse.bass as bass
import concourse.mybir as mybir
from concourse.bass_utils import run_bass_kernel_spmd

F32 = mybir.dt.float32
BF16 = mybir.dt.bfloat16
I32 = mybir.dt.int32
U32 = mybir.dt.uint32
ALU = mybir.AluOpType
AF = mybir.ActivationFunctionType

D = 1024
NLAT = 8192
NCTX = 256
NALL = NLAT + NCTX
NPAD = NALL + 128
ALPHA = 4 ** 0.25
LAM_INIT = 0.8 - 0.6 * math.exp(-0.3)
CAP = 192
ECAP = 1024
CCAP = 32
AW = 50000


class Tok:
    __slots__ = ("w", "r", "name")

    def __init__(self, name=""):
        self.w = None
        self.r = {}
        self.name = name


class Tile:
    def __init__(self, ap, name=""):
        self.ap = ap
        self.tok = Tok(name)

    def __getitem__(self, k):
        return self.ap[k]


def _toks(xs):
    return [x.tok if isinstance(x, Tile) else x for x in xs]


class Sched:
    EPOCH = 24000
    NDMA = 48

    def __init__(self, nc):
        self.nc = nc
        self.eng = {"pe": nc.tensor, "act": nc.scalar, "dve": nc.vector, "pool": nc.gpsimd, "sp": nc.sync}
        self.cnt = {e: 0 for e in self.eng}
        self.sems = {e: [] for e in self.eng}
        self.known = {e: {} for e in self.eng}
        self.pending = {e: [] for e in self.eng}
        self.dma_sems = [nc.alloc_semaphore(f"dq{i}") for i in range(self.NDMA)]
        self.dma_val = [0] * self.NDMA
        self.dma_next = 0

    def _sem(self, e, seq):
        k = (seq - 1) // self.EPOCH
        while len(self.sems[e]) <= k:
            self.sems[e].append(self.nc.alloc_semaphore(f"c_{e}_{len(self.sems[e])}"))
        return self.sems[e][k], (seq - 1) % self.EPOCH + 1

    def _wait(self, e, ev):
        if ev is None:
            return
        kind, src, val = ev
        if kind == "eng":
            if src == e and e == "pe":
                return
            key = ("eng", src)
            if self.known[e].get(key, 0) >= val:
                return
            if src == e and val <= self.cnt[e] - 2:
                return
            sem, v = self._sem(src, val)
            self.eng[e].wait_ge(sem, v)
            self.known[e][key] = val
        else:
            key = ("dma", src)
            if self.known[e].get(key, 0) >= val:
                return
            self.eng[e].wait_ge(self.dma_sems[src], val)
            self.known[e][key] = val

    def _deps(self, e, reads, writes):
        for t in reads:
            self._wait(e, t.w)
        for t in writes:
            self._wait(e, t.w)
            for ev in list(t.r.values()):
                self._wait(e, ev)

    def op(self, e, fn, reads=(), writes=(), sig=True):
        reads = _toks(reads)
        writes = _toks(writes)
        self._deps(e, reads, writes)
        ins = fn(self.eng[e])
        if not sig:
            self.pending[e].append((reads, writes))
            return ins
        self.cnt[e] += 1
        seq = self.cnt[e]
        sem, v = self._sem(e, seq)
        ins.then_inc(sem, 1)
        me = ("eng", e, seq)
        groups = self.pending[e] + [(reads, writes)]
        self.pending[e] = []
        for rs, ws in groups:
            for t in rs:
                t.r[("eng", e)] = me
            for t in ws:
                t.w = me
                t.r = {}
        return ins

    def dma(self, q, fn, reads=(), writes=()):
        reads = _toks(reads)
        writes = _toks(writes)
        self._deps(q, reads, writes)
        k = self.dma_next
        self.dma_next = (k + 1) % self.NDMA
        if self.dma_val[k] > 0:
            self._wait(q, ("dma", k, self.dma_val[k]))
        ins = fn(self.eng[q])
        self.dma_val[k] += 16
        ins.then_inc(self.dma_sems[k], 16)
        me = ("dma", k, self.dma_val[k])
        for t in reads:
            t.r[("dma", k)] = me
        for t in writes:
            t.w = me
            t.r = {}
        return ins

    def barrier(self, engines=None):
        engines = engines or list(self.eng)
        for e in self.eng:
            assert not self.pending[e]
        for e in engines:
            for f in self.eng:
                if f != e and self.cnt[f] > 0:
                    self._wait(e, ("eng", f, self.cnt[f]))
            for k in range(self.NDMA):
                if self.dma_val[k] > 0:
                    self._wait(e, ("dma", k, self.dma_val[k]))


def _dsize(dt):
    return 2 if dt == BF16 else 4


class KB:
    def __init__(self, dbg=False, stop_after=None):
        self.dbg = dbg
        self.stop_after = stop_after
        nc = self.nc = bass.Bass("TRN2", target_bir_lowering=False)
        self.s = Sched(nc)
        self.arena = nc.alloc_sbuf_tensor("arena", [128, AW], F32)
        self.off = 0
        self.persist = 0
        self.psum = nc.alloc_psum_tensor("psum_all", [128, 4096], F32)
        self.banks = [Tile(self.psum[:, i * 512:(i + 1) * 512], f"bank{i}") for i in range(8)]
        self.ext = {}
        self.outs = {}

    def tile(self, shape, dt=F32, name=""):
        P = shape[0]
        n = 1
        for d in shape[1:]:
            n *= d
        words = (n * _dsize(dt) + 3) // 4
        words = (words + 7) // 8 * 8
        assert self.off + words <= AW, f"arena overflow {name} {self.off}+{words}"
        ap = self.arena[0:P, self.off:self.off + words]
        self.off += words
        if dt != F32:
            ap = ap.bitcast(dt)
        ap = ap[:, 0:n]
        if len(shape) == 3:
            ap = ap.rearrange("p (a b) -> p a b", a=shape[1])
        elif len(shape) == 4:
            ap = ap.rearrange("p (a b c) -> p a b c", a=shape[1], b=shape[2])
        return Tile(ap, name)

    def phase(self):
        self.s.barrier()
        self.off = self.persist
        for b in self.banks:
            b.tok = Tok(b.tok.name)

    def keep(self):
        self.persist = self.off

    def bank_bf(self, i):
        return self.banks[i].ap.bitcast(BF16)

    def bank2(self, i):
        return self.psum[:, i * 512:(i + 2) * 512]

    def inp(self, name, shape, dt=F32):
        t = self.nc.dram_tensor(name, list(shape), dt, kind="ExternalInput")
        self.ext[name] = (tuple(shape), dt)
        return t.ap()

    def scratch(self, name, shape, dt):
        if self.dbg:
            t = self.nc.dram_tensor(name, list(shape), dt, kind="ExternalOutput")
            self.outs[name] = (tuple(shape), dt)
        else:
            t = self.nc.dram_tensor(name, list(shape), dt)
        return t.ap()

    def dma(self, q, out, in_, reads=(), writes=(), **kw):
        return self.s.dma(q, lambda e: e.dma_start(out=out, in_=in_, **kw), reads, writes)

    def mm(self, out, lhsT, rhs, start, stop, reads, writes, sig=True):
        return self.s.op("pe", lambda e: e.matmul(out, lhsT=lhsT, rhs=rhs, start=start, stop=stop), reads, writes, sig)

    def tr(self, out, in_, ident, reads, writes, sig=True):
        return self.s.op("pe", lambda e: e.transpose(out=out, in_=in_, identity=ident), reads, writes, sig)

    def act(self, out, in_, func, reads, writes, bias=0.0, scale=1.0, accum=None):
        if accum is None:
            return self.s.op("act", lambda e: e.activation(out=out, in_=in_, func=func, bias=bias, scale=scale), reads, writes)
        return self.s.op("act", lambda e: e.activation(out=out, in_=in_, func=func, bias=bias, scale=scale, accum_out=accum), reads, writes)

    def v(self, fn, reads, writes):
        return self.s.op("dve", fn, reads, writes)

    def g(self, fn, reads, writes):
        return self.s.op("pool", fn, reads, writes)

    def tt(self, out, a, b, op, reads, writes, eng="dve"):
        return self.s.op(eng, lambda e: e.tensor_tensor(out=out, in0=a, in1=b, op=op), reads, writes)

    def ts(self, out, a, s1, s2, op0, op1, reads, writes, eng="dve", accum=None):
        if accum is not None:
            return self.s.op(eng, lambda e: e.tensor_scalar(out=out, in0=a, scalar1=s1, scalar2=s2, op0=op0, op1=op1, accum_out=accum), reads, writes)
        if s2 is None:
            return self.s.op(eng, lambda e: e.tensor_scalar(out=out, in0=a, scalar1=s1, scalar2=None, op0=op0), reads, writes)
        return self.s.op(eng, lambda e: e.tensor_scalar(out=out, in0=a, scalar1=s1, scalar2=s2, op0=op0, op1=op1), reads, writes)

    def stt(self, out, a, sc, b, op0, op1, reads, writes, eng="dve"):
        return self.s.op(eng, lambda e: e.scalar_tensor_tensor(out=out, in0=a, scalar=sc, in1=b, op0=op0, op1=op1), reads, writes)

    def rsq(self, out, in_, scale, eps, tmp, reads, writes):
        self.act(tmp, in_, AF.Sqrt, reads, writes, bias=eps, scale=scale)
        self.s.op("dve", lambda e: e.reciprocal(out=out, in_=tmp), _toks(writes), _toks(writes))

    def cp(self, out, in_, reads, writes, eng="dve"):
        if eng == "act":
            return self.s.op("act", lambda e: e.copy(out=out, in_=in_), reads, writes)
        return self.s.op(eng, lambda e: e.tensor_copy(out=out, in_=in_), reads, writes)

    def rsqrt(self, out, in_, scale, eps, reads, writes, tmp, neghalf):
        self.ts(tmp.ap if isinstance(tmp, Tile) else tmp, in_, scale, eps, ALU.mult, ALU.add, reads, [tmp])
        self.tt(out, tmp.ap if isinstance(tmp, Tile) else tmp, neghalf, ALU.pow, [tmp], writes, eng="pool")


class Rot:
    def __init__(self, tiles):
        self.t = tiles
        self.i = -1

    def next(self):
        self.i = (self.i + 1) % len(self.t)
        return self.t[self.i]


MCH0 = [(0, 128), (128, 256), (256, 384), (384, 512), (512, 640), (640, 768), (768, 896), (896, 960)]


def rows_of(E, L, i):
    if L == 0:
        if i < 64:
            return E["x"][i * 128:(i + 1) * 128, :]
        return E["ctx"][(i - 64) * 128:(i - 63) * 128, :]
    return E["X2"][i * 128:(i + 1) * 128, :]


def setup_consts(kb, E):
    C = {}
    C["ident_f"] = kb.tile([128, 128], F32, "ident_f")
    C["ident_b"] = kb.tile([128, 128], BF16, "ident_b")
    C["ones_b"] = kb.tile([128, 128], BF16, "ones_b")
    C["ones_f"] = kb.tile([128, 128], F32, "ones_f")
    C["neghalf"] = kb.tile([128, 512], F32, "neghalf")
    C["mc"] = kb.tile([128, 4], F32, "mc")
    C["bd"] = kb.tile([128, 128], F32, "bd")
    C["affT"] = kb.tile([128, 1024], F32, "affT")
    C["affc"] = kb.tile([16, 256], F32, "affc")
    C["modT"] = [kb.tile([128, 48, 2], F32, f"modT{l}") for l in range(2)]
    C["neglam"] = kb.tile([128, 1], F32, "neglam")
    kb.dma("sp", C["ident_f"].ap, E["ident"], [], [C["ident_f"]])
    kb.dma("sp", C["mc"].ap, E["mconst"], [], [C["mc"]])
    kb.dma("sp", C["bd"].ap, E["bdiag"], [], [C["bd"]])
    kb.cp(C["ident_b"].ap, C["ident_f"].ap, [C["ident_f"]], [C["ident_b"]])
    kb.v(lambda e: e.memset(C["ones_b"].ap, 1.0), [], [C["ones_b"]])
    kb.v(lambda e: e.memset(C["ones_f"].ap, 1.0), [], [C["ones_f"]])
    kb.v(lambda e: e.memset(C["neghalf"].ap, -0.5), [], [C["neghalf"]])
    kb.keep()
    return C


def phase_mod(kb, E, C):
    b = kb.banks
    cc = kb.tile([128, 8, 2], F32, "cc")
    sc = kb.tile([128, 8, 2], F32, "sc")
    kb.dma("sp", cc.ap, E["ccT"], [], [cc])
    kb.act(sc.ap, cc.ap, AF.Silu, [cc], [sc])
    wts = Rot([kb.tile([128, 8, 512], F32, f"wmod{i}") for i in range(2)])
    for l in range(2):
        brow = kb.tile([2, 6144], F32, "brow")
        bT = kb.tile([128, 48], F32, "bT")
        modsb = kb.tile([2, 6144], F32, "modsb")
        kb.dma("sp", brow[0:1, :], E[f"bmod{l}"], [], [brow])
        kb.dma("sp", brow[1:2, :], E[f"bmod{l}"], [], [brow])
        kb.dma("sp", bT.ap, E[f"bmodT{l}"], [], [bT])
        for j in range(12):
            wt = wts.next()
            kb.dma("sp", wt.ap, E[f"wmod{l}"][j], [], [wt])
            bk = b[j % 2]
            for k in range(8):
                kb.mm(bk[0:2, :], sc[:, k, :], wt[:, k, :], k == 0, k == 7, [sc, wt], [bk], sig=(k == 7))
            kb.tt(modsb[:, j * 512:(j + 1) * 512], bk[0:2, :], brow[:, j * 512:(j + 1) * 512], ALU.add, [bk, brow], [modsb])
            for q in range(4):
                c48 = j * 4 + q
                if c48 >= 16:
                    continue
                bk2 = b[2 + c48 % 2]
                for k in range(8):
                    kb.mm(bk2[:, 0:2], wt[:, k, q * 128:(q + 1) * 128], sc[:, k, :], k == 0, k == 7, [sc, wt], [bk2], sig=(k == 7))
                kb.ts(C["modT"][l][:, c48, :], bk2[:, 0:2], bT[:, c48:c48 + 1], None, ALU.add, None, [bk2, bT], [C["modT"][l]])
        kb.dma("sp", E["MOD"][l], modsb.ap, [modsb], [])


def bc_load(kb, E, l, st, ch, name, plus1=False):
    t = kb.tile([128, 1024], F32, name)
    src = E["MOD"][l][st, ch * 1024:(ch + 1) * 1024].partition_broadcast(128)
    kb.dma("sp", t.ap, src, [], [t])
    if plus1:
        kb.ts(t.ap, t.ap, 1.0, None, ALU.add, None, [t], [t], eng="pool")
    return t


def bc_row(kb, src_row, name):
    t = kb.tile([128, 1024], F32, name)
    kb.dma("sp", t.ap, src_row.partition_broadcast(128), [], [t])
    return t


def phase_A0(kb, E, C):
    b = kb.banks
    modT = C["modT"][0]
    ident_b, ones_b, ones_f, neghalf = C["ident_b"], C["ones_b"], C["ones_f"], C["neghalf"]
    W = [kb.tile([128, 8, 1472], BF16, f"W{st}") for st in range(2)]
    bcol = kb.tile([128, 8, 2], F32, "bcol")
    brow = [kb.tile([1, 512], BF16, f"brow{st}") for st in range(2)]
    Wq = kb.tile([128, 2, 1024], BF16, "Wq")
    Wk = kb.tile([128, 1024], BF16, "Wk")
    Wv = kb.tile([128, 512], BF16, "Wv")
    wsT = kb.tile([128, 512], BF16, "wsT")
    gln = kb.tile([128, 8], F32, "gln")
    Rt = kb.tile([128, 512], F32, "Rt")
    mark = kb.off
    wtmp = kb.tile([128, 8, 1472], F32, "wtmp")
    kb.dma("sp", wtmp.ap, E["win0"], [], [wtmp])
    Wun = kb.tile([128, 8, 1472], BF16, "Wun")
    for k in range(8):
        kb.cp(Wun[:, k, :], wtmp[:, k, :], [wtmp], [Wun], eng=("act" if k % 2 else "dve"))
    shc = kb.tile([128, 8, 2], BF16, "shc")
    kb.cp(shc.ap, modT[:, 0:8, :], [modT], [shc])
    A1 = kb.tile([128, 8, 2], F32, "A1")
    kb.ts(A1.ap, modT[:, 8:16, :], 1.0, None, ALU.add, None, [modT], [A1])
    for st in range(2):
        for k in range(8):
            if k % 2:
                kb.act(W[st][:, k, :], wtmp[:, k, :], AF.Identity, [wtmp, A1], [W[st]], scale=A1[:, k, st:st + 1])
            else:
                kb.ts(W[st][:, k, :], wtmp[:, k, :], A1[:, k, st:st + 1], None, ALU.mult, None, [wtmp, A1], [W[st]])
    for mi, (a, bb) in enumerate(MCH0):
        for k in range(8):
            kb.mm(b[0][0:bb - a, mi * 2:mi * 2 + 2], Wun[:, k, a:bb], shc[:, k, :], k == 0, k == 7, [Wun, shc], [b[0]], sig=(k == 7))
    kb.cp(bcol[:, 0:7, :], b[0][:, 0:14].rearrange("p (a b) -> p a b", a=7), [b[0]], [bcol])
    kb.cp(bcol[0:64, 7, :], b[0][0:64, 14:16], [b[0]], [bcol])
    for st in range(2):
        for k in range(8):
            kb.mm(b[1][0:1, :], shc[:, k, st:st + 1], Wun[:, k, 960:1472], k == 0, k == 7, [Wun, shc], [b[1]], sig=(k == 7))
        kb.cp(brow[st].ap, b[1][0:1, :], [b[1]], [brow[st]])
    qn = kb.tile([128, 2], F32, "qn")
    kvn = kb.tile([128, 1], F32, "kvn")
    kb.dma("sp", qn.ap, E["qnorm"], [], [qn])
    kb.dma("sp", kvn.ap, E["kvnorm"], [], [kvn])
    wq_f = kb.tile([128, 2, 1024], F32, "wq_f")
    kb.dma("sp", wq_f.ap, E["wuqx"], [], [wq_f])
    for r in range(2):
        kb.ts(Wq[:, r, :], wq_f[:, r, :], qn[:, r:r + 1], None, ALU.mult, None, [wq_f, qn], [Wq])
    wk_f = kb.tile([128, 1024], F32, "wk_f")
    kb.dma("sp", wk_f.ap, E["wukx"], [], [wk_f])
    kb.ts(Wk.ap, wk_f.ap, kvn[:, 0:1], None, ALU.mult, None, [wk_f, kvn], [Wk])
    wv_f = kb.tile([128, 512], F32, "wv_f")
    kb.dma("sp", wv_f.ap, E["wuv"], [], [wv_f])
    kb.ts(Wv.ap, wv_f.ap, kvn[:, 0:1], None, ALU.mult, None, [wv_f, kvn], [Wv])
    wsT_f = kb.tile([128, 512], F32, "wsT_f")
    kb.dma("sp", wsT_f.ap, E["wsT"], [], [wsT_f])
    kb.cp(wsT.ap, wsT_f.ap, [wsT_f], [wsT])
    kb.dma("sp", gln.ap, E["gln"], [], [gln])
    bs_bc = kb.tile([128, 512], F32, "bs_bc")
    kb.dma("sp", bs_bc.ap, E["gbs"][0, :].partition_broadcast(128), [], [bs_bc])
    kb.mm(b[2].ap, ones_f.ap, wsT_f.ap, True, True, [ones_f, wsT_f], [b[2]])
    for g in range(4):
        gs = slice(g * 128, (g + 1) * 128)
        kb.stt(Rt[:, gs], b[2][:, gs], gln[:, 4 + g:5 + g], bs_bc[:, gs], ALU.mult, ALU.add, [b[2], gln, bs_bc], [Rt])
    kb.s.barrier()
    kb.off = mark
    xTs = Rot([kb.tile([128, 8, 512], BF16, f"xT{i}") for i in range(2)])
    xbs = Rot([kb.tile([128, 1024], BF16, f"xb{i}") for i in range(8)])
    guT = kb.tile([128, 4, 512], BF16, "guT")
    tq = kb.tile([128, 2, 512], F32, "tq")
    tkv = kb.tile([128, 512], F32, "tkv")
    sq = kb.tile([128, 3, 512], BF16, "sq")
    krx = kb.tile([64, 512], F32, "krx")
    css = Rot([kb.tile([64, 512], F32, f"cs{i}") for i in range(2)])
    tmpA = kb.tile([128, 512], F32, "tmpA")
    rstd_q = kb.tile([128, 512], F32, "rstd_q")
    rstd_kv = kb.tile([128, 512], F32, "rstd_kv")
    cqn = kb.tile([128, 2, 512], BF16, "cqn")
    ckvn = kb.tile([128, 512], BF16, "ckvn")
    t1 = kb.tile([64, 512], F32, "t1")
    t2 = kb.tile([32, 512], F32, "t2")
    krr = kb.tile([32, 512], BF16, "krr")
    kts = Rot([kb.tile([128, 512], BF16, f"kt{i}") for i in range(2)])
    qts = Rot([kb.tile([128, 512], BF16, f"qt{i}") for i in range(2)])
    vaugs = Rot([kb.tile([128, 8, 128], BF16, f"vaug{i}") for i in range(2)])
    for t in kts.t + qts.t:
        kb.v(lambda e: e.memset(t.ap, 0.0), [], [t])
    for t in vaugs.t:
        kb.v(lambda e: e.memset(t.ap, 1.0), [], [t])
    gv = kb.tile([128, 512], F32, "gv")
    st6 = kb.tile([128, 6], F32, "st6")
    mv = kb.tile([128, 2], F32, "mv")
    rs1 = kb.tile([128, 2], F32, "rs1")
    vhat = kb.tile([128, 512], BF16, "vhat")
    tmpg = kb.tile([128, 512], F32, "tmpg")
    aTs = Rot([kb.tile([128, 4, 128], BF16, f"aT{i}") for i in range(2)])
    AT0v = E["AT0"].rearrange("(g c) t -> c g t", c=128)
    V0v = E["V0"].rearrange("h p kt d -> p h kt d")

    def loadX(gi_):
        res = []
        for tl_ in range(4 if gi_ < 16 else 2):
            xb_ = xbs.next()
            kb.dma("pool", xb_.ap, rows_of(E, 0, gi_ * 4 + tl_), [], [xb_])
            res.append(xb_)
        return res

    def stageL(gi):
        ntl = 4 if gi < 16 else 2
        Wd = ntl * 128
        st = 0 if gi < 16 else 1
        col0 = gi * 512
        xT = xTs.next()
        if gi == 0:
            xbox[0] = loadX(0)
        xb_cur = xbox[0]
        if gi + 1 < 17:
            xbox[0] = loadX(gi + 1)
        for tl in range(ntl):
            i = gi * 4 + tl
            xb = xb_cur[tl]
            pb = kb.bank_bf(0)
            for k in range(8):
                kb.tr(pb[:, k * 128:(k + 1) * 128], xb[:, k * 128:(k + 1) * 128], ident_b.ap, [xb, ident_b], [b[0]], sig=(k == 7))
            kb.cp(xT[:, :, tl * 128:(tl + 1) * 128], pb.rearrange("p (a b) -> p a b", a=8), [b[0]], [xT], eng=("act" if tl % 2 else "dve"))
        return xT

    def stageP(gi, xT):
        ntl = 4 if gi < 16 else 2
        Wd = ntl * 128
        st = 0 if gi < 16 else 1
        col0 = gi * 512
        cs = css.next()
        kb.dma("sp", cs[:, 0:Wd], E["cs0"][:, col0:col0 + Wd], [], [cs])
        for mi, (a, bb) in enumerate(MCH0):
            M_ = bb - a
            bk = b[1 + mi % 2]
            for k in range(8):
                kb.mm(bk[0:M_, 0:Wd], W[st][:, k, a:bb], xT[:, k, 0:Wd], k == 0, k == 7, [W[st], xT], [bk], sig=(k == 7))
            bias = bcol[0:M_, mi, st:st + 1]
            if mi < 4:
                kb.act(guT[:, mi, 0:Wd], bk[:, 0:Wd], AF.Gelu, [bk, bcol], [guT], bias=bias)
            elif mi < 6:
                r = mi - 4
                kb.act(tq[:, r, 0:Wd], bk[:, 0:Wd], AF.Identity, [bk, bcol], [tq], bias=bias)
                kb.act(sq[:, r, 0:Wd], bk[:, 0:Wd], AF.Square, [bk, bcol], [sq], bias=bias)
            elif mi == 6:
                kb.act(tkv[:, 0:Wd], bk[:, 0:Wd], AF.Identity, [bk, bcol], [tkv], bias=bias)
                kb.act(sq[:, 2, 0:Wd], bk[:, 0:Wd], AF.Square, [bk, bcol], [sq], bias=bias)
            else:
                kb.act(krx[:, 0:Wd], bk[0:64, 0:Wd], AF.Identity, [bk, bcol], [krx], bias=bias)
        kb.mm(b[5][:, 0:Wd], ones_b.ap, sq[:, 0, 0:Wd], True, False, [ones_b, sq], [b[5]], sig=False)
        kb.mm(b[5][:, 0:Wd], ones_b.ap, sq[:, 1, 0:Wd], False, True, [ones_b, sq], [b[5]])
        kb.rsq(rstd_q[:, 0:Wd], b[5][:, 0:Wd], 1.0 / 256, 1e-6, tmpA[:, 0:Wd], [b[5]], [tmpA, rstd_q])
        kb.mm(b[5][:, 0:Wd], ones_b.ap, sq[:, 2, 0:Wd], True, True, [ones_b, sq], [b[5]])
        kb.rsq(rstd_kv[:, 0:Wd], b[5][:, 0:Wd], 1.0 / 128, 1e-6, tmpA[:, 0:Wd], [b[5]], [tmpA, rstd_kv])
        for r in range(2):
            kb.tt(cqn[:, r, 0:Wd], tq[:, r, 0:Wd], rstd_q[:, 0:Wd], ALU.mult, [tq, rstd_q], [cqn])
        kb.tt(ckvn[:, 0:Wd], tkv[:, 0:Wd], rstd_kv[:, 0:Wd], ALU.mult, [tkv, rstd_kv], [ckvn])
        kb.tt(t1[0:32, 0:Wd], krx[0:32, 0:Wd], cs[0:32, 0:Wd], ALU.mult, [krx, cs], [t1])
        kb.tt(t2[0:32, 0:Wd], krx[32:64, 0:Wd], cs[32:64, 0:Wd], ALU.mult, [krx, cs], [t2])
        kb.tt(krr[:, 0:Wd], t1[0:32, 0:Wd], t2[0:32, 0:Wd], ALU.add, [t1, t2], [krr])
        for h in range(8):
            bk = b[6 + h % 2]
            kb.mm(bk[:, 0:Wd], Wk[:, h * 128:(h + 1) * 128], ckvn[:, 0:Wd], True, True, [Wk, ckvn], [bk])
            kt = kts.next()
            kb.cp(kt[64:128, 0:Wd], bk[64:128, 0:Wd], [bk], [kt], eng="act")
            kb.cp(kt[0:32, 0:Wd], krr[:, 0:Wd], [krr], [kt], eng="pool")
            kb.dma("sp", E["KT"][h][:, col0:col0 + Wd], kt[:, 0:Wd], [kt], [])
        for h in range(8):
            bk = b[6 + h % 2]
            for r in range(2):
                kb.mm(bk[:, 0:Wd], Wq[:, r, h * 128:(h + 1) * 128], cqn[:, r, 0:Wd], r == 0, r == 1, [Wq, cqn], [bk], sig=(r == 1))
            qt = qts.next()
            kb.cp(qt[64:128, 0:Wd], bk[64:128, 0:Wd], [bk], [qt], eng="act")
            kb.tt(t1[0:32, 0:Wd], bk[0:32, 0:Wd], cs[0:32, 0:Wd], ALU.mult, [bk, cs], [t1])
            kb.tt(t2[0:32, 0:Wd], bk[32:64, 0:Wd], cs[32:64, 0:Wd], ALU.mult, [bk, cs], [t2])
            kb.tt(qt[0:32, 0:Wd], t1[0:32, 0:Wd], t2[0:32, 0:Wd], ALU.add, [t1, t2], [qt])
            kb.dma("sp", E["QT"][h][:, col0:col0 + Wd], qt[:, 0:Wd], [qt], [])
        for tl in range(ntl):
            i = gi * 4 + tl
            ts_ = slice(tl * 128, (tl + 1) * 128)
            kb.mm(b[3].ap, ckvn[:, ts_], Wv.ap, True, True, [ckvn, Wv], [b[3]])
            va = vaugs.next()
            kb.cp(va[:, :, 0:64], b[3].ap.rearrange("p (a b) -> p a b", a=8), [b[3]], [va], eng="act")
            kb.dma("sp", V0v[:, :, i, :], va.ap, [va], [])
            for k in range(8):
                kb.mm(b[4].ap, xT[:, k, ts_], W[st][:, k, 960:1472], k == 0, False, [xT, W[st]], [b[4]], sig=False)
            kb.mm(b[4].ap, ones_b[0:1, 0:128], brow[st].ap, False, True, [ones_b, brow[st]], [b[4]])
            kb.act(gv.ap, b[4].ap, AF.Gelu, [b[4]], [gv])
            kb.v(lambda e: e.bn_stats(out=st6.ap, in_=gv.ap), [gv], [st6])
            kb.v(lambda e: e.bn_aggr(out=mv.ap, in_=st6.ap), [st6], [mv])
            kb.ts(rs1[:, 0:1], mv[:, 1:2], 1e-5, None, ALU.add, None, [mv], [rs1])
            kb.tt(rs1[:, 1:2], rs1[:, 0:1], neghalf[:, 0:1], ALU.pow, [rs1, neghalf], [rs1], eng="pool")
            kb.ts(vhat.ap, gv.ap, mv[:, 0:1], rs1[:, 1:2], ALU.subtract, ALU.mult, [gv, mv, rs1], [vhat])
            for g in range(4):
                gs = slice(g * 128, (g + 1) * 128)
                kb.mm(b[5][:, gs], vhat[:, gs], wsT[:, gs], True, True, [vhat, wsT], [b[5]], sig=(g == 3))
            for g in range(4):
                gs = slice(g * 128, (g + 1) * 128)
                kb.stt(tmpg[:, gs], b[5][:, gs], gln[:, g:g + 1], Rt[:, gs], ALU.mult, ALU.add, [b[5], gln, Rt], [tmpg])
            aT = aTs.next()
            kb.tt(aT.ap, tmpg.ap.rearrange("p (a b) -> p a b", a=4), guT[:, :, ts_], ALU.mult, [tmpg, guT], [aT])
            kb.dma("sp", AT0v[:, :, i * 128:(i + 1) * 128], aT.ap, [aT], [])

    xbox = [None]
    xT_prev = None
    for gi in range(17):
        xT_cur = stageL(gi)
        if xT_prev is not None:
            stageP(gi - 1, xT_prev)
        xT_prev = xT_cur
    stageP(16, xT_prev)


def phase_attn(kb, E, C, L):
    b = kb.banks
    ones_b, ones_f = C["ones_b"], C["ones_f"]
    scale = (96 ** -0.5) if L == 0 else 0.125
    nqb = 17 if L == 0 else 16
    Vd = E["V0"] if L == 0 else E["V1"]
    OT = E["OT"]
    KTb = [kb.tile([128, NALL], BF16, f"KTb{i}") for i in range(2)]
    Vb = [kb.tile([128, 66, 128], BF16, f"Vb{i}") for i in range(2)]
    qts = Rot([kb.tile([128, 512], BF16, f"aq{i}") for i in range(2)])
    if L == 0:
        Ps = Rot([kb.tile([128, 512], BF16, f"P{j}") for j in range(3)])
        rl = [kb.tile([128, 512], F32, f"rl{i}") for i in range(2)]
        ots = Rot([kb.tile([64, 512], BF16, f"ot{i}") for i in range(3)])
    else:
        Ps = Rot([kb.tile([128, 2, 512], BF16, f"P12_{j}") for j in range(4)])
        accs = Rot([kb.tile([128, 512], F32, f"acc{j}") for j in range(2)])
        rls = [kb.tile([128, 512], F32, f"rl{i}") for i in range(2)]
        tAs = [kb.tile([128, 512], F32, f"tA{i}") for i in range(2)]
        ox = kb.tile([128, 512], F32, "ox")
        osb = [kb.tile([128, 512], F32, f"osb{i}") for i in range(2)]
        l2s = kb.tile([128, 512], F32, "l2s")
        sqx = kb.tile([128, 512], BF16, "sqx")
        tmpR = kb.tile([128, 512], F32, "tmpR")
        rstd = kb.tile([128, 512], F32, "rstdo")
        ots = Rot([kb.tile([128, 512], BF16, f"ot{i}") for i in range(2)])
        subg = kb.tile([128, 1], F32, "subg")
        kb.dma("sp", subg.ap, E["subln"], [], [subg])
        kb.ts(subg.ap, subg.ap, 1.0 - LAM_INIT, None, ALU.mult, None, [subg], [subg])
        neglam = C["neglam"]

    def load_head(h, bi):
        kb.dma("sp", KTb[bi].ap, E["KT"][h], [], [KTb[bi]])
        kb.dma("sp", Vb[bi].ap, Vd[h], [], [Vb[bi]])

    def loadQ(h_, qb_):
        Wd_ = 512 if qb_ < 16 else 256
        qt_ = qts.next()
        kb.dma("sp", qt_[:, 0:Wd_], E["QT"][h_][:, qb_ * 512:qb_ * 512 + Wd_], [], [qt_])
        return qt_

    pending_epi = []
    load_head(0, 0)
    for h in range(8):
        bi = h % 2
        if h + 1 < 8:
            load_head(h + 1, (h + 1) % 2)
        K_ = KTb[bi]
        V_ = Vb[bi]
        for qb in range(nqb):
            Wd = 512 if qb < 16 else 256
            col0 = qb * 512
            ktl = list(range(66)) if qb < 16 else [64, 65]
            n = len(ktl)
            if h == 0 and qb == 0:
                qt_next = loadQ(0, 0)
            qt = qt_next
            if qb + 1 < nqb:
                qt_next = loadQ(h, qb + 1)
            elif h + 1 < 8:
                qt_next = loadQ(h + 1, 0)
            if L == 0:
                O = b[4 + qb % 2]

                def S_(j):
                    kt = ktl[j]
                    sb = b[j % 3]
                    kb.mm(sb[:, 0:Wd], K_[:, kt * 128:(kt + 1) * 128], qt[:, 0:Wd], True, True, [K_, qt], [sb])
                S_(0)
                if n > 1:
                    S_(1)
                for j in range(n):
                    if j + 2 < n:
                        S_(j + 2)
                    sb = b[j % 3]
                    P = Ps.next()
                    kb.act(P[:, 0:Wd], sb[:, 0:Wd], AF.Exp, [sb], [P], scale=scale)
                    kb.mm(O[:, 0:Wd], V_[:, ktl[j], :], P[:, 0:Wd], j == 0, j == n - 1, [V_, P], [O])
                r = rl[qb % 2]
                kb.v(lambda e: e.reciprocal(out=r[64:128, 0:Wd], in_=O[64:128, 0:Wd]), [O], [r])
                ot = ots.next()
                kb.tt(ot[0:64, 0:Wd], O[0:64, 0:Wd], r[64:128, 0:Wd], ALU.mult, [O, r], [ot])
                kb.dma("sp", OT[h * 64:(h + 1) * 64, col0:col0 + Wd], ot[:, 0:Wd], [ot], [])
            else:
                O12 = [b[4], b[5]]
                L2 = b[6]
                acc = accs.next()

                def S_(j):
                    kt = ktl[j]
                    for i in range(2):
                        sb = b[(j % 2) * 2 + i]
                        ps = slice(i * 64, (i + 1) * 64)
                        kb.mm(sb.ap, K_[ps, kt * 128:(kt + 1) * 128], qt[ps, :], True, True, [K_, qt], [sb])
                S_(0)
                for j in range(n):
                    if j + 1 < n:
                        S_(j + 1)
                    s0 = (j % 2) * 2
                    P = Ps.next()
                    kb.act(P.ap.rearrange("p a b -> p (a b)"), kb.bank2(s0), AF.Exp, [b[s0], b[s0 + 1]], [P], scale=scale)
                    for i in range(2):
                        kb.mm(O12[i].ap, V_[:, ktl[j], :], P[:, i, :], j == 0, j == n - 1, [V_, P], [O12[i]])
                    kb.mm(L2.ap, ones_b.ap, P[:, 1, :], j == 0, j == n - 1, [ones_b, P], [L2])
                    if j == 0:
                        kb.cp(acc.ap, P[:, 0, :], [P], [acc])
                    else:
                        kb.tt(acc.ap, acc.ap, P[:, 0, :], ALU.add, [acc, P], [acc])
                    if j % 6 == 5 and pending_epi:
                        pending_epi.pop(0)()
                while pending_epi:
                    pending_epi.pop(0)()
                kb.cp(osb[0].ap, O12[0].ap, [O12[0]], [osb[0]], eng="act")
                kb.cp(osb[1].ap, O12[1].ap, [O12[1]], [osb[1]])
                kb.cp(l2s.ap, L2.ap, [L2], [l2s])
                def mk_epi(acc=acc, h=h, col0=col0, Wd=Wd):
                    def e0():
                        kb.mm(b[7].ap, ones_f.ap, acc.ap, True, True, [ones_f, acc], [b[7]])
                        kb.v(lambda e: e.reciprocal(out=rls[0].ap, in_=b[7].ap), [b[7]], [rls[0]])

                    def e1():
                        kb.v(lambda e: e.reciprocal(out=rls[1].ap, in_=l2s.ap), [l2s], [rls[1]])

                    def e2():
                        for i in range(2):
                            kb.tt(tAs[i].ap, osb[i].ap, rls[i].ap, ALU.mult, [osb[i], rls[i]], [tAs[i]], eng="pool")

                    def e3():
                        kb.stt(ox.ap, tAs[1].ap, neglam[:, 0:1], tAs[0].ap, ALU.mult, ALU.add, [tAs[0], tAs[1], neglam], [ox])
                        kb.tt(sqx.ap, ox.ap, ox.ap, ALU.mult, [ox], [sqx], eng="pool")

                    def e4():
                        kb.mm(b[7].ap, ones_b.ap, sqx.ap, True, True, [ones_b, sqx], [b[7]])
                        kb.act(tmpR.ap, b[7].ap, AF.Sqrt, [b[7]], [tmpR], bias=1e-6, scale=1.0 / 128)

                    def e5():
                        kb.v(lambda e: e.reciprocal(out=rstd.ap, in_=tmpR.ap), [tmpR], [rstd])

                    def e6():
                        ot = ots.next()
                        kb.stt(ot.ap, ox.ap, subg[:, 0:1], rstd.ap, ALU.mult, ALU.mult, [ox, subg, rstd], [ot])
                        kb.dma("sp", OT[h * 128:(h + 1) * 128, col0:col0 + Wd], ot.ap, [ot], [])
                    return [e0, e1, e2, e3, e4, e5, e6]
                while pending_epi:
                    pending_epi.pop(0)()
                pending_epi.extend(mk_epi())
                if h == 7 and qb == nqb - 1:
                    while pending_epi:
                        pending_epi.pop(0)()


def layer_norm_tile(kb, y, gam, bet, out, st12, mv, rs1, neghalf):
    kb.v(lambda e: e.bn_stats(out=st12[:, 0:6], in_=y[:, 0:512]), [y], [st12])
    kb.v(lambda e: e.bn_stats(out=st12[:, 6:12], in_=y[:, 512:1024]), [y], [st12])
    kb.v(lambda e: e.bn_aggr(out=mv.ap, in_=st12.ap), [st12], [mv])
    kb.ts(rs1[:, 0:1], mv[:, 1:2], 1e-5, None, ALU.add, None, [mv], [rs1])
    kb.tt(rs1[:, 1:2], rs1[:, 0:1], neghalf[:, 0:1], ALU.pow, [rs1, neghalf], [rs1], eng="pool")
    kb.stt(rs1[:, 0:1], mv[:, 0:1], -1.0, rs1[:, 1:2], ALU.mult, ALU.mult, [mv, rs1], [rs1])
    kb.act(y.ap, y.ap, AF.Identity, [y, rs1], [y], bias=rs1[:, 0:1], scale=rs1[:, 1:2])
    kb.tt(y.ap, y.ap, gam.ap, ALU.mult, [y, gam], [y], eng="pool")
    kb.tt(out.ap, y.ap, bet.ap, ALU.add, [y, bet], [out])


def lockstep(gens):
    gens = list(gens)
    while gens:
        for g in list(gens):
            try:
                next(g)
            except StopIteration:
                gens.remove(g)


def phase_C(kb, E, C, L):
    b = kb.banks
    ntiles = 66 if L == 0 else 64
    nst = 2 if L == 0 else 1
    ident_b, ident_f, neghalf = C["ident_b"], C["ident_f"], C["neghalf"]
    wout = kb.tile([128, 8, 1024], BF16, "wout")
    kb.dma("pool", wout.ap, E[f"wout{L}"], [], [wout])
    router_f = kb.tile([128, 8, 16], F32, "router_f")
    kb.dma("sp", router_f.ap, E[f"router{L}"], [], [router_f])
    router_b = kb.tile([128, 8, 16], BF16, "router_b")
    kb.cp(router_b.ap, router_f.ap, [router_f], [router_b])
    gate = [bc_load(kb, E, L, st, 2, f"gate{st}", True) for st in range(nst)]
    G1 = kb.tile([128, 1024], F32, "G1")
    B1 = kb.tile([128, 1024], F32, "B1")
    G2 = [kb.tile([128, 1024], F32, f"G2_{st}") for st in range(nst)]
    B2 = [kb.tile([128, 1024], F32, f"B2_{st}") for st in range(nst)]
    mark = kb.off
    gam = bc_row(kb, E[f"lnmix{L}"][0, :], "gam")
    bet = bc_row(kb, E[f"lnmix{L}"][1, :], "bet")
    kb.ts(G1.ap, gam.ap, ALPHA, None, ALU.mult, None, [gam], [G1])
    kb.ts(B1.ap, bet.ap, ALPHA, None, ALU.mult, None, [bet], [B1])
    for st in range(nst):
        Af = bc_load(kb, E, L, st, 4, f"Af{st}", True)
        shf = bc_load(kb, E, L, st, 3, f"shf{st}")
        kb.tt(G2[st].ap, gam.ap, Af.ap, ALU.mult, [gam, Af], [G2[st]])
        kb.tt(B2[st].ap, bet.ap, Af.ap, ALU.mult, [bet, Af], [B2[st]])
        kb.tt(B2[st].ap, B2[st].ap, shf.ap, ALU.add, [B2[st], shf], [B2[st]])
    kb.s.barrier()
    kb.off = mark
    affx = [kb.tile([128, 16, 8], F32, f"affx{s}") for s in range(8)]
    for t in affx:
        kb.v(lambda e: e.memset(t.ap, 0.0), [], [t])
    catTs = Rot([kb.tile([128, 8, 128], BF16, f"catT{i}") for i in range(4)])
    xts = Rot([kb.tile([128, 1024], F32, f"xt{i}") for i in range(4)])

    def mk(shape, dt, nm):
        return [Rot([kb.tile(shape, dt, f"{nm}{sl}_{i}") for i in range(2)]) for sl in range(2)]
    tmps, ysC, x1as, t2s = mk([128, 1024], F32, "tmpC"), mk([128, 1024], F32, "yC"), mk([128, 1024], F32, "x1a"), mk([128, 1024], F32, "t2C")
    hfs = mk([128, 1024], BF16, "hf")
    hfTs = mk([128, 8, 128], BF16, "hfT")
    st12s, mvs, rs1s, sms, exs = (mk([128, 12], F32, "st12"), mk([128, 2], F32, "mvC"), mk([128, 2], F32, "rs1C"),
                                  mk([128, 4], F32, "sm"), mk([128, 16], F32, "ex"))
    affc_t = kb.tile([128, 16], F32, "affc_t")
    AT0v = E["AT0"].rearrange("(g c) t -> c g t", c=128)
    OTv = E["OT"].rearrange("(k p) t -> p k t", p=128)

    def loadsC(i):
        cols = slice(i * 128, (i + 1) * 128)
        catT = catTs.next()
        if L == 0:
            kb.dma("sp", catT[:, 0:4, :], AT0v[:, :, cols], [], [catT])
            kb.dma("sp", catT[:, 4:8, :], OTv[:, 0:4, cols], [], [catT])
        else:
            kb.dma("sp", catT.ap, OTv[:, :, cols], [], [catT])
        xt = xts.next()
        kb.dma("sp", xt.ap, rows_of(E, L, i), [], [xt])
        return catT, xt

    def tileA(i, sl, ld, out):
        st = 0 if i < 64 else 1
        catT, xt = ld
        tmp, y, st12, mv, rs1 = tmps[sl].next(), ysC[sl].next(), st12s[sl].next(), mvs[sl].next(), rs1s[sl].next()
        mb = (b[0], b[1]) if sl == 0 else (b[6], b[7])
        for half in range(2):
            hs = slice(half * 512, (half + 1) * 512)
            for k in range(8):
                kb.mm(mb[half].ap, catT[:, k, :], wout[:, k, hs], k == 0, k == 7, [catT, wout], [mb[half]], sig=(k == 7))
            yield
            kb.tt(tmp[:, hs], mb[half].ap, gate[st][:, hs], ALU.mult, [mb[half], gate[st]], [tmp])
            yield
        kb.stt(y.ap, xt.ap, ALPHA, tmp.ap, ALU.mult, ALU.add, [xt, tmp], [y])
        yield
        kb.v(lambda e: e.bn_stats(out=st12[:, 0:6], in_=y[:, 0:512]), [y], [st12])
        yield
        kb.v(lambda e: e.bn_stats(out=st12[:, 6:12], in_=y[:, 512:1024]), [y], [st12])
        yield
        kb.v(lambda e: e.bn_aggr(out=mv.ap, in_=st12.ap), [st12], [mv])
        kb.ts(rs1[:, 0:1], mv[:, 1:2], 1e-5, None, ALU.add, None, [mv], [rs1])
        yield
        kb.tt(rs1[:, 1:2], rs1[:, 0:1], neghalf[:, 0:1], ALU.pow, [rs1, neghalf], [rs1], eng="pool")
        yield
        kb.stt(rs1[:, 0:1], mv[:, 0:1], -1.0, rs1[:, 1:2], ALU.mult, ALU.mult, [mv, rs1], [rs1])
        yield
        kb.act(y.ap, y.ap, AF.Identity, [y, rs1], [y], bias=rs1[:, 0:1], scale=rs1[:, 1:2])
        yield
        x1a, t2, hf = x1as[sl].next(), t2s[sl].next(), hfs[sl].next()
        kb.tt(tmp.ap, y.ap, G1.ap, ALU.mult, [y, G1], [tmp], eng="pool")
        kb.tt(t2.ap, y.ap, G2[st].ap, ALU.mult, [y, G2[st]], [t2])
        yield
        kb.tt(hf.ap, t2.ap, B2[st].ap, ALU.add, [t2, B2[st]], [hf])
        kb.dma("sp", E["HF"][i * 128:(i + 1) * 128, :], hf.ap, [hf], [])
        yield
        kb.tt(x1a.ap, tmp.ap, B1.ap, ALU.add, [tmp, B1], [x1a])
        kb.dma("sp", E["FACC"][i * 128:(i + 1) * 128, :], x1a.ap, [x1a], [])
        out.append(hf)
        yield

    def tileB(i, sl, hf):
        hfT, sm, ex = hfTs[sl].next(), sms[sl].next(), exs[sl].next()
        tb = 2 + sl
        pb = kb.bank_bf(tb)
        for k in range(8):
            kb.tr(pb[:, k * 128:(k + 1) * 128], hf[:, k * 128:(k + 1) * 128], ident_b.ap, [hf, ident_b], [b[tb]], sig=(k == 7))
        yield
        kb.cp(hfT.ap, pb.rearrange("p (a b) -> p a b", a=8), [b[tb]], [hfT], eng="act")
        yield
        lg = b[tb][:, 512 - 16:512]
        for k in range(8):
            kb.mm(lg, hfT[:, k, :], router_b[:, k, :], k == 0, k == 7, [hfT, router_b], [b[tb]], sig=(k == 7))
        yield
        kb.v(lambda e: e.reduce_max(out=sm[:, 0:1], in_=lg, axis=mybir.AxisListType.X), [b[tb]], [sm])
        yield
        kb.ts(sm[:, 1:2], sm[:, 0:1], -1.0, None, ALU.mult, None, [sm], [sm])
        yield
        kb.act(ex.ap, lg, AF.Exp, [b[tb], sm], [ex, sm], bias=sm[:, 1:2], accum=sm[:, 2:3])
        yield
        kb.v(lambda e: e.reciprocal(out=sm[:, 3:4], in_=sm[:, 2:3]), [sm], [sm])
        yield
        if i < 64:
            s_, j = i % 8, i // 8
            ax = affx[s_]
            kb.ts(ax[:, :, s_], ex.ap, sm[:, 3:4], None, ALU.mult, None, [ex, sm], [ax])
            yield
            bk = b[4 + j // 4]
            kb.mm(bk[:, (j % 4) * 128:(j % 4 + 1) * 128], ax.ap.rearrange("p a b -> p (a b)"), ident_f.ap, s_ == 0, s_ == 7,
                  [ax, ident_f], [bk])
        else:
            kb.ts(affc_t.ap, ex.ap, sm[:, 3:4], None, ALU.mult, None, [ex, sm], [affc_t])
            yield
            kb.tr(b[2][0:16, (i - 64) * 128:(i - 63) * 128], affc_t.ap, ident_f.ap, [affc_t, ident_f], [b[2]])
            if i == 65:
                kb.cp(C["affc"].ap, b[2][0:16, 0:256], [b[2]], [C["affc"]])
        yield

    npairs = ntiles // 2
    lds = [loadsC(0), loadsC(1)]
    prev = None
    for p in range(npairs):
        cur_ld = lds
        if p + 1 < npairs:
            lds = [loadsC(2 * p + 2), loadsC(2 * p + 3)]
        outs = [[], []]
        lockstep([tileA(2 * p, 0, cur_ld[0], outs[0]), tileA(2 * p + 1, 1, cur_ld[1], outs[1])])
        if prev is not None:
            lockstep([tileB(2 * (p - 1), 0, prev[0][0]), tileB(2 * (p - 1) + 1, 1, prev[1][0])])
        prev = outs
    lockstep([tileB(2 * (npairs - 1), 0, prev[0][0]), tileB(2 * (npairs - 1) + 1, 1, prev[1][0])])
    kb.cp(C["affT"][:, 0:512], b[4].ap, [b[4]], [C["affT"]])
    kb.cp(C["affT"][:, 512:1024], b[5].ap, [b[5]], [C["affT"]])


def phase_D(kb, E, C, L):
    b = kb.banks
    has_ctx = (L == 0)
    nst = 2 if L == 0 else 1
    ident_b, ident_f, mc, bd = C["ident_b"], C["ident_f"], C["mc"], C["bd"]
    affT = C["affT"]
    gatef = [bc_load(kb, E, L, st, 5, f"gatef{st}", True) for st in range(nst)]
    Wt = [[kb.tile([128, 8, 1024], BF16, f"w{nm}{i}") for nm in ("gate", "up", "down")] for i in range(2)]

    def load_w(e_, bi):
        for wi, nm in enumerate(("gate", "up", "down")):
            kb.dma("pool", Wt[bi][wi].ap, E[f"w{nm}{L}"][e_].rearrange("(k p) n -> p k n", p=128), [], [Wt[bi][wi]])

    load_w(0, 0)
    work = kb.tile([128, 1024], F32, "work")
    kb.cp(work.ap, affT.ap, [affT], [work])
    vals = kb.tile([128, CAP], F32, "vals")
    idxu = kb.tile([128, CAP], U32, "idxu")
    for it in range(CAP // 8):
        sl = slice(it * 8, (it + 1) * 8)
        kb.v(lambda e: e.max(out=vals[:, sl], in_=work.ap), [work], [vals])
        kb.v(lambda e: e.max_index(out=idxu[:, sl], in_max=vals[:, sl], in_values=work.ap), [work, vals], [idxu])
        kb.v(lambda e: e.match_replace(out=work.ap, in_to_replace=vals[:, sl], in_values=work.ap, imm_value=-1.0), [vals, work], [work])
    lo = kb.tile([128, 1], F32, "lo")
    hi = kb.tile([128, 1], F32, "hi")
    mid = kb.tile([128, 1], F32, "mid")
    cnt = kb.tile([128, 1], F32, "cnt")
    d1 = kb.tile([128, 1], F32, "d1")
    ge = kb.tile([128, 1], F32, "ge")
    junk = kb.tile([128, CAP], F32, "junk")
    kb.v(lambda e: e.memset(lo.ap, 0.0), [], [lo])
    kb.v(lambda e: e.memset(hi.ap, 1.0), [], [hi])
    for it in range(32):
        kb.tt(mid.ap, lo.ap, hi.ap, ALU.add, [lo, hi], [mid])
        kb.ts(mid.ap, mid.ap, 0.5, None, ALU.mult, None, [mid], [mid])
        kb.ts(junk.ap, vals.ap, mid[:, 0:1], 0.0, ALU.is_ge, ALU.add, [vals, mid], [junk, cnt], accum=cnt.ap)
        kb.mm(b[0][:, 0:1], bd.ap, cnt.ap, True, True, [bd, cnt], [b[0]])
        kb.ts(ge.ap, b[0][:, 0:1], ECAP - 0.5, None, ALU.is_ge, None, [b[0]], [ge])
        kb.tt(d1.ap, mid.ap, lo.ap, ALU.subtract, [mid, lo], [d1])
        kb.stt(lo.ap, d1.ap, ge[:, 0:1], lo.ap, ALU.mult, ALU.add, [d1, ge, lo], [lo])
        kb.tt(d1.ap, hi.ap, mid.ap, ALU.subtract, [hi, mid], [d1])
        kb.stt(hi.ap, d1.ap, ge[:, 0:1], mid.ap, ALU.mult, ALU.add, [d1, ge, mid], [hi])
    valid = kb.tile([128, CAP], F32, "valid")
    gvv = kb.tile([128, CAP], F32, "gvv")
    kb.ts(valid.ap, vals.ap, lo[:, 0:1], None, ALU.is_ge, None, [vals, lo], [valid])
    kb.tt(gvv.ap, vals.ap, valid.ap, ALU.mult, [vals, valid], [gvv])
    jbi = kb.tile([128, CAP], I32, "jbi")
    kb.v(lambda e: e.tensor_single_scalar(out=jbi.ap, in_=idxu.ap.bitcast(I32), scalar=7, op=ALU.arith_shift_right), [idxu], [jbi])
    idxf = kb.tile([128, CAP], F32, "idxf")
    jbf = kb.tile([128, CAP], F32, "jbf")
    tokf = kb.tile([128, CAP], F32, "tokf")
    kb.cp(idxf.ap, idxu.ap, [idxu], [idxf])
    kb.cp(jbf.ap, jbi.ap, [jbi], [jbf])
    kb.stt(tokf.ap, jbf.ap, 896.0, idxf.ap, ALU.mult, ALU.add, [jbf, idxf], [tokf])
    kb.ts(tokf.ap, tokf.ap, mc[:, 0:1], None, ALU.add, None, [tokf, mc], [tokf])
    padL = kb.tile([128, 128], F32, "padL")
    padR = kb.tile([128, 128], F32, "padR")
    kb.v(lambda e: e.memset(padL.ap, 0.0), [], [padL])
    kb.v(lambda e: e.memset(padR.ap, 0.0), [], [padR])
    mains, tails = [], []
    for ai, arr in enumerate((tokf, gvv, valid)):
        bk = b[1 + ai]
        kb.cp(padL[:, 0:64], arr[:, 128:192], [arr], [padL])
        kb.cp(padR[:, 64:128], arr[:, 128:192], [arr], [padR])
        kb.tr(bk[:, 0:128], arr[:, 0:128], ident_f.ap, [arr, ident_f], [bk])
        kb.tr(bk[:, 128:256], padL.ap, ident_f.ap, [padL, ident_f], [bk])
        kb.tr(bk[:, 256:384], padR.ap, ident_f.ap, [padR, ident_f], [bk])
        m_ = kb.tile([128, 128], F32, f"main{ai}")
        t_ = kb.tile([128, 16, 4], F32, f"tail{ai}")
        kb.cp(m_.ap, bk[:, 0:128], [bk], [m_])
        kb.cp(t_.ap, bk[:, 128:256].rearrange("p (e k two) -> p e k two", e=16, k=4)[:, :, :, 0], [bk], [t_])
        kb.tt(t_.ap, t_.ap, bk[:, 256:384].rearrange("p (e k two) -> p e k two", e=16, k=4)[:, :, :, 1], ALU.add, [bk, t_], [t_])
        mains.append(m_)
        tails.append(t_)
    IDXM = kb.tile([128, 128], I32, "IDXM")
    IDXT = kb.tile([128, 64], I32, "IDXT")
    for src, dst, n_ in ((mains, IDXM, 128), (tails, IDXT, 64)):
        tk = src[0].ap if n_ == 128 else src[0].ap.rearrange("p a b -> p (a b)")
        vl = src[2].ap if n_ == 128 else src[2].ap.rearrange("p a b -> p (a b)")
        tmpi = kb.tile([128, n_], F32, "tmpi")
        kb.ts(tmpi.ap, tk, mc[:, 1:2], None, ALU.subtract, None, [src[0], mc], [tmpi])
        kb.tt(tmpi.ap, tmpi.ap, vl, ALU.mult, [tmpi, src[2]], [tmpi])
        kb.ts(tmpi.ap, tmpi.ap, mc[:, 1:2], None, ALU.add, None, [tmpi, mc], [tmpi])
        kb.cp(dst.ap, tmpi.ap, [tmpi], [dst])
    GM = mains[1]
    GT = tails[1]
    if has_ctx:
        cwork = kb.tile([16, 256], F32, "cwork")
        kb.cp(cwork.ap, C["affc"].ap, [C["affc"]], [cwork])
        cvals = kb.tile([16, CCAP], F32, "cvals")
        cidx = kb.tile([16, CCAP], U32, "cidx")
        for it in range(CCAP // 8):
            sl = slice(it * 8, (it + 1) * 8)
            kb.v(lambda e: e.max(out=cvals[:, sl], in_=cwork.ap), [cwork], [cvals])
            kb.v(lambda e: e.max_index(out=cidx[:, sl], in_max=cvals[:, sl], in_values=cwork.ap), [cwork, cvals], [cidx])
            kb.v(lambda e: e.match_replace(out=cwork.ap, in_to_replace=cvals[:, sl], in_values=cwork.ap, imm_value=-1.0), [cvals, cwork], [cwork])
        cidf = kb.tile([16, CCAP], F32, "cidf")
        kb.cp(cidf.ap, cidx.ap, [cidx], [cidf])
        kb.ts(cidf.ap, cidf.ap, float(NLAT), None, ALU.add, None, [cidf], [cidf])
        kb.tr(b[4][0:32, 0:16], cidf.ap, ident_f[0:16, 0:16], [cidf, ident_f], [b[4]])
        kb.tr(b[4][0:32, 16:32], cvals.ap, ident_f[0:16, 0:16], [cvals, ident_f], [b[4]])
        CIDX = kb.tile([32, 16], I32, "CIDX")
        CG = kb.tile([32, 16], F32, "CG")
        kb.cp(CIDX.ap, b[4][0:32, 0:16], [b[4]], [CIDX])
        kb.cp(CG.ap, b[4][0:32, 16:32], [b[4]], [CG])
    xss = Rot([kb.tile([128, 1024], BF16, f"xs{i}") for i in range(8)])
    xsTs = Rot([kb.tile([128, 8, 512], BF16, f"xsT{i}") for i in range(2)])
    hidTs = Rot([kb.tile([128, 8, 512], BF16, f"hidT{i}") for i in range(2)])
    sgs = Rot([kb.tile([128, 512], F32, f"sg{i}") for i in range(2)])
    ysbs = Rot([kb.tile([128, 1024], F32, f"ysb{i}") for i in range(3)])

    work_items = []
    for e_ in range(16):
        chunks = [(IDXM[:, e_ * 8 + s_:e_ * 8 + s_ + 1], GM[:, e_ * 8 + s_:e_ * 8 + s_ + 1], 128, 0, IDXM, GM) for s_ in range(8)]
        chunks += [(IDXT[:, e_ * 4 + k:e_ * 4 + k + 1], GT[:, e_, k:k + 1], 128, 0, IDXT, GT) for k in range(4)]
        blocks = [chunks[0:4], chunks[4:8], chunks[8:12]]
        if has_ctx:
            blocks.append([(CIDX[:, e_:e_ + 1], CG[:, e_:e_ + 1], 32, 1, CIDX, CG)])
        for bi_, blk in enumerate(blocks):
            work_items.append((e_, bi_, blk))

    def gathers(blk):
        res = []
        R = blk[0][2]
        for (icol, gcol, R_, st, it_, gt_) in blk:
            xs = xss.next()
            kb.s.dma("pool", lambda q: q.indirect_dma_start(out=xs[0:R, :], out_offset=None, in_=E["HF"],
                                                             in_offset=bass.IndirectOffsetOnAxis(ap=icol, axis=0)),
                     _toks([it_]), _toks([xs]))
            res.append(xs)
        return res

    prev_sc = []
    cur_sc = []
    g_next = gathers(work_items[0][2])
    for wi_, (e_, bi_, blk) in enumerate(work_items):
        bi = e_ % 2
        if bi_ == 0:
            if e_ + 1 < 16:
                load_w(e_ + 1, (e_ + 1) % 2)
            prev_sc = cur_sc
            cur_sc = []
        wg, wu, wd = Wt[bi]
        R = blk[0][2]
        Wd = R * len(blk)
        xs_list = g_next
        xsT = xsTs.next()
        hidT = hidTs.next()
        for cl, xs in enumerate(xs_list):
            pb = kb.bank_bf(0)
            for k in range(8):
                kb.tr(pb[:, k * 128:k * 128 + R], xs[0:R, k * 128:(k + 1) * 128], ident_b[0:R, 0:R], [xs, ident_b], [b[0]], sig=(k == 7))
            kb.cp(xsT[:, :, cl * R:(cl + 1) * R], pb.rearrange("p (a b) -> p a b", a=8)[:, :, 0:R], [b[0]], [xsT],
                  eng=("act" if cl % 2 else "dve"))
        if wi_ + 1 < len(work_items):
            g_next = gathers(work_items[wi_ + 1][2])
        for ffc in range(8):
            fs = slice(ffc * 128, (ffc + 1) * 128)
            G_ = b[1 + ffc % 2]
            U_ = b[3 + ffc % 2]
            for k in range(8):
                kb.mm(G_[:, 0:Wd], wg[:, k, fs], xsT[:, k, 0:Wd], k == 0, k == 7, [wg, xsT], [G_], sig=(k == 7))
            for k in range(8):
                kb.mm(U_[:, 0:Wd], wu[:, k, fs], xsT[:, k, 0:Wd], k == 0, k == 7, [wu, xsT], [U_], sig=(k == 7))
            sg = sgs.next()
            kb.act(sg[:, 0:Wd], G_[:, 0:Wd], AF.Silu, [G_], [sg])
            kb.tt(hidT[:, ffc, 0:Wd], sg[:, 0:Wd], U_[:, 0:Wd], ALU.mult, [sg, U_], [hidT])
        for cl, (icol, gcol, R_, st, it_, gt_) in enumerate(blk):
            ysb = ysbs.next()
            for half in range(2):
                hs = slice(half * 512, (half + 1) * 512)
                Y_ = b[5 + half]
                for ffc in range(8):
                    kb.mm(Y_[0:R, :], hidT[:, ffc, cl * R:(cl + 1) * R], wd[:, ffc, hs], ffc == 0, ffc == 7, [hidT, wd], [Y_], sig=(ffc == 7))
                kb.stt(ysb[0:R, hs], Y_[0:R, :], gcol, gatef[st][0:R, hs], ALU.mult, ALU.mult, [Y_, gt_, gatef[st]], [ysb])
            tk = Tok("sc")
            kb.s.dma("pool", lambda q: q.indirect_dma_start(out=E["FACC"], out_offset=bass.IndirectOffsetOnAxis(ap=icol, axis=0),
                                                             in_=ysb[0:R, :], in_offset=None, compute_op=ALU.add),
                     _toks([ysb, it_]) + prev_sc, [tk])
            cur_sc.append(tk)


def phase_E(kb, E, C):
    neghalf = C["neghalf"]
    gam = bc_row(kb, E["lnffn1"][0, :], "gamE")
    bet = bc_row(kb, E["lnffn1"][1, :], "betE")
    ys = Rot([kb.tile([128, 1024], F32, f"yE{i}") for i in range(4)])
    outs = Rot([kb.tile([128, 1024], F32, f"oE{i}") for i in range(4)])
    st12 = kb.tile([128, 12], F32, "st12E")
    mv = kb.tile([128, 2], F32, "mvE")
    rs1 = kb.tile([128, 2], F32, "rs1E")
    def loadE(i):
        y = ys.next()
        kb.dma("sp", y.ap, E["FACC"][i * 128:(i + 1) * 128, :], [], [y])
        return y

    st12s = [kb.tile([128, 12], F32, f"st12E{i}") for i in range(2)]
    mvs = [kb.tile([128, 2], F32, f"mvE{i}") for i in range(2)]
    rs1s = [kb.tile([128, 2], F32, f"rs1E{i}") for i in range(2)]

    def tileE(i, sl, y):
        o = outs.next()
        st12_, mv_, rs1_ = st12s[sl], mvs[sl], rs1s[sl]
        kb.v(lambda e: e.bn_stats(out=st12_[:, 0:6], in_=y[:, 0:512]), [y], [st12_])
        yield
        kb.v(lambda e: e.bn_stats(out=st12_[:, 6:12], in_=y[:, 512:1024]), [y], [st12_])
        yield
        kb.v(lambda e: e.bn_aggr(out=mv_.ap, in_=st12_.ap), [st12_], [mv_])
        kb.ts(rs1_[:, 0:1], mv_[:, 1:2], 1e-5, None, ALU.add, None, [mv_], [rs1_])
        yield
        kb.tt(rs1_[:, 1:2], rs1_[:, 0:1], neghalf[:, 0:1], ALU.pow, [rs1_, neghalf], [rs1_], eng="pool")
        yield
        kb.stt(rs1_[:, 0:1], mv_[:, 0:1], -1.0, rs1_[:, 1:2], ALU.mult, ALU.mult, [mv_, rs1_], [rs1_])
        yield
        kb.act(y.ap, y.ap, AF.Identity, [y, rs1_], [y], bias=rs1_[:, 0:1], scale=rs1_[:, 1:2])
        yield
        kb.tt(y.ap, y.ap, gam.ap, ALU.mult, [y, gam], [y], eng="pool")
        yield
        kb.tt(o.ap, y.ap, bet.ap, ALU.add, [y, bet], [o])
        kb.dma("sp", E["out"][i * 128:(i + 1) * 128, :], o.ap, [o], [])
        yield

    lds = [loadE(0), loadE(1)]
    for p in range(32):
        cur = lds
        if p + 1 < 32:
            lds = [loadE(2 * p + 2), loadE(2 * p + 3)]
        lockstep([tileE(2 * p, 0, cur[0]), tileE(2 * p + 1, 1, cur[1])])


def phase_A1(kb, E, C):
    b = kb.banks
    modT = C["modT"][1]
    ident_b, ones_b, ones_f, neghalf = C["ident_b"], C["ones_b"], C["ones_f"], C["neghalf"]
    l4 = kb.tile([1, 256], F32, "l4")
    kb.dma("sp", l4.ap, E["lam4"], [], [l4])
    lp = kb.tile([1, 128], F32, "lp")
    lsum = kb.tile([1, 4], F32, "lsum")
    kb.tt(lp[:, 0:64], l4[:, 0:64], l4[:, 64:128], ALU.mult, [l4], [lp])
    kb.tt(lp[:, 64:128], l4[:, 128:192], l4[:, 192:256], ALU.mult, [l4], [lp])
    kb.v(lambda e: e.reduce_sum(out=lsum[:, 0:1], in_=lp[:, 0:64], axis=mybir.AxisListType.X), [lp], [lsum])
    kb.v(lambda e: e.reduce_sum(out=lsum[:, 1:2], in_=lp[:, 64:128], axis=mybir.AxisListType.X), [lp], [lsum])
    kb.act(lsum[:, 2:4], lsum[:, 0:2], AF.Exp, [lsum], [lsum])
    kb.tt(lsum[:, 0:1], lsum[:, 3:4], lsum[:, 2:3], ALU.subtract, [lsum], [lsum])
    kb.ts(lsum[:, 1:2], lsum[:, 0:1], -LAM_INIT, None, ALU.add, None, [lsum], [lsum])
    kb.mm(b[0][:, 0:1], ones_f[0:1, :], lsum[:, 1:2], True, True, [ones_f, lsum], [b[0]])
    kb.cp(C["neglam"].ap, b[0][:, 0:1], [b[0]], [C["neglam"]])
    shc = kb.tile([128, 8, 2], BF16, "shc1")
    kb.cp(shc.ap, modT[:, 0:8, :], [modT], [shc])
    A1 = kb.tile([128, 8, 2], F32, "A1_1")
    kb.ts(A1.ap, modT[:, 8:16, :], 1.0, None, ALU.add, None, [modT], [A1])
    W = [kb.tile([128, 8, 3072], BF16, f"W1_{st}") for st in range(2)]
    bcol = kb.tile([128, 16, 2], F32, "bcol1")
    brow = [kb.tile([1, 1024], BF16, f"brow1_{st}") for st in range(2)]
    mark = kb.off
    wtmp = kb.tile([128, 8, 1024], F32, "wtmp1")
    Wun = kb.tile([128, 8, 1024], BF16, "Wun1")
    for third in range(3):
        cs_ = slice(third * 1024, (third + 1) * 1024)
        kb.dma("sp", wtmp.ap, E["win1"][:, :, cs_], [], [wtmp])
        for k in range(8):
            kb.cp(Wun[:, k, :], wtmp[:, k, :], [wtmp], [Wun], eng=("act" if k % 2 else "dve"))
        for st in range(2):
            for k in range(8):
                if k % 2:
                    kb.act(W[st][:, k, cs_], wtmp[:, k, :], AF.Identity, [wtmp, A1], [W[st]], scale=A1[:, k, st:st + 1])
                else:
                    kb.ts(W[st][:, k, cs_], wtmp[:, k, :], A1[:, k, st:st + 1], None, ALU.mult, None, [wtmp, A1], [W[st]])
        if third < 2:
            for m in range(8):
                mi = third * 8 + m
                for k in range(8):
                    kb.mm(b[0][:, mi * 2:mi * 2 + 2], Wun[:, k, m * 128:(m + 1) * 128], shc[:, k, :], k == 0, k == 7, [Wun, shc], [b[0]], sig=(k == 7))
        else:
            for st in range(2):
                for half in range(2):
                    for k in range(8):
                        kb.mm(b[1 + half][0:1, :], shc[:, k, st:st + 1], Wun[:, k, half * 512:(half + 1) * 512], k == 0, k == 7,
                              [Wun, shc], [b[1 + half]], sig=(k == 7))
                    kb.cp(brow[st][:, half * 512:(half + 1) * 512], b[1 + half][0:1, :], [b[1 + half]], [brow[st]])
    kb.cp(bcol.ap, b[0][:, 0:32].rearrange("p (a b) -> p a b", a=16), [b[0]], [bcol])
    kb.s.barrier()
    kb.off = mark
    gam = bc_row(kb, E["lnffn0"][0, :], "gamA1")
    bet = bc_row(kb, E["lnffn0"][1, :], "betA1")
    ys = Rot([kb.tile([128, 1024], F32, f"yA{i}") for i in range(3)])
    x2s = Rot([kb.tile([128, 1024], F32, f"x2_{i}") for i in range(2)])
    xbs = Rot([kb.tile([128, 1024], BF16, f"xbA1_{i}") for i in range(2)])
    st12 = kb.tile([128, 12], F32, "st12A")
    mv = kb.tile([128, 2], F32, "mvA")
    rs1 = kb.tile([128, 2], F32, "rs1A")
    xTs = Rot([kb.tile([128, 8, 512], BF16, f"xTA{i}") for i in range(2)])
    cosTs = Rot([kb.tile([128, 512], F32, f"cosT{i}") for i in range(2)])
    sinTs = Rot([kb.tile([128, 512], F32, f"sinT{i}") for i in range(2)])
    qbs = Rot([kb.tile([128, 512], BF16, f"qb{i}") for i in range(3)])
    tAs = Rot([kb.tile([128, 512], F32, f"tA1_{i}") for i in range(2)])
    tBs = Rot([kb.tile([128, 512], F32, f"tB1_{i}") for i in range(2)])
    outs = Rot([kb.tile([128, 512], BF16, f"qk{i}") for i in range(3)])
    vaugs = Rot([kb.tile([128, 8, 128], BF16, f"vaug1_{i}") for i in range(2)])
    psw_f = kb.tile([128, 128], F32, "psw_f")
    kb.dma("sp", psw_f.ap, E["psw"], [], [psw_f])
    psw = kb.tile([128, 128], BF16, "psw")
    kb.cp(psw.ap, psw_f.ap, [psw_f], [psw])
    V1v = E["V1"].rearrange("h p kt d -> p h kt d")

    def loadA(i):
        y = ys.next()
        kb.dma("sp", y.ap, E["FACC"][i * 128:(i + 1) * 128, :], [], [y])
        return y

    def stageL(gi):
        ntl = 4 if gi < 16 else 2
        xT = xTs.next()

        def one(tl):
            i = gi * 4 + tl
            if i == 0:
                ybox[0] = loadA(0)
            y = ybox[0]
            if i + 1 < 66:
                ybox[0] = loadA(i + 1)
            x2 = x2s.next()
            layer_norm_tile(kb, y, gam, bet, x2, st12, mv, rs1, neghalf)
            if i < 64:
                kb.dma("sp", E["X2"][i * 128:(i + 1) * 128, :], x2.ap, [x2], [])
            xb = xbs.next()
            kb.cp(xb.ap, x2.ap, [x2], [xb], eng="act")
            pb = kb.bank_bf(0)
            for k in range(8):
                kb.tr(pb[:, k * 128:(k + 1) * 128], xb[:, k * 128:(k + 1) * 128], ident_b.ap, [xb, ident_b], [b[0]], sig=(k == 7))
            kb.cp(xT[:, :, tl * 128:(tl + 1) * 128], pb.rearrange("p (a b) -> p a b", a=8), [b[0]], [xT], eng=("act" if tl % 2 else "dve"))
        return xT, [(lambda tl=tl: one(tl)) for tl in range(ntl)]

    def stageP(gi, xT, Lsteps):
        ntl = 4 if gi < 16 else 2
        Wd = ntl * 128
        st = 0 if gi < 16 else 1
        col0 = gi * 512
        cosT, sinT = cosTs.next(), sinTs.next()
        kb.dma("sp", cosT[:, 0:Wd], E["cos1"][:, col0:col0 + Wd], [], [cosT])
        kb.dma("sp", sinT[:, 0:Wd], E["sin1"][:, col0:col0 + Wd], [], [sinT])

        def finish(mi, qb_):
            br = b[5 + mi % 2]
            kb.mm(br[:, 0:Wd], psw.ap, qb_[:, 0:Wd], True, True, [psw, qb_], [br])
            tA_ = tAs.next()
            kb.tt(tA_[:, 0:Wd], qb_[:, 0:Wd], cosT[:, 0:Wd], ALU.mult, [qb_, cosT], [tA_], eng="pool")
            tB_ = tBs.next()
            kb.tt(tB_[:, 0:Wd], br[:, 0:Wd], sinT[:, 0:Wd], ALU.mult, [br, sinT], [tB_])
            o = outs.next()
            kb.tt(o[:, 0:Wd], tA_[:, 0:Wd], tB_[:, 0:Wd], ALU.add, [tA_, tB_], [o])
            dst_d = E["QT"] if mi < 8 else E["KT"]
            kb.dma("sp", dst_d[mi % 8][:, col0:col0 + Wd], o[:, 0:Wd], [o], [])

        pend = None
        pbanks = (b[1], b[2], b[7])
        for mi in range(16):
            bk = pbanks[mi % 3]
            for k in range(8):
                kb.mm(bk[:, 0:Wd], W[st][:, k, mi * 128:(mi + 1) * 128], xT[:, k, 0:Wd], k == 0, k == 7, [W[st], xT], [bk], sig=(k == 7))
            qb_ = qbs.next()
            kb.act(qb_[:, 0:Wd], bk[:, 0:Wd], AF.Identity, [bk, bcol], [qb_], bias=bcol[:, mi, st:st + 1])
            if pend is not None:
                finish(*pend)
            pend = (mi, qb_)
            if mi % 4 == 3 and Lsteps:
                Lsteps.pop(0)()
        finish(*pend)
        for tl in range(ntl):
            i = gi * 4 + tl
            ts_ = slice(tl * 128, (tl + 1) * 128)
            va = vaugs.next()
            for half in range(2):
                bk = b[3 + half]
                for k in range(8):
                    kb.mm(bk.ap, xT[:, k, ts_], W[st][:, k, 2048 + half * 512:2048 + (half + 1) * 512], k == 0, False, [xT, W[st]], [bk], sig=False)
                kb.mm(bk.ap, ones_b[0:1, 0:128], brow[st][:, half * 512:(half + 1) * 512], False, True, [ones_b, brow[st]], [bk])
                kb.cp(va[:, half * 4:(half + 1) * 4, :], bk.ap.rearrange("p (a b) -> p a b", a=4), [bk], [va], eng=("act" if half else "dve"))
            kb.dma("sp", V1v[:, :, i, :], va.ap, [va], [])
        while Lsteps:
            Lsteps.pop(0)()

    ybox = [None]
    xT_cur, steps = stageL(0)
    for f_ in steps:
        f_()
    for gi in range(17):
        if gi + 1 < 17:
            xT_next, steps = stageL(gi + 1)
        else:
            xT_next, steps = None, []
        stageP(gi, xT_cur, steps)
        xT_cur = xT_next


PHASES = ["mod", "A0", "B0", "C0", "D0", "A1", "B1", "C1", "D1", "E"]


def build(dbg=False, stop_after=None, only=None):
    kb = KB(dbg, stop_after)
    E = {}
    inp = kb.inp
    E["x"] = inp("x", [NLAT, D])
    E["ctx"] = inp("ctx", [NCTX, D])
    E["ccT"] = inp("ccT", [128, 8, 2])
    E["ident"] = inp("ident", [128, 128])
    E["mconst"] = inp("mconst", [128, 4])
    E["bdiag"] = inp("bdiag", [128, 128])
    E["cs0"] = inp("cs0", [64, NALL])
    E["cos1"] = inp("cos1", [128, NALL])
    E["sin1"] = inp("sin1", [128, NALL])
    for l in range(2):
        E[f"wmod{l}"] = inp(f"wmod{l}", [12, 128, 8, 512])
        E[f"bmod{l}"] = inp(f"bmod{l}", [1, 6144])
        E[f"bmodT{l}"] = inp(f"bmodT{l}", [128, 48])
        E[f"wout{l}"] = inp(f"wout{l}", [128, 8, 1024])
        E[f"router{l}"] = inp(f"router{l}", [128, 8, 16])
        E[f"lnmix{l}"] = inp(f"lnmix{l}", [2, 1024])
        E[f"lnffn{l}"] = inp(f"lnffn{l}", [2, 1024])
        for nm in ("gate", "up", "down"):
            E[f"w{nm}{l}"] = inp(f"w{nm}{l}", [16, 1024, 1024])
    E["win0"] = inp("win0", [128, 8, 1472])
    E["wuqx"] = inp("wuqx", [128, 2, 1024])
    E["wukx"] = inp("wukx", [128, 1024])
    E["wuv"] = inp("wuv", [128, 512])
    E["qnorm"] = inp("qnorm", [128, 2])
    E["kvnorm"] = inp("kvnorm", [128, 1])
    E["gln"] = inp("gln", [128, 8])
    E["wsT"] = inp("wsT", [128, 512])
    E["gbs"] = inp("gbs", [1, 512])
    E["win1"] = inp("win1", [128, 8, 3072])
    E["lam4"] = inp("lam4", [1, 256])
    E["subln"] = inp("subln", [128, 1])
    E["psw"] = inp("psw", [128, 128])
    sc = kb.scratch
    E["MOD"] = sc("MOD", [2, 2, 6144], F32)
    E["AT0"] = sc("AT0", [512, NALL], BF16)
    E["QT"] = sc("QT", [8, 128, NALL], BF16)
    E["KT"] = sc("KT", [8, 128, NALL], BF16)
    E["V0"] = sc("V0", [8, 128, 66, 128], BF16)
    E["V1"] = sc("V1", [8, 128, 66, 128], BF16)
    E["OT"] = sc("OT", [1024, NALL], BF16)
    E["FACC"] = sc("FACC", [NPAD, D], F32)
    E["HF"] = sc("HF", [NPAD, D], BF16)
    E["X2"] = sc("X2", [NLAT, D], F32)
    E["out"] = kb.nc.dram_tensor("out", [NLAT, D], F32, kind="ExternalOutput").ap()
    C = setup_consts(kb, E)
    fns = {
        "mod": lambda: phase_mod(kb, E, C),
        "A0": lambda: phase_A0(kb, E, C),
        "B0": lambda: phase_attn(kb, E, C, 0),
        "C0": lambda: phase_C(kb, E, C, 0),
        "D0": lambda: phase_D(kb, E, C, 0),
        "A1": lambda: phase_A1(kb, E, C),
        "B1": lambda: phase_attn(kb, E, C, 1),
        "C1": lambda: phase_C(kb, E, C, 1),
        "D1": lambda: phase_D(kb, E, C, 1),
        "E": lambda: phase_E(kb, E, C),
    }
    for ph in PHASES:
        if only is None or ph in only:
            fns[ph]()
            kb.phase()
        if ph == stop_after:
            break
    kb.s.barrier()
    return kb


def _rope_tables():
    t = np.arange(NLAT)
    row = (t // 64).astype(np.float32)
    col = (t % 64).astype(np.float32)

    def ang(dim):
        nf = dim // 4
        inv = (np.float32(10000.0) ** (-np.arange(nf, dtype=np.float32) / np.float32(nf))).astype(np.float32)
        return np.concatenate([row[:, None] * inv, col[:, None] * inv], -1).astype(np.float32)

    a0 = ang(32)
    c0, s0 = np.cos(a0).T, np.sin(a0).T
    cs0 = np.zeros((64, NALL), np.float32)
    cs0[0:32, NLAT:] = 1.0
    cs0[0:16, :NLAT] = c0
    cs0[16:32, :NLAT] = c0
    cs0[32:48, :NLAT] = -s0
    cs0[48:64, :NLAT] = s0
    a1 = ang(64)
    c1, s1 = np.cos(a1).T, np.sin(a1).T
    cos1 = np.ones((128, NALL), np.float32)
    sin1 = np.zeros((128, NALL), np.float32)
    for blk in range(4):
        cos1[blk * 32:(blk + 1) * 32, :NLAT] = c1
        sin1[blk * 32:(blk + 1) * 32, :NLAT] = (-s1 if blk % 2 == 0 else s1)
    return cs0, cos1, sin1


def _pk(w):
    K = w.shape[0] // 128
    return np.ascontiguousarray(w.reshape(K, 128, -1).transpose(1, 0, 2))


def prep_shared(I):
    f = lambda a: np.ascontiguousarray(np.asarray(a, dtype=np.float32))
    S = {}
    S["ident"] = np.eye(128, dtype=np.float32)
    p = np.arange(128)
    mc = np.zeros((128, 4), np.float32)
    mc[:, 0] = 128 * (p % 8)
    mc[:, 1] = NALL + p
    S["mconst"] = mc
    S["bdiag"] = (p[:, None] // 8 == p[None, :] // 8).astype(np.float32)
    S["cs0"], S["cos1"], S["sin1"] = _rope_tables()
    for l in range(2):
        wm = f(I[f"w_mod_{l}"])
        S[f"wmod{l}"] = np.ascontiguousarray(wm.reshape(8, 128, 12, 512).transpose(2, 1, 0, 3))
        bm = f(I[f"b_mod_{l}"])
        S[f"bmod{l}"] = bm.reshape(1, 6144)
        S[f"bmodT{l}"] = np.ascontiguousarray(bm.reshape(48, 128).T)
        S[f"wout{l}"] = _pk(f(I[f"w_out_{l}"]))
        S[f"router{l}"] = _pk(f(I[f"router_{l}"]))
        S[f"lnmix{l}"] = np.stack([f(I[f"ln_mix_g_{l}"]), f(I[f"ln_mix_b_{l}"])])
        S[f"lnffn{l}"] = np.stack([f(I[f"ln_ffn_g_{l}"]), f(I[f"ln_ffn_b_{l}"])])
        for nm in ("gate", "up", "down"):
            S[f"w{nm}{l}"] = f(I[f"w_{nm}_{l}"])
    w = f(I["w_in_0"])
    kr = w[:, 1408:1440]
    krE, krO = kr[:, 0::2], kr[:, 1::2]
    S["win0"] = _pk(np.concatenate([w[:, 0:512], w[:, 1024:1280], w[:, 1280:1408], krE, krO, krO, krE, w[:, 512:1024]], 1))
    wuq = f(I["mla_w_uq_0"])
    blocks = []
    for h in range(8):
        nope = wuq[:, h * 96:h * 96 + 64]
        rp = wuq[:, h * 96 + 64:h * 96 + 96]
        rE, rO = rp[:, 0::2], rp[:, 1::2]
        blocks.append(np.concatenate([rE, rO, rO, rE, nope], 1))
    S["wuqx"] = _pk(np.concatenate(blocks, 1))
    wukv = f(I["mla_w_ukv_0"])
    S["wukx"] = np.ascontiguousarray(np.concatenate(
        [np.concatenate([np.zeros((128, 64), np.float32), wukv[:, h * 128:h * 128 + 64]], 1) for h in range(8)], 1))
    S["wuv"] = np.ascontiguousarray(np.concatenate([wukv[:, h * 128 + 64:h * 128 + 128] for h in range(8)], 1))
    S["qnorm"] = np.ascontiguousarray(f(I["mla_q_norm_0"]).reshape(2, 128).T)
    S["kvnorm"] = f(I["mla_kv_norm_0"]).reshape(128, 1)
    S["gln"] = np.ascontiguousarray(np.concatenate([f(I["gmlp_ln_g_0"]).reshape(4, 128).T, f(I["gmlp_ln_b_0"]).reshape(4, 128).T], 1))
    S["wsT"] = np.ascontiguousarray(f(I["gmlp_ws_0"]).transpose(2, 0, 1).reshape(128, 512))
    S["gbs"] = f(I["gmlp_bs_0"]).reshape(1, 512)
    w1 = f(I["w_in_1"])
    cols = []
    for part in range(2):
        for j in range(16):
            blk = w1[:, part * 1024 + j * 64:part * 1024 + (j + 1) * 64]
            cols += [blk[:, 0::2], blk[:, 1::2]]
    cols.append(w1[:, 2048:3072])
    S["win1"] = _pk(np.concatenate(cols, 1))
    S["lam4"] = np.concatenate([f(I["lambda_q1_1"]), f(I["lambda_k1_1"]), f(I["lambda_q2_1"]), f(I["lambda_k2_1"])]).reshape(1, 256)
    S["subln"] = f(I["subln_g_1"]).reshape(128, 1)
    S["psw"] = np.eye(128, dtype=np.float32)[:, np.arange(128) ^ 32]
    return S


def prep_core(I, S, bidx):
    m = dict(S)
    m["x"] = np.ascontiguousarray(np.asarray(I["x"][bidx], dtype=np.float32))
    m["ctx"] = np.ascontiguousarray(np.asarray(I["ctx"][bidx], dtype=np.float32))
    cc = np.stack([np.asarray(I["c"][bidx], np.float32), np.asarray(I["c_ctx"], np.float32)], -1)
    m["ccT"] = np.ascontiguousarray(cc.reshape(8, 128, 2).transpose(1, 0, 2))
    return m


_KB_CACHE = {}


def kernel(**inputs):
    if "kb" not in _KB_CACHE:
        _KB_CACHE["kb"] = build()
    kb = _KB_CACHE["kb"]
    S = prep_shared(inputs)
    in_maps = [prep_core(inputs, S, bidx) for bidx in range(8)]
    res = run_bass_kernel_spmd(kb.nc, in_maps, core_ids=list(range(8)))
    return np.stack([np.asarray(r["out"], dtype=np.float32) for r in res.results], 0)
```

```python
import math
import numpy as np
import concourse.bass as bass
import concourse.mybir as mybir
from concourse.bass_utils import run_bass_kernel_spmd

F32 = mybir.dt.float32
BF16 = mybir.dt.bfloat16
I32 = mybir.dt.int32
U32 = mybir.dt.uint32
ALU = mybir.AluOpType
AF = mybir.ActivationFunctionType

D = 1024
NLAT = 8192
NCTX = 256
NALL = NLAT + NCTX
NPAD = NALL + 128
ALPHA = 4 ** 0.25
LAM_INIT = 0.8 - 0.6 * math.exp(-0.3)
CAP = 192
ECAP = 1024
CCAP = 32
AW = 51500


class Tok:
    __slots__ = ("w", "r", "name")

    def __init__(self, name=""):
        self.w = None
        self.r = {}
        self.name = name


class Tile:
    def __init__(self, ap, name=""):
        self.ap = ap
        self.tok = Tok(name)

    def __getitem__(self, k):
        return self.ap[k]


def _toks(xs):
    return [x.tok if isinstance(x, Tile) else x for x in xs]


class Sched:
    EPOCH = 24000
    NDMA = 48

    def __init__(self, nc):
        self.nc = nc
        self.eng = {"pe": nc.tensor, "act": nc.scalar, "dve": nc.vector, "pool": nc.gpsimd, "sp": nc.sync}
        self.cnt = {e: 0 for e in self.eng}
        self.sems = {e: [] for e in self.eng}
        self.known = {e: {} for e in self.eng}
        self.pending = {e: [] for e in self.eng}
        self.dma_sems = [nc.alloc_semaphore(f"dq{i}") for i in range(self.NDMA)]
        self.dma_val = [0] * self.NDMA
        self.dma_next = 0

    def _sem(self, e, seq):
        k = (seq - 1) // self.EPOCH
        while len(self.sems[e]) <= k:
            self.sems[e].append(self.nc.alloc_semaphore(f"c_{e}_{len(self.sems[e])}"))
        return self.sems[e][k], (seq - 1) % self.EPOCH + 1

    def _wait(self, e, ev):
        if ev is None:
            return
        kind, src, val = ev
        if kind == "eng":
            if src == e and e == "pe":
                return
            key = ("eng", src)
            if self.known[e].get(key, 0) >= val:
                return
            if src == e and val <= self.cnt[e] - 2:
                return
            sem, v = self._sem(src, val)
            self.eng[e].wait_ge(sem, v)
            self.known[e][key] = val
        else:
            key = ("dma", src)
            if self.known[e].get(key, 0) >= val:
                return
            self.eng[e].wait_ge(self.dma_sems[src], val)
            self.known[e][key] = val

    def _deps(self, e, reads, writes):
        for t in reads:
            self._wait(e, t.w)
        for t in writes:
            self._wait(e, t.w)
            for ev in list(t.r.values()):
                self._wait(e, ev)

    def op(self, e, fn, reads=(), writes=(), sig=True):
        reads = _toks(reads)
        writes = _toks(writes)
        self._deps(e, reads, writes)
        ins = fn(self.eng[e])
        if not sig:
            self.pending[e].append((reads, writes))
            return ins
        self.cnt[e] += 1
        seq = self.cnt[e]
        sem, v = self._sem(e, seq)
        ins.then_inc(sem, 1)
        me = ("eng", e, seq)
        groups = self.pending[e] + [(reads, writes)]
        self.pending[e] = []
        for rs, ws in groups:
            for t in rs:
                t.r[("eng", e)] = me
            for t in ws:
                t.w = me
                t.r = {}
        return ins

    def dma(self, q, fn, reads=(), writes=()):
        reads = _toks(reads)
        writes = _toks(writes)
        self._deps(q, reads, writes)
        k = self.dma_next
        self.dma_next = (k + 1) % self.NDMA
        if self.dma_val[k] > 0:
            self._wait(q, ("dma", k, self.dma_val[k]))
        ins = fn(self.eng[q])
        self.dma_val[k] += 16
        ins.then_inc(self.dma_sems[k], 16)
        me = ("dma", k, self.dma_val[k])
        for t in reads:
            t.r[("dma", k)] = me
        for t in writes:
            t.w = me
            t.r = {}
        return ins

    def barrier(self, engines=None):
        engines = engines or list(self.eng)
        for e in self.eng:
            assert not self.pending[e]
        for e in engines:
            for f in self.eng:
                if f != e and self.cnt[f] > 0:
                    self._wait(e, ("eng", f, self.cnt[f]))
            for k in range(self.NDMA):
                if self.dma_val[k] > 0:
                    self._wait(e, ("dma", k, self.dma_val[k]))


def _dsize(dt):
    return 2 if dt == BF16 else 4


class KB:
    def __init__(self, dbg=False, stop_after=None):
        self.dbg = dbg
        self.stop_after = stop_after
        nc = self.nc = bass.Bass("TRN2", target_bir_lowering=False)
        self.s = Sched(nc)
        self.arena = nc.alloc_sbuf_tensor("arena", [128, AW], F32)
        self.off = 0
        self.persist = 0
        self.psum = nc.alloc_psum_tensor("psum_all", [128, 4096], F32)
        self.banks = [Tile(self.psum[:, i * 512:(i + 1) * 512], f"bank{i}") for i in range(8)]
        self.ext = {}
        self.outs = {}

    def tile(self, shape, dt=F32, name=""):
        P = shape[0]
        n = 1
        for d in shape[1:]:
            n *= d
        words = (n * _dsize(dt) + 3) // 4
        words = (words + 7) // 8 * 8
        assert self.off + words <= AW, f"arena overflow {name} {self.off}+{words}"
        ap = self.arena[0:P, self.off:self.off + words]
        self.off += words
        if dt != F32:
            ap = ap.bitcast(dt)
        ap = ap[:, 0:n]
        if len(shape) == 3:
            ap = ap.rearrange("p (a b) -> p a b", a=shape[1])
        elif len(shape) == 4:
            ap = ap.rearrange("p (a b c) -> p a b c", a=shape[1], b=shape[2])
        return Tile(ap, name)

    def phase(self):
        self.s.barrier()
        self.off = self.persist
        for b in self.banks:
            b.tok = Tok(b.tok.name)

    def keep(self):
        self.persist = self.off

    def bank_bf(self, i):
        return self.banks[i].ap.bitcast(BF16)

    def bank2(self, i):
        return self.psum[:, i * 512:(i + 2) * 512]

    def inp(self, name, shape, dt=F32):
        t = self.nc.dram_tensor(name, list(shape), dt, kind="ExternalInput")
        self.ext[name] = (tuple(shape), dt)
        return t.ap()

    def scratch(self, name, shape, dt):
        if self.dbg:
            t = self.nc.dram_tensor(name, list(shape), dt, kind="ExternalOutput")
            self.outs[name] = (tuple(shape), dt)
        else:
            t = self.nc.dram_tensor(name, list(shape), dt)
        return t.ap()

    def dma(self, q, out, in_, reads=(), writes=(), **kw):
        return self.s.dma(q, lambda e: e.dma_start(out=out, in_=in_, **kw), reads, writes)

    def mm(self, out, lhsT, rhs, start, stop, reads, writes, sig=True):
        return self.s.op("pe", lambda e: e.matmul(out, lhsT=lhsT, rhs=rhs, start=start, stop=stop), reads, writes, sig)

    def tr(self, out, in_, ident, reads, writes, sig=True):
        return self.s.op("pe", lambda e: e.transpose(out=out, in_=in_, identity=ident), reads, writes, sig)

    def act(self, out, in_, func, reads, writes, bias=0.0, scale=1.0, accum=None):
        if accum is None:
            return self.s.op("act", lambda e: e.activation(out=out, in_=in_, func=func, bias=bias, scale=scale), reads, writes)
        return self.s.op("act", lambda e: e.activation(out=out, in_=in_, func=func, bias=bias, scale=scale, accum_out=accum), reads, writes)

    def v(self, fn, reads, writes):
        return self.s.op("dve", fn, reads, writes)

    def g(self, fn, reads, writes):
        return self.s.op("pool", fn, reads, writes)

    def tt(self, out, a, b, op, reads, writes, eng="dve"):
        return self.s.op(eng, lambda e: e.tensor_tensor(out=out, in0=a, in1=b, op=op), reads, writes)

    def ts(self, out, a, s1, s2, op0, op1, reads, writes, eng="dve", accum=None):
        if accum is not None:
            return self.s.op(eng, lambda e: e.tensor_scalar(out=out, in0=a, scalar1=s1, scalar2=s2, op0=op0, op1=op1, accum_out=accum), reads, writes)
        if s2 is None:
            return self.s.op(eng, lambda e: e.tensor_scalar(out=out, in0=a, scalar1=s1, scalar2=None, op0=op0), reads, writes)
        return self.s.op(eng, lambda e: e.tensor_scalar(out=out, in0=a, scalar1=s1, scalar2=s2, op0=op0, op1=op1), reads, writes)

    def stt(self, out, a, sc, b, op0, op1, reads, writes, eng="dve"):
        return self.s.op(eng, lambda e: e.scalar_tensor_tensor(out=out, in0=a, scalar=sc, in1=b, op0=op0, op1=op1), reads, writes)

    def rsq(self, out, in_, scale, eps, tmp, reads, writes):
        self.act(tmp, in_, AF.Sqrt, reads, writes, bias=eps, scale=scale)
        self.s.op("dve", lambda e: e.reciprocal(out=out, in_=tmp), _toks(writes), _toks(writes))

    def cp(self, out, in_, reads, writes, eng="dve"):
        if eng == "act":
            return self.s.op("act", lambda e: e.copy(out=out, in_=in_), reads, writes)
        return self.s.op(eng, lambda e: e.tensor_copy(out=out, in_=in_), reads, writes)

    def rsqrt(self, out, in_, scale, eps, reads, writes, tmp, neghalf):
        self.ts(tmp.ap if isinstance(tmp, Tile) else tmp, in_, scale, eps, ALU.mult, ALU.add, reads, [tmp])
        self.tt(out, tmp.ap if isinstance(tmp, Tile) else tmp, neghalf, ALU.pow, [tmp], writes, eng="pool")


class Rot:
    def __init__(self, tiles):
        self.t = tiles
        self.i = -1

    def next(self):
        self.i = (self.i + 1) % len(self.t)
        return self.t[self.i]


MCH0 = [(0, 128), (128, 256), (256, 384), (384, 512), (512, 640), (640, 768), (768, 896), (896, 960)]


def rows_of(E, L, i):
    if L == 0:
        if i < 64:
            return E["x"][i * 128:(i + 1) * 128, :]
        return E["ctx"][(i - 64) * 128:(i - 63) * 128, :]
    return E["X2"][i * 128:(i + 1) * 128, :]


def setup_consts(kb, E):
    C = {}
    C["ident_f"] = kb.tile([128, 128], F32, "ident_f")
    C["ident_b"] = kb.tile([128, 128], BF16, "ident_b")
    C["ones_b"] = kb.tile([128, 128], BF16, "ones_b")
    C["ones_f"] = kb.tile([128, 128], F32, "ones_f")
    C["neghalf"] = kb.tile([128, 512], F32, "neghalf")
    C["mc"] = kb.tile([128, 4], F32, "mc")
    C["bd"] = kb.tile([128, 128], F32, "bd")
    C["affT"] = kb.tile([128, 1024], F32, "affT")
    C["affc"] = kb.tile([16, 256], F32, "affc")
    C["modT"] = [kb.tile([128, 48, 2], F32, f"modT{l}") for l in range(2)]
    C["neglam"] = kb.tile([128, 1], F32, "neglam")
    kb.dma("sp", C["ident_f"].ap, E["ident"], [], [C["ident_f"]])
    kb.dma("sp", C["mc"].ap, E["mconst"], [], [C["mc"]])
    kb.dma("sp", C["bd"].ap, E["bdiag"], [], [C["bd"]])
    kb.cp(C["ident_b"].ap, C["ident_f"].ap, [C["ident_f"]], [C["ident_b"]])
    kb.v(lambda e: e.memset(C["ones_b"].ap, 1.0), [], [C["ones_b"]])
    kb.v(lambda e: e.memset(C["ones_f"].ap, 1.0), [], [C["ones_f"]])
    kb.v(lambda e: e.memset(C["neghalf"].ap, -0.5), [], [C["neghalf"]])
    kb.keep()
    return C


def phase_mod(kb, E, C):
    b = kb.banks
    cc = kb.tile([128, 8, 2], F32, "cc")
    sc = kb.tile([128, 8, 2], F32, "sc")
    kb.dma("sp", cc.ap, E["ccT"], [], [cc])
    kb.act(sc.ap, cc.ap, AF.Silu, [cc], [sc])
    wts = Rot([kb.tile([128, 8, 512], F32, f"wmod{i}") for i in range(2)])
    for l in range(2):
        brow = kb.tile([2, 6144], F32, "brow")
        bT = kb.tile([128, 48], F32, "bT")
        modsb = kb.tile([2, 6144], F32, "modsb")
        kb.dma("sp", brow[0:1, :], E[f"bmod{l}"], [], [brow])
        kb.dma("sp", brow[1:2, :], E[f"bmod{l}"], [], [brow])
        kb.dma("sp", bT.ap, E[f"bmodT{l}"], [], [bT])
        for j in range(12):
            wt = wts.next()
            kb.dma("sp", wt.ap, E[f"wmod{l}"][j], [], [wt])
            bk = b[j % 2]
            for k in range(8):
                kb.mm(bk[0:2, :], sc[:, k, :], wt[:, k, :], k == 0, k == 7, [sc, wt], [bk], sig=(k == 7))
            kb.tt(modsb[:, j * 512:(j + 1) * 512], bk[0:2, :], brow[:, j * 512:(j + 1) * 512], ALU.add, [bk, brow], [modsb])
            for q in range(4):
                c48 = j * 4 + q
                if c48 >= 16:
                    continue
                bk2 = b[2 + c48 % 2]
                for k in range(8):
                    kb.mm(bk2[:, 0:2], wt[:, k, q * 128:(q + 1) * 128], sc[:, k, :], k == 0, k == 7, [sc, wt], [bk2], sig=(k == 7))
                kb.ts(C["modT"][l][:, c48, :], bk2[:, 0:2], bT[:, c48:c48 + 1], None, ALU.add, None, [bk2, bT], [C["modT"][l]])
        kb.dma("sp", E["MOD"][l], modsb.ap, [modsb], [])


def bc_load(kb, E, l, st, ch, name, plus1=False):
    t = kb.tile([128, 1024], F32, name)
    src = E["MOD"][l][st, ch * 1024:(ch + 1) * 1024].partition_broadcast(128)
    kb.dma("sp", t.ap, src, [], [t])
    if plus1:
        kb.ts(t.ap, t.ap, 1.0, None, ALU.add, None, [t], [t], eng="pool")
    return t


def bc_row(kb, src_row, name):
    t = kb.tile([128, 1024], F32, name)
    kb.dma("sp", t.ap, src_row.partition_broadcast(128), [], [t])
    return t


def phase_A0(kb, E, C):
    b = kb.banks
    modT = C["modT"][0]
    ident_b, ones_b, ones_f, neghalf = C["ident_b"], C["ones_b"], C["ones_f"], C["neghalf"]
    W = [kb.tile([128, 8, 1472], BF16, f"W{st}") for st in range(2)]
    bcol = kb.tile([128, 8, 2], F32, "bcol")
    brow = [kb.tile([1, 512], BF16, f"brow{st}") for st in range(2)]
    Wq = kb.tile([128, 2, 1024], BF16, "Wq")
    Wk = kb.tile([128, 1024], BF16, "Wk")
    Wv = kb.tile([128, 512], BF16, "Wv")
    wsT = kb.tile([128, 512], BF16, "wsT")
    gln = kb.tile([128, 8], F32, "gln")
    Rt = kb.tile([128, 512], F32, "Rt")
    mark = kb.off
    wtmp = kb.tile([128, 8, 1472], F32, "wtmp")
    kb.dma("sp", wtmp.ap, E["win0"], [], [wtmp])
    Wun = kb.tile([128, 8, 1472], BF16, "Wun")
    for k in range(8):
        kb.cp(Wun[:, k, :], wtmp[:, k, :], [wtmp], [Wun], eng=("act" if k % 2 else "dve"))
    shc = kb.tile([128, 8, 2], BF16, "shc")
    kb.cp(shc.ap, modT[:, 0:8, :], [modT], [shc])
    A1 = kb.tile([128, 8, 2], F32, "A1")
    kb.ts(A1.ap, modT[:, 8:16, :], 1.0, None, ALU.add, None, [modT], [A1])
    for st in range(2):
        for k in range(8):
            if k % 2:
                kb.act(W[st][:, k, :], wtmp[:, k, :], AF.Identity, [wtmp, A1], [W[st]], scale=A1[:, k, st:st + 1])
            else:
                kb.ts(W[st][:, k, :], wtmp[:, k, :], A1[:, k, st:st + 1], None, ALU.mult, None, [wtmp, A1], [W[st]])
    for mi, (a, bb) in enumerate(MCH0):
        for k in range(8):
            kb.mm(b[0][0:bb - a, mi * 2:mi * 2 + 2], Wun[:, k, a:bb], shc[:, k, :], k == 0, k == 7, [Wun, shc], [b[0]], sig=(k == 7))
    kb.cp(bcol[:, 0:7, :], b[0][:, 0:14].rearrange("p (a b) -> p a b", a=7), [b[0]], [bcol])
    kb.cp(bcol[0:64, 7, :], b[0][0:64, 14:16], [b[0]], [bcol])
    for st in range(2):
        for k in range(8):
            kb.mm(b[1][0:1, :], shc[:, k, st:st + 1], Wun[:, k, 960:1472], k == 0, k == 7, [Wun, shc], [b[1]], sig=(k == 7))
        kb.cp(brow[st].ap, b[1][0:1, :], [b[1]], [brow[st]])
    qn = kb.tile([128, 2], F32, "qn")
    kvn = kb.tile([128, 1], F32, "kvn")
    kb.dma("sp", qn.ap, E["qnorm"], [], [qn])
    kb.dma("sp", kvn.ap, E["kvnorm"], [], [kvn])
    wq_f = kb.tile([128, 2, 1024], F32, "wq_f")
    kb.dma("sp", wq_f.ap, E["wuqx"], [], [wq_f])
    for r in range(2):
        kb.ts(Wq[:, r, :], wq_f[:, r, :], qn[:, r:r + 1], None, ALU.mult, None, [wq_f, qn], [Wq])
    wk_f = kb.tile([128, 1024], F32, "wk_f")
    kb.dma("sp", wk_f.ap, E["wukx"], [], [wk_f])
    kb.ts(Wk.ap, wk_f.ap, kvn[:, 0:1], None, ALU.mult, None, [wk_f, kvn], [Wk])
    wv_f = kb.tile([128, 512], F32, "wv_f")
    kb.dma("sp", wv_f.ap, E["wuv"], [], [wv_f])
    kb.ts(Wv.ap, wv_f.ap, kvn[:, 0:1], None, ALU.mult, None, [wv_f, kvn], [Wv])
    wsT_f = kb.tile([128, 512], F32, "wsT_f")
    kb.dma("sp", wsT_f.ap, E["wsT"], [], [wsT_f])
    kb.cp(wsT.ap, wsT_f.ap, [wsT_f], [wsT])
    kb.dma("sp", gln.ap, E["gln"], [], [gln])
    bs_bc = kb.tile([128, 512], F32, "bs_bc")
    kb.dma("sp", bs_bc.ap, E["gbs"][0, :].partition_broadcast(128), [], [bs_bc])
    kb.mm(b[2].ap, ones_f.ap, wsT_f.ap, True, True, [ones_f, wsT_f], [b[2]])
    for g in range(4):
        gs = slice(g * 128, (g + 1) * 128)
        kb.stt(Rt[:, gs], b[2][:, gs], gln[:, 4 + g:5 + g], bs_bc[:, gs], ALU.mult, ALU.add, [b[2], gln, bs_bc], [Rt])
    kb.s.barrier()
    kb.off = mark
    xTs = Rot([kb.tile([128, 8, 512], BF16, f"xT{i}") for i in range(2)])
    xbs = Rot([kb.tile([128, 1024], BF16, f"xb{i}") for i in range(8)])
    guT = kb.tile([128, 4, 512], BF16, "guT")
    tq = kb.tile([128, 2, 512], F32, "tq")
    tkv = kb.tile([128, 512], F32, "tkv")
    sq = kb.tile([128, 3, 512], BF16, "sq")
    krx = kb.tile([64, 512], F32, "krx")
    css = Rot([kb.tile([64, 512], F32, f"cs{i}") for i in range(2)])
    tmpA = kb.tile([128, 512], F32, "tmpA")
    rstd_q = kb.tile([128, 512], F32, "rstd_q")
    rstd_kv = kb.tile([128, 512], F32, "rstd_kv")
    cqn = kb.tile([128, 2, 512], BF16, "cqn")
    ckvn = kb.tile([128, 512], BF16, "ckvn")
    t1 = kb.tile([64, 512], F32, "t1")
    t2 = kb.tile([32, 512], F32, "t2")
    krr = kb.tile([32, 512], BF16, "krr")
    kts = Rot([kb.tile([128, 512], BF16, f"kt{i}") for i in range(2)])
    qts = Rot([kb.tile([128, 512], BF16, f"qt{i}") for i in range(2)])
    vaugs = Rot([kb.tile([128, 8, 128], BF16, f"vaug{i}") for i in range(2)])
    for t in kts.t + qts.t:
        kb.v(lambda e: e.memset(t.ap, 0.0), [], [t])
    for t in vaugs.t:
        kb.v(lambda e: e.memset(t.ap, 1.0), [], [t])
    gv = kb.tile([128, 512], F32, "gv")
    st6 = kb.tile([128, 6], F32, "st6")
    mv = kb.tile([128, 2], F32, "mv")
    rs1 = kb.tile([128, 2], F32, "rs1")
    vhat = kb.tile([128, 512], BF16, "vhat")
    tmpg = kb.tile([128, 512], F32, "tmpg")
    aTs = Rot([kb.tile([128, 4, 128], BF16, f"aT{i}") for i in range(2)])
    AT0v = E["AT0"].rearrange("(g c) t -> c g t", c=128)
    V0v = E["V0"].rearrange("h p kt d -> p h kt d")

    def loadX(gi_):
        res = []
        for tl_ in range(4 if gi_ < 16 else 2):
            xb_ = xbs.next()
            kb.dma("pool", xb_.ap, rows_of(E, 0, gi_ * 4 + tl_), [], [xb_])
            res.append(xb_)
        return res

    def stageL(gi):
        ntl = 4 if gi < 16 else 2
        Wd = ntl * 128
        st = 0 if gi < 16 else 1
        col0 = gi * 512
        xT = xTs.next()
        if gi == 0:
            xbox[0] = loadX(0)
        xb_cur = xbox[0]
        if gi + 1 < 17:
            xbox[0] = loadX(gi + 1)
        for tl in range(ntl):
            i = gi * 4 + tl
            xb = xb_cur[tl]
            pb = kb.bank_bf(0)
            for k in range(8):
                kb.tr(pb[:, k * 128:(k + 1) * 128], xb[:, k * 128:(k + 1) * 128], ident_b.ap, [xb, ident_b], [b[0]], sig=(k == 7))
            kb.cp(xT[:, :, tl * 128:(tl + 1) * 128], pb.rearrange("p (a b) -> p a b", a=8), [b[0]], [xT], eng=("act" if tl % 2 else "dve"))
        return xT

    def stageP(gi, xT):
        ntl = 4 if gi < 16 else 2
        Wd = ntl * 128
        st = 0 if gi < 16 else 1
        col0 = gi * 512
        cs = css.next()
        kb.dma("sp", cs[:, 0:Wd], E["cs0"][:, col0:col0 + Wd], [], [cs])
        for mi, (a, bb) in enumerate(MCH0):
            M_ = bb - a
            bk = b[1 + mi % 2]
            for k in range(8):
                kb.mm(bk[0:M_, 0:Wd], W[st][:, k, a:bb], xT[:, k, 0:Wd], k == 0, k == 7, [W[st], xT], [bk], sig=(k == 7))
            bias = bcol[0:M_, mi, st:st + 1]
            if mi < 4:
                kb.act(guT[:, mi, 0:Wd], bk[:, 0:Wd], AF.Gelu, [bk, bcol], [guT], bias=bias)
            elif mi < 6:
                r = mi - 4
                kb.act(tq[:, r, 0:Wd], bk[:, 0:Wd], AF.Identity, [bk, bcol], [tq], bias=bias)
                kb.act(sq[:, r, 0:Wd], bk[:, 0:Wd], AF.Square, [bk, bcol], [sq], bias=bias)
            elif mi == 6:
                kb.act(tkv[:, 0:Wd], bk[:, 0:Wd], AF.Identity, [bk, bcol], [tkv], bias=bias)
                kb.act(sq[:, 2, 0:Wd], bk[:, 0:Wd], AF.Square, [bk, bcol], [sq], bias=bias)
            else:
                kb.act(krx[:, 0:Wd], bk[0:64, 0:Wd], AF.Identity, [bk, bcol], [krx], bias=bias)
        kb.mm(b[5][:, 0:Wd], ones_b.ap, sq[:, 0, 0:Wd], True, False, [ones_b, sq], [b[5]], sig=False)
        kb.mm(b[5][:, 0:Wd], ones_b.ap, sq[:, 1, 0:Wd], False, True, [ones_b, sq], [b[5]])
        kb.rsq(rstd_q[:, 0:Wd], b[5][:, 0:Wd], 1.0 / 256, 1e-6, tmpA[:, 0:Wd], [b[5]], [tmpA, rstd_q])
        kb.mm(b[5][:, 0:Wd], ones_b.ap, sq[:, 2, 0:Wd], True, True, [ones_b, sq], [b[5]])
        kb.rsq(rstd_kv[:, 0:Wd], b[5][:, 0:Wd], 1.0 / 128, 1e-6, tmpA[:, 0:Wd], [b[5]], [tmpA, rstd_kv])
        for r in range(2):
            kb.tt(cqn[:, r, 0:Wd], tq[:, r, 0:Wd], rstd_q[:, 0:Wd], ALU.mult, [tq, rstd_q], [cqn])
        kb.tt(ckvn[:, 0:Wd], tkv[:, 0:Wd], rstd_kv[:, 0:Wd], ALU.mult, [tkv, rstd_kv], [ckvn])
        kb.tt(t1[0:32, 0:Wd], krx[0:32, 0:Wd], cs[0:32, 0:Wd], ALU.mult, [krx, cs], [t1])
        kb.tt(t2[0:32, 0:Wd], krx[32:64, 0:Wd], cs[32:64, 0:Wd], ALU.mult, [krx, cs], [t2])
        kb.tt(krr[:, 0:Wd], t1[0:32, 0:Wd], t2[0:32, 0:Wd], ALU.add, [t1, t2], [krr])
        for h in range(8):
            bk = b[6 + h % 2]
            kb.mm(bk[:, 0:Wd], Wk[:, h * 128:(h + 1) * 128], ckvn[:, 0:Wd], True, True, [Wk, ckvn], [bk])
            kt = kts.next()
            kb.cp(kt[64:128, 0:Wd], bk[64:128, 0:Wd], [bk], [kt], eng="act")
            kb.cp(kt[0:32, 0:Wd], krr[:, 0:Wd], [krr], [kt], eng="pool")
            kb.dma("sp", E["KT"][h][:, col0:col0 + Wd], kt[:, 0:Wd], [kt], [])
        for h in range(8):
            bk = b[6 + h % 2]
            for r in range(2):
                kb.mm(bk[:, 0:Wd], Wq[:, r, h * 128:(h + 1) * 128], cqn[:, r, 0:Wd], r == 0, r == 1, [Wq, cqn], [bk], sig=(r == 1))
            qt = qts.next()
            kb.cp(qt[64:128, 0:Wd], bk[64:128, 0:Wd], [bk], [qt], eng="act")
            kb.tt(t1[0:32, 0:Wd], bk[0:32, 0:Wd], cs[0:32, 0:Wd], ALU.mult, [bk, cs], [t1])
            kb.tt(t2[0:32, 0:Wd], bk[32:64, 0:Wd], cs[32:64, 0:Wd], ALU.mult, [bk, cs], [t2])
            kb.tt(qt[0:32, 0:Wd], t1[0:32, 0:Wd], t2[0:32, 0:Wd], ALU.add, [t1, t2], [qt])
            kb.dma("sp", E["QT"][h][:, col0:col0 + Wd], qt[:, 0:Wd], [qt], [])
        for tl in range(ntl):
            i = gi * 4 + tl
            ts_ = slice(tl * 128, (tl + 1) * 128)
            kb.mm(b[3].ap, ckvn[:, ts_], Wv.ap, True, True, [ckvn, Wv], [b[3]])
            va = vaugs.next()
            kb.cp(va[:, :, 0:64], b[3].ap.rearrange("p (a b) -> p a b", a=8), [b[3]], [va], eng="act")
            kb.dma("sp", V0v[:, :, i, :], va.ap, [va], [])
            for k in range(8):
                kb.mm(b[4].ap, xT[:, k, ts_], W[st][:, k, 960:1472], k == 0, False, [xT, W[st]], [b[4]], sig=False)
            kb.mm(b[4].ap, ones_b[0:1, 0:128], brow[st].ap, False, True, [ones_b, brow[st]], [b[4]])
            kb.act(gv.ap, b[4].ap, AF.Gelu, [b[4]], [gv])
            kb.v(lambda e: e.bn_stats(out=st6.ap, in_=gv.ap), [gv], [st6])
            kb.v(lambda e: e.bn_aggr(out=mv.ap, in_=st6.ap), [st6], [mv])
            kb.ts(rs1[:, 0:1], mv[:, 1:2], 1e-5, None, ALU.add, None, [mv], [rs1])
            kb.tt(rs1[:, 1:2], rs1[:, 0:1], neghalf[:, 0:1], ALU.pow, [rs1, neghalf], [rs1], eng="pool")
            kb.ts(vhat.ap, gv.ap, mv[:, 0:1], rs1[:, 1:2], ALU.subtract, ALU.mult, [gv, mv, rs1], [vhat])
            for g in range(4):
                gs = slice(g * 128, (g + 1) * 128)
                kb.mm(b[5][:, gs], vhat[:, gs], wsT[:, gs], True, True, [vhat, wsT], [b[5]], sig=(g == 3))
            for g in range(4):
                gs = slice(g * 128, (g + 1) * 128)
                kb.stt(tmpg[:, gs], b[5][:, gs], gln[:, g:g + 1], Rt[:, gs], ALU.mult, ALU.add, [b[5], gln, Rt], [tmpg])
            aT = aTs.next()
            kb.tt(aT.ap, tmpg.ap.rearrange("p (a b) -> p a b", a=4), guT[:, :, ts_], ALU.mult, [tmpg, guT], [aT])
            kb.dma("sp", AT0v[:, :, i * 128:(i + 1) * 128], aT.ap, [aT], [])

    xbox = [None]
    xT_prev = None
    for gi in range(17):
        xT_cur = stageL(gi)
        if xT_prev is not None:
            stageP(gi - 1, xT_prev)
        xT_prev = xT_cur
    stageP(16, xT_prev)


def phase_attn(kb, E, C, L):
    b = kb.banks
    ones_b, ones_f = C["ones_b"], C["ones_f"]
    scale = (96 ** -0.5) if L == 0 else 0.125
    nqb = 17 if L == 0 else 16
    Vd = E["V0"] if L == 0 else E["V1"]
    OT = E["OT"]
    KTb = [kb.tile([128, NALL], BF16, f"KTb{i}") for i in range(2)]
    Vb = [kb.tile([128, 66, 128], BF16, f"Vb{i}") for i in range(2)]
    qts = Rot([kb.tile([128, 512], BF16, f"aq{i}") for i in range(2)])
    if L == 0:
        Ps = Rot([kb.tile([128, 512], BF16, f"P{j}") for j in range(3)])
        rl = [kb.tile([128, 512], F32, f"rl{i}") for i in range(2)]
        ots = Rot([kb.tile([64, 512], BF16, f"ot{i}") for i in range(3)])
    else:
        Ps = Rot([kb.tile([128, 2, 512], BF16, f"P12_{j}") for j in range(4)])
        accs = Rot([kb.tile([128, 512], F32, f"acc{j}") for j in range(2)])
        rls = [kb.tile([128, 512], F32, f"rl{i}") for i in range(2)]
        tAs = [kb.tile([128, 512], F32, f"tA{i}") for i in range(2)]
        ox = kb.tile([128, 512], F32, "ox")
        osb = [kb.tile([128, 512], F32, f"osb{i}") for i in range(2)]
        l2s = kb.tile([128, 512], F32, "l2s")
        sqx = kb.tile([128, 512], BF16, "sqx")
        tmpR = kb.tile([128, 512], F32, "tmpR")
        rstd = kb.tile([128, 512], F32, "rstdo")
        ots = Rot([kb.tile([128, 512], BF16, f"ot{i}") for i in range(2)])
        subg = kb.tile([128, 1], F32, "subg")
        kb.dma("sp", subg.ap, E["subln"], [], [subg])
        kb.ts(subg.ap, subg.ap, 1.0 - LAM_INIT, None, ALU.mult, None, [subg], [subg])
        neglam = C["neglam"]

    def load_head(h, bi):
        kb.dma("sp", KTb[bi].ap, E["KT"][h], [], [KTb[bi]])
        kb.dma("sp", Vb[bi].ap, Vd[h], [], [Vb[bi]])

    def loadQ(h_, qb_):
        Wd_ = 512 if qb_ < 16 else 256
        qt_ = qts.next()
        kb.dma("sp", qt_[:, 0:Wd_], E["QT"][h_][:, qb_ * 512:qb_ * 512 + Wd_], [], [qt_])
        return qt_

    pending_epi = []
    load_head(0, 0)
    for h in range(8):
        bi = h % 2
        if h + 1 < 8:
            load_head(h + 1, (h + 1) % 2)
        K_ = KTb[bi]
        V_ = Vb[bi]
        for qb in range(nqb):
            Wd = 512 if qb < 16 else 256
            col0 = qb * 512
            ktl = list(range(66)) if qb < 16 else [64, 65]
            n = len(ktl)
            if h == 0 and qb == 0:
                qt_next = loadQ(0, 0)
            qt = qt_next
            if qb + 1 < nqb:
                qt_next = loadQ(h, qb + 1)
            elif h + 1 < 8:
                qt_next = loadQ(h + 1, 0)
            if L == 0:
                O = b[4 + qb % 2]

                def S_(j):
                    kt = ktl[j]
                    sb = b[j % 3]
                    kb.mm(sb[:, 0:Wd], K_[:, kt * 128:(kt + 1) * 128], qt[:, 0:Wd], True, True, [K_, qt], [sb])
                S_(0)
                if n > 1:
                    S_(1)
                for j in range(n):
                    if j + 2 < n:
                        S_(j + 2)
                    sb = b[j % 3]
                    P = Ps.next()
                    kb.act(P[:, 0:Wd], sb[:, 0:Wd], AF.Exp, [sb], [P], scale=scale)
                    kb.mm(O[:, 0:Wd], V_[:, ktl[j], :], P[:, 0:Wd], j == 0, j == n - 1, [V_, P], [O])
                r = rl[qb % 2]
                kb.v(lambda e: e.reciprocal(out=r[64:128, 0:Wd], in_=O[64:128, 0:Wd]), [O], [r])
                ot = ots.next()
                kb.tt(ot[0:64, 0:Wd], O[0:64, 0:Wd], r[64:128, 0:Wd], ALU.mult, [O, r], [ot])
                kb.dma("sp", OT[h * 64:(h + 1) * 64, col0:col0 + Wd], ot[:, 0:Wd], [ot], [])
            else:
                O12 = [b[4], b[5]]
                L2 = b[6]
                acc = accs.next()

                def S_(j):
                    kt = ktl[j]
                    for i in range(2):
                        sb = b[(j % 2) * 2 + i]
                        ps = slice(i * 64, (i + 1) * 64)
                        kb.mm(sb.ap, K_[ps, kt * 128:(kt + 1) * 128], qt[ps, :], True, True, [K_, qt], [sb], sig=(i == 1))
                S_(0)
                for j in range(n):
                    if j + 1 < n:
                        S_(j + 1)
                    s0 = (j % 2) * 2
                    P = Ps.next()
                    kb.act(P.ap.rearrange("p a b -> p (a b)"), kb.bank2(s0), AF.Exp, [b[s0], b[s0 + 1]], [P], scale=scale)
                    for i in range(2):
                        kb.mm(O12[i].ap, V_[:, ktl[j], :], P[:, i, :], j == 0, j == n - 1, [V_, P], [O12[i]], sig=False)
                    kb.mm(L2.ap, ones_b.ap, P[:, 1, :], j == 0, j == n - 1, [ones_b, P], [L2])
                    if j == 0:
                        kb.cp(acc.ap, P[:, 0, :], [P], [acc])
                    else:
                        kb.tt(acc.ap, acc.ap, P[:, 0, :], ALU.add, [acc, P], [acc])
                    if j % 6 == 5 and pending_epi:
                        pending_epi.pop(0)()
                while pending_epi:
                    pending_epi.pop(0)()
                kb.cp(osb[0].ap, O12[0].ap, [O12[0]], [osb[0]], eng="act")
                kb.cp(osb[1].ap, O12[1].ap, [O12[1]], [osb[1]])
                kb.cp(l2s.ap, L2.ap, [L2], [l2s])
                def mk_epi(acc=acc, h=h, col0=col0, Wd=Wd):
                    def e0():
                        kb.mm(b[7].ap, ones_f.ap, acc.ap, True, True, [ones_f, acc], [b[7]])
                        kb.v(lambda e: e.reciprocal(out=rls[0].ap, in_=b[7].ap), [b[7]], [rls[0]])

                    def e1():
                        kb.v(lambda e: e.reciprocal(out=rls[1].ap, in_=l2s.ap), [l2s], [rls[1]])

                    def e2():
                        for i in range(2):
                            kb.tt(tAs[i].ap, osb[i].ap, rls[i].ap, ALU.mult, [osb[i], rls[i]], [tAs[i]], eng="pool")

                    def e3():
                        kb.stt(ox.ap, tAs[1].ap, neglam[:, 0:1], tAs[0].ap, ALU.mult, ALU.add, [tAs[0], tAs[1], neglam], [ox])
                        kb.tt(sqx.ap, ox.ap, ox.ap, ALU.mult, [ox], [sqx], eng="pool")

                    def e4():
                        kb.mm(b[7].ap, ones_b.ap, sqx.ap, True, True, [ones_b, sqx], [b[7]])
                        kb.act(tmpR.ap, b[7].ap, AF.Sqrt, [b[7]], [tmpR], bias=1e-6, scale=1.0 / 128)

                    def e5():
                        kb.v(lambda e: e.reciprocal(out=rstd.ap, in_=tmpR.ap), [tmpR], [rstd])

                    def e6():
                        ot = ots.next()
                        kb.stt(ot.ap, ox.ap, subg[:, 0:1], rstd.ap, ALU.mult, ALU.mult, [ox, subg, rstd], [ot])
                        kb.dma("sp", OT[h * 128:(h + 1) * 128, col0:col0 + Wd], ot.ap, [ot], [])
                    return [e0, e1, e2, e3, e4, e5, e6]
                while pending_epi:
                    pending_epi.pop(0)()
                pending_epi.extend(mk_epi())
                if h == 7 and qb == nqb - 1:
                    while pending_epi:
                        pending_epi.pop(0)()


def layer_norm_tile(kb, y, gam, bet, out, st12, mv, rs1, neghalf):
    kb.v(lambda e: e.bn_stats(out=st12[:, 0:6], in_=y[:, 0:512]), [y], [st12])
    kb.v(lambda e: e.bn_stats(out=st12[:, 6:12], in_=y[:, 512:1024]), [y], [st12])
    kb.v(lambda e: e.bn_aggr(out=mv.ap, in_=st12.ap), [st12], [mv])
    kb.ts(rs1[:, 0:1], mv[:, 1:2], 1e-5, None, ALU.add, None, [mv], [rs1])
    kb.tt(rs1[:, 1:2], rs1[:, 0:1], neghalf[:, 0:1], ALU.pow, [rs1, neghalf], [rs1], eng="pool")
    kb.stt(rs1[:, 0:1], mv[:, 0:1], -1.0, rs1[:, 1:2], ALU.mult, ALU.mult, [mv, rs1], [rs1])
    kb.act(y.ap, y.ap, AF.Identity, [y, rs1], [y], bias=rs1[:, 0:1], scale=rs1[:, 1:2])
    kb.tt(y.ap, y.ap, gam.ap, ALU.mult, [y, gam], [y], eng="pool")
    kb.tt(out.ap, y.ap, bet.ap, ALU.add, [y, bet], [out])


def lockstep(gens):
    gens = list(gens)
    while gens:
        for g in list(gens):
            try:
                next(g)
            except StopIteration:
                gens.remove(g)


def phase_C(kb, E, C, L):
    b = kb.banks
    ntiles = 66 if L == 0 else 64
    nst = 2 if L == 0 else 1
    ident_b, ident_f, neghalf = C["ident_b"], C["ident_f"], C["neghalf"]
    wout = kb.tile([128, 8, 1024], BF16, "wout")
    kb.dma("pool", wout.ap, E[f"wout{L}"], [], [wout])
    router_f = kb.tile([128, 8, 16], F32, "router_f")
    kb.dma("sp", router_f.ap, E[f"router{L}"], [], [router_f])
    router_b = kb.tile([128, 8, 16], BF16, "router_b")
    kb.cp(router_b.ap, router_f.ap, [router_f], [router_b])
    gate = [bc_load(kb, E, L, st, 2, f"gate{st}", True) for st in range(nst)]
    G1 = kb.tile([128, 1024], F32, "G1")
    B1 = kb.tile([128, 1024], F32, "B1")
    G2 = [kb.tile([128, 1024], F32, f"G2_{st}") for st in range(nst)]
    B2 = [kb.tile([128, 1024], F32, f"B2_{st}") for st in range(nst)]
    mark = kb.off
    gam = bc_row(kb, E[f"lnmix{L}"][0, :], "gam")
    bet = bc_row(kb, E[f"lnmix{L}"][1, :], "bet")
    kb.ts(G1.ap, gam.ap, ALPHA, None, ALU.mult, None, [gam], [G1])
    kb.ts(B1.ap, bet.ap, ALPHA, None, ALU.mult, None, [bet], [B1])
    for st in range(nst):
        Af = bc_load(kb, E, L, st, 4, f"Af{st}", True)
        shf = bc_load(kb, E, L, st, 3, f"shf{st}")
        kb.tt(G2[st].ap, gam.ap, Af.ap, ALU.mult, [gam, Af], [G2[st]])
        kb.tt(B2[st].ap, bet.ap, Af.ap, ALU.mult, [bet, Af], [B2[st]])
        kb.tt(B2[st].ap, B2[st].ap, shf.ap, ALU.add, [B2[st], shf], [B2[st]])
    kb.s.barrier()
    kb.off = mark
    affx = [kb.tile([128, 16, 8], F32, f"affx{s}") for s in range(8)]
    for t in affx:
        kb.v(lambda e: e.memset(t.ap, 0.0), [], [t])
    catTs = Rot([kb.tile([128, 8, 128], BF16, f"catT{i}") for i in range(4)])
    xts = Rot([kb.tile([128, 1024], F32, f"xt{i}") for i in range(4)])

    def mk(shape, dt, nm):
        return [Rot([kb.tile(shape, dt, f"{nm}{sl}_{i}") for i in range(2)]) for sl in range(2)]
    tmps, ysC, x1as, t2s = mk([128, 1024], F32, "tmpC"), mk([128, 1024], F32, "yC"), mk([128, 1024], F32, "x1a"), mk([128, 1024], F32, "t2C")
    hfs = mk([128, 1024], BF16, "hf")
    hfTs = mk([128, 8, 128], BF16, "hfT")
    st12s, mvs, rs1s, sms, exs = (mk([128, 12], F32, "st12"), mk([128, 2], F32, "mvC"), mk([128, 2], F32, "rs1C"),
                                  mk([128, 4], F32, "sm"), mk([128, 16], F32, "ex"))
    affc_t = kb.tile([128, 16], F32, "affc_t")
    affrows = mk([128, 16], F32, "affrow")
    zrow = kb.tile([128, 16], F32, "zrow")
    kb.v(lambda e: e.memset(zrow.ap, 0.0), [], [zrow])
    kb.dma("sp", E["AFF"][NALL:NPAD, :], zrow.ap, [zrow], [])
    AT0v = E["AT0"].rearrange("(g c) t -> c g t", c=128)
    OTv = E["OT"].rearrange("(k p) t -> p k t", p=128)

    def loadsC(i):
        cols = slice(i * 128, (i + 1) * 128)
        catT = catTs.next()
        if L == 0:
            kb.dma("sp", catT[:, 0:4, :], AT0v[:, :, cols], [], [catT])
            kb.dma("sp", catT[:, 4:8, :], OTv[:, 0:4, cols], [], [catT])
        else:
            kb.dma("sp", catT.ap, OTv[:, :, cols], [], [catT])
        xt = xts.next()
        kb.dma("sp", xt.ap, rows_of(E, L, i), [], [xt])
        return catT, xt

    def tileA(i, sl, ld, out):
        st = 0 if i < 64 else 1
        catT, xt = ld
        tmp, y, st12, mv, rs1 = tmps[sl].next(), ysC[sl].next(), st12s[sl].next(), mvs[sl].next(), rs1s[sl].next()
        mb = (b[0], b[1]) if sl == 0 else (b[6], b[7])
        for half in range(2):
            hs = slice(half * 512, (half + 1) * 512)
            for k in range(8):
                kb.mm(mb[half].ap, catT[:, k, :], wout[:, k, hs], k == 0, k == 7, [catT, wout], [mb[half]], sig=(k == 7))
            yield
            kb.tt(tmp[:, hs], mb[half].ap, gate[st][:, hs], ALU.mult, [mb[half], gate[st]], [tmp])
            yield
        kb.stt(y.ap, xt.ap, ALPHA, tmp.ap, ALU.mult, ALU.add, [xt, tmp], [y])
        yield
        kb.v(lambda e: e.bn_stats(out=st12[:, 0:6], in_=y[:, 0:512]), [y], [st12])
        yield
        kb.v(lambda e: e.bn_stats(out=st12[:, 6:12], in_=y[:, 512:1024]), [y], [st12])
        yield
        kb.v(lambda e: e.bn_aggr(out=mv.ap, in_=st12.ap), [st12], [mv])
        kb.ts(rs1[:, 0:1], mv[:, 1:2], 1e-5, None, ALU.add, None, [mv], [rs1])
        yield
        kb.tt(rs1[:, 1:2], rs1[:, 0:1], neghalf[:, 0:1], ALU.pow, [rs1, neghalf], [rs1], eng="pool")
        yield
        kb.stt(rs1[:, 0:1], mv[:, 0:1], -1.0, rs1[:, 1:2], ALU.mult, ALU.mult, [mv, rs1], [rs1])
        yield
        kb.act(y.ap, y.ap, AF.Identity, [y, rs1], [y], bias=rs1[:, 0:1], scale=rs1[:, 1:2])
        yield
        x1a, t2, hf = x1as[sl].next(), t2s[sl].next(), hfs[sl].next()
        kb.tt(tmp.ap, y.ap, G1.ap, ALU.mult, [y, G1], [tmp], eng="pool")
        kb.tt(t2.ap, y.ap, G2[st].ap, ALU.mult, [y, G2[st]], [t2])
        yield
        kb.tt(hf.ap, t2.ap, B2[st].ap, ALU.add, [t2, B2[st]], [hf])
        kb.dma("sp", E["HF"][i * 128:(i + 1) * 128, :], hf.ap, [hf], [])
        yield
        kb.tt(x1a.ap, tmp.ap, B1.ap, ALU.add, [tmp, B1], [x1a], eng="pool")
        kb.dma("sp", E["FACC"][i * 128:(i + 1) * 128, :], x1a.ap, [x1a], [])
        out.append(hf)
        yield

    def tileB(i, sl, hf):
        hfT, sm, ex = hfTs[sl].next(), sms[sl].next(), exs[sl].next()
        tb = 2 + sl
        pb = kb.bank_bf(tb)
        for k in range(8):
            kb.tr(pb[:, k * 128:(k + 1) * 128], hf[:, k * 128:(k + 1) * 128], ident_b.ap, [hf, ident_b], [b[tb]], sig=(k == 7))
        yield
        kb.cp(hfT.ap, pb.rearrange("p (a b) -> p a b", a=8), [b[tb]], [hfT], eng="act")
        yield
        lg = b[tb][:, 512 - 16:512]
        for k in range(8):
            kb.mm(lg, hfT[:, k, :], router_b[:, k, :], k == 0, k == 7, [hfT, router_b], [b[tb]], sig=(k == 7))
        yield
        kb.v(lambda e: e.reduce_max(out=sm[:, 0:1], in_=lg, axis=mybir.AxisListType.X), [b[tb]], [sm])
        yield
        kb.ts(sm[:, 1:2], sm[:, 0:1], -1.0, None, ALU.mult, None, [sm], [sm])
        yield
        kb.act(ex.ap, lg, AF.Exp, [b[tb], sm], [ex, sm], bias=sm[:, 1:2], accum=sm[:, 2:3])
        yield
        kb.v(lambda e: e.reciprocal(out=sm[:, 3:4], in_=sm[:, 2:3]), [sm], [sm])
        yield
        if i < 64:
            s_, j = i % 8, i // 8
            ax = affx[s_]
            ar = affrows[sl].next()
            kb.ts(ar.ap, ex.ap, sm[:, 3:4], None, ALU.mult, None, [ex, sm], [ar])
            kb.dma("sp", E["AFF"][i * 128:(i + 1) * 128, :], ar.ap, [ar], [])
            kb.cp(ax[:, :, s_], ar.ap, [ar], [ax])
            yield
            bk = b[4 + j // 4]
            kb.mm(bk[:, (j % 4) * 128:(j % 4 + 1) * 128], ax.ap.rearrange("p a b -> p (a b)"), ident_f.ap, s_ == 0, s_ == 7,
                  [ax, ident_f], [bk])
        else:
            kb.ts(affc_t.ap, ex.ap, sm[:, 3:4], None, ALU.mult, None, [ex, sm], [affc_t])
            yield
            kb.tr(b[2][0:16, (i - 64) * 128:(i - 63) * 128], affc_t.ap, ident_f.ap, [affc_t, ident_f], [b[2]])
            if i == 65:
                kb.cp(C["affc"].ap, b[2][0:16, 0:256], [b[2]], [C["affc"]])
        yield

    npairs = ntiles // 2
    lds = [loadsC(0), loadsC(1)]
    prev = None
    for p in range(npairs):
        cur_ld = lds
        if p + 1 < npairs:
            lds = [loadsC(2 * p + 2), loadsC(2 * p + 3)]
        outs = [[], []]
        lockstep([tileA(2 * p, 0, cur_ld[0], outs[0]), tileA(2 * p + 1, 1, cur_ld[1], outs[1])])
        if prev is not None:
            lockstep([tileB(2 * (p - 1), 0, prev[0][0]), tileB(2 * (p - 1) + 1, 1, prev[1][0])])
        prev = outs
    lockstep([tileB(2 * (npairs - 1), 0, prev[0][0]), tileB(2 * (npairs - 1) + 1, 1, prev[1][0])])
    kb.cp(C["affT"][:, 0:512], b[4].ap, [b[4]], [C["affT"]])
    kb.cp(C["affT"][:, 512:1024], b[5].ap, [b[5]], [C["affT"]])


def phase_D(kb, E, C, L):
    b = kb.banks
    has_ctx = (L == 0)
    nst = 2 if L == 0 else 1
    ident_b, ident_f, mc, bd = C["ident_b"], C["ident_f"], C["mc"], C["bd"]
    affT = C["affT"]
    gatef = [bc_load(kb, E, L, st, 5, f"gatef{st}", True) for st in range(nst)]
    Wt = [[kb.tile([128, 8, 1024], BF16, f"w{nm}{i}") for nm in ("gate", "up", "down")] for i in range(2)]

    def load_w(e_, bi):
        for wi, nm in enumerate(("gate", "up", "down")):
            kb.dma("pool", Wt[bi][wi].ap, E[f"w{nm}{L}"][e_].rearrange("(k p) n -> p k n", p=128), [], [Wt[bi][wi]])

    load_w(0, 0)
    work = kb.tile([128, 1024], F32, "work")
    kb.cp(work.ap, affT.ap, [affT], [work])
    vals = kb.tile([128, CAP], F32, "vals")
    idxu = kb.tile([128, CAP], U32, "idxu")
    for it in range(CAP // 8):
        sl = slice(it * 8, (it + 1) * 8)
        kb.v(lambda e: e.max(out=vals[:, sl], in_=work.ap), [work], [vals])
        kb.v(lambda e: e.max_index(out=idxu[:, sl], in_max=vals[:, sl], in_values=work.ap), [work, vals], [idxu])
        kb.v(lambda e: e.match_replace(out=work.ap, in_to_replace=vals[:, sl], in_values=work.ap, imm_value=-1.0), [vals, work], [work])
    lo = kb.tile([128, 1], F32, "lo")
    hi = kb.tile([128, 1], F32, "hi")
    mid = kb.tile([128, 1], F32, "mid")
    cnt = kb.tile([128, 1], F32, "cnt")
    d1 = kb.tile([128, 1], F32, "d1")
    ge = kb.tile([128, 1], F32, "ge")
    junk = kb.tile([128, CAP], F32, "junk")
    kb.v(lambda e: e.memset(lo.ap, 0.0), [], [lo])
    kb.v(lambda e: e.memset(hi.ap, 1.0), [], [hi])
    for it in range(30):
        kb.tt(mid.ap, lo.ap, hi.ap, ALU.add, [lo, hi], [mid])
        kb.ts(mid.ap, mid.ap, 0.5, None, ALU.mult, None, [mid], [mid])
        kb.ts(junk.ap, vals.ap, mid[:, 0:1], 0.0, ALU.is_ge, ALU.add, [vals, mid], [junk, cnt], accum=cnt.ap)
        kb.mm(b[0][:, 0:1], bd.ap, cnt.ap, True, True, [bd, cnt], [b[0]])
        kb.ts(ge.ap, b[0][:, 0:1], ECAP - 0.5, None, ALU.is_ge, None, [b[0]], [ge])
        kb.tt(d1.ap, mid.ap, lo.ap, ALU.subtract, [mid, lo], [d1])
        kb.stt(lo.ap, d1.ap, ge[:, 0:1], lo.ap, ALU.mult, ALU.add, [d1, ge, lo], [lo])
        kb.tt(d1.ap, hi.ap, mid.ap, ALU.subtract, [hi, mid], [d1])
        kb.stt(hi.ap, d1.ap, ge[:, 0:1], mid.ap, ALU.mult, ALU.add, [d1, ge, mid], [hi])
    valid = kb.tile([128, CAP], F32, "valid")
    gvv = kb.tile([128, CAP], F32, "gvv")
    kb.ts(valid.ap, vals.ap, lo[:, 0:1], None, ALU.is_ge, None, [vals, lo], [valid])
    kb.tt(gvv.ap, vals.ap, valid.ap, ALU.mult, [vals, valid], [gvv])
    jbi = kb.tile([128, CAP], I32, "jbi")
    kb.v(lambda e: e.tensor_single_scalar(out=jbi.ap, in_=idxu.ap.bitcast(I32), scalar=7, op=ALU.arith_shift_right), [idxu], [jbi])
    idxf = kb.tile([128, CAP], F32, "idxf")
    jbf = kb.tile([128, CAP], F32, "jbf")
    tokf = kb.tile([128, CAP], F32, "tokf")
    kb.cp(idxf.ap, idxu.ap, [idxu], [idxf])
    kb.cp(jbf.ap, jbi.ap, [jbi], [jbf])
    kb.stt(tokf.ap, jbf.ap, 896.0, idxf.ap, ALU.mult, ALU.add, [jbf, idxf], [tokf])
    kb.ts(tokf.ap, tokf.ap, mc[:, 0:1], None, ALU.add, None, [tokf, mc], [tokf])
    mains = []
    for ai, arr in enumerate((tokf, gvv, valid)):
        bk = b[1 + ai]
        kb.tr(bk[:, 0:128], arr[:, 0:128], ident_f.ap, [arr, ident_f], [bk])
        m_ = kb.tile([128, 128], F32, f"main{ai}")
        kb.cp(m_.ap, bk[:, 0:128], [bk], [m_])
        mains.append(m_)
    IDXM = kb.tile([128, 128], I32, "IDXM")
    tmpi = kb.tile([128, 128], F32, "tmpi")
    kb.ts(tmpi.ap, mains[0].ap, mc[:, 1:2], None, ALU.subtract, None, [mains[0], mc], [tmpi])
    kb.tt(tmpi.ap, tmpi.ap, mains[2].ap, ALU.mult, [tmpi, mains[2]], [tmpi])
    kb.ts(tmpi.ap, tmpi.ap, mc[:, 1:2], None, ALU.add, None, [tmpi, mc], [tmpi])
    kb.cp(IDXM.ap, tmpi.ap, [tmpi], [IDXM])
    GM = mains[1]
    keyt = kb.tile([128, 64], F32, "keyt")
    kb.stt(keyt.ap, tokf[:, 128:192], 1.0, valid[:, 128:192], ALU.add, ALU.mult, [tokf, valid], [keyt])
    kdram = Tok("keys")
    kb.dma("sp", E["KEYS"], keyt.ap, [keyt], [kdram])
    kc = kb.tile([16, 512], F32, "kc")
    kb.dma("sp", kc.ap, E["KEYS"].rearrange("(e s) r -> e (s r)", s=8), [kdram], [kc])
    tkey = kb.tile([16, 128], F32, "tkey")
    for it in range(16):
        sl = slice(it * 8, (it + 1) * 8)
        kb.v(lambda e: e.max(out=tkey[:, sl], in_=kc.ap), [kc], [tkey])
        kb.v(lambda e: e.match_replace(out=kc.ap, in_to_replace=tkey[:, sl], in_values=kc.ap, imm_value=-1.0), [tkey, kc], [kc])
    drow = kb.tile([16, 128], F32, "drow")
    kb.dma("sp", drow.ap, E["drow"], [], [drow])
    tval = kb.tile([16, 128], F32, "tval")
    kb.ts(tval.ap, tkey.ap, 0.5, None, ALU.is_ge, None, [tkey], [tval])
    tix = kb.tile([16, 128], F32, "tix")
    kb.stt(tix.ap, tkey.ap, -1.0, drow.ap, ALU.add, ALU.subtract, [tkey, drow], [tix])
    kb.tt(tix.ap, tix.ap, tval.ap, ALU.mult, [tix, tval], [tix])
    kb.tt(tix.ap, tix.ap, drow.ap, ALU.add, [tix, drow], [tix])
    kb.tr(b[4][:, 0:16], tix.ap, ident_f[0:16, 0:16], [tix, ident_f], [b[4]])
    IDXT = kb.tile([128, 16], I32, "IDXT")
    kb.cp(IDXT.ap, b[4][:, 0:16], [b[4]], [IDXT])
    gas = [kb.tile([128, 16], F32, f"ga{e_}") for e_ in range(16)]
    if has_ctx:
        cwork = kb.tile([16, 256], F32, "cwork")
        kb.cp(cwork.ap, C["affc"].ap, [C["affc"]], [cwork])
        cvals = kb.tile([16, CCAP], F32, "cvals")
        cidx = kb.tile([16, CCAP], U32, "cidx")
        for it in range(CCAP // 8):
            sl = slice(it * 8, (it + 1) * 8)
            kb.v(lambda e: e.max(out=cvals[:, sl], in_=cwork.ap), [cwork], [cvals])
            kb.v(lambda e: e.max_index(out=cidx[:, sl], in_max=cvals[:, sl], in_values=cwork.ap), [cwork, cvals], [cidx])
            kb.v(lambda e: e.match_replace(out=cwork.ap, in_to_replace=cvals[:, sl], in_values=cwork.ap, imm_value=-1.0), [cvals, cwork], [cwork])
        cidf = kb.tile([16, CCAP], F32, "cidf")
        kb.cp(cidf.ap, cidx.ap, [cidx], [cidf])
        kb.ts(cidf.ap, cidf.ap, float(NLAT), None, ALU.add, None, [cidf], [cidf])
        kb.tr(b[4][0:32, 0:16], cidf.ap, ident_f[0:16, 0:16], [cidf, ident_f], [b[4]])
        kb.tr(b[4][0:32, 16:32], cvals.ap, ident_f[0:16, 0:16], [cvals, ident_f], [b[4]])
        CIDX = kb.tile([32, 16], I32, "CIDX")
        CG = kb.tile([32, 16], F32, "CG")
        kb.cp(CIDX.ap, b[4][0:32, 0:16], [b[4]], [CIDX])
        kb.cp(CG.ap, b[4][0:32, 16:32], [b[4]], [CG])
    xss = Rot([kb.tile([128, 1024], BF16, f"xs{i}") for i in range(6)])
    xsTs = Rot([kb.tile([128, 8, 512], BF16, f"xsT{i}") for i in range(2)])
    hidTs = Rot([kb.tile([128, 8, 512], BF16, f"hidT{i}") for i in range(2)])
    sgs = Rot([kb.tile([128, 512], F32, f"sg{i}") for i in range(2)])
    ysbs = Rot([kb.tile([128, 1024], F32, f"ysb{i}") for i in range(3)])

    work_items = []
    for e_ in range(16):
        chunks = [(IDXM[:, e_ * 8 + s_:e_ * 8 + s_ + 1], GM[:, e_ * 8 + s_:e_ * 8 + s_ + 1], 128, 0, IDXM, GM) for s_ in range(8)]
        chunks += [(IDXT[:, e_:e_ + 1], gas[e_][:, e_:e_ + 1], 128, 0, IDXT, gas[e_])]
        blocks = [chunks[0:4], chunks[4:8], chunks[8:9]]
        if has_ctx:
            blocks.append([(CIDX[:, e_:e_ + 1], CG[:, e_:e_ + 1], 32, 1, CIDX, CG)])
        for bi_, blk in enumerate(blocks):
            work_items.append((e_, bi_, blk))

    def gathers(blk):
        res = []
        R = blk[0][2]
        for (icol, gcol, R_, st, it_, gt_) in blk:
            xs = xss.next()
            kb.s.dma("pool", lambda q: q.indirect_dma_start(out=xs[0:R, :], out_offset=None, in_=E["HF"],
                                                             in_offset=bass.IndirectOffsetOnAxis(ap=icol, axis=0)),
                     _toks([it_]), _toks([xs]))
            if it_ is IDXT:
                kb.s.dma("pool", lambda q: q.indirect_dma_start(out=gt_.ap, out_offset=None, in_=E["AFF"],
                                                                 in_offset=bass.IndirectOffsetOnAxis(ap=icol, axis=0)),
                         _toks([it_]), _toks([gt_]))
            res.append(xs)
        return res

    prev_sc = []
    cur_sc = []
    g_next = gathers(work_items[0][2])
    for wi_, (e_, bi_, blk) in enumerate(work_items):
        bi = e_ % 2
        if bi_ == 0:
            if e_ + 1 < 16:
                load_w(e_ + 1, (e_ + 1) % 2)
            prev_sc = cur_sc
            cur_sc = []
        wg, wu, wd = Wt[bi]
        R = blk[0][2]
        Wd = R * len(blk)
        xs_list = g_next
        xsT = xsTs.next()
        hidT = hidTs.next()
        for cl, xs in enumerate(xs_list):
            pb = kb.bank_bf(0)
            for k in range(8):
                kb.tr(pb[:, k * 128:k * 128 + R], xs[0:R, k * 128:(k + 1) * 128], ident_b[0:R, 0:R], [xs, ident_b], [b[0]], sig=(k == 7))
            kb.cp(xsT[:, :, cl * R:(cl + 1) * R], pb.rearrange("p (a b) -> p a b", a=8)[:, :, 0:R], [b[0]], [xsT],
                  eng=("act" if cl % 2 else "dve"))
        if wi_ + 1 < len(work_items):
            g_next = gathers(work_items[wi_ + 1][2])
        for ffc in range(8):
            fs = slice(ffc * 128, (ffc + 1) * 128)
            G_ = b[1 + ffc % 2]
            U_ = b[3 + ffc % 2]
            for k in range(8):
                kb.mm(G_[:, 0:Wd], wg[:, k, fs], xsT[:, k, 0:Wd], k == 0, k == 7, [wg, xsT], [G_], sig=(k == 7))
            for k in range(8):
                kb.mm(U_[:, 0:Wd], wu[:, k, fs], xsT[:, k, 0:Wd], k == 0, k == 7, [wu, xsT], [U_], sig=(k == 7))
            sg = sgs.next()
            kb.act(sg[:, 0:Wd], G_[:, 0:Wd], AF.Silu, [G_], [sg])
            kb.tt(hidT[:, ffc, 0:Wd], sg[:, 0:Wd], U_[:, 0:Wd], ALU.mult, [sg, U_], [hidT])
        for cl, (icol, gcol, R_, st, it_, gt_) in enumerate(blk):
            ysb = ysbs.next()
            for half in range(2):
                hs = slice(half * 512, (half + 1) * 512)
                Y_ = b[5 + half]
                for ffc in range(8):
                    kb.mm(Y_[0:R, :], hidT[:, ffc, cl * R:(cl + 1) * R], wd[:, ffc, hs], ffc == 0, ffc == 7, [hidT, wd], [Y_], sig=(ffc == 7))
                kb.stt(ysb[0:R, hs], Y_[0:R, :], gcol, gatef[st][0:R, hs], ALU.mult, ALU.mult, [Y_, gt_, gatef[st]], [ysb])
            tk = Tok("sc")
            kb.s.dma("pool", lambda q: q.indirect_dma_start(out=E["FACC"], out_offset=bass.IndirectOffsetOnAxis(ap=icol, axis=0),
                                                             in_=ysb[0:R, :], in_offset=None, compute_op=ALU.add),
                     _toks([ysb, it_]) + prev_sc, [tk])
            cur_sc.append(tk)


def phase_E(kb, E, C):
    neghalf = C["neghalf"]
    gam = bc_row(kb, E["lnffn1"][0, :], "gamE")
    bet = bc_row(kb, E["lnffn1"][1, :], "betE")
    ys = Rot([kb.tile([128, 1024], F32, f"yE{i}") for i in range(4)])
    outs = Rot([kb.tile([128, 1024], F32, f"oE{i}") for i in range(4)])
    st12 = kb.tile([128, 12], F32, "st12E")
    mv = kb.tile([128, 2], F32, "mvE")
    rs1 = kb.tile([128, 2], F32, "rs1E")
    def loadE(i):
        y = ys.next()
        kb.dma("sp", y.ap, E["FACC"][i * 128:(i + 1) * 128, :], [], [y])
        return y

    st12s = [kb.tile([128, 12], F32, f"st12E{i}") for i in range(2)]
    mvs = [kb.tile([128, 2], F32, f"mvE{i}") for i in range(2)]
    rs1s = [kb.tile([128, 2], F32, f"rs1E{i}") for i in range(2)]

    def tileE(i, sl, y):
        o = outs.next()
        st12_, mv_, rs1_ = st12s[sl], mvs[sl], rs1s[sl]
        kb.v(lambda e: e.bn_stats(out=st12_[:, 0:6], in_=y[:, 0:512]), [y], [st12_])
        yield
        kb.v(lambda e: e.bn_stats(out=st12_[:, 6:12], in_=y[:, 512:1024]), [y], [st12_])
        yield
        kb.v(lambda e: e.bn_aggr(out=mv_.ap, in_=st12_.ap), [st12_], [mv_])
        kb.ts(rs1_[:, 0:1], mv_[:, 1:2], 1e-5, None, ALU.add, None, [mv_], [rs1_])
        yield
        kb.tt(rs1_[:, 1:2], rs1_[:, 0:1], neghalf[:, 0:1], ALU.pow, [rs1_, neghalf], [rs1_], eng="pool")
        yield
        kb.stt(rs1_[:, 0:1], mv_[:, 0:1], -1.0, rs1_[:, 1:2], ALU.mult, ALU.mult, [mv_, rs1_], [rs1_])
        yield
        kb.act(y.ap, y.ap, AF.Identity, [y, rs1_], [y], bias=rs1_[:, 0:1], scale=rs1_[:, 1:2])
        yield
        kb.tt(y.ap, y.ap, gam.ap, ALU.mult, [y, gam], [y], eng="pool")
        yield
        kb.tt(o.ap, y.ap, bet.ap, ALU.add, [y, bet], [o])
        kb.dma("sp", E["out"][i * 128:(i + 1) * 128, :], o.ap, [o], [])
        yield

    lds = [loadE(0), loadE(1)]
    for p in range(32):
        cur = lds
        if p + 1 < 32:
            lds = [loadE(2 * p + 2), loadE(2 * p + 3)]
        lockstep([tileE(2 * p, 0, cur[0]), tileE(2 * p + 1, 1, cur[1])])


def phase_A1(kb, E, C):
    b = kb.banks
    modT = C["modT"][1]
    ident_b, ones_b, ones_f, neghalf = C["ident_b"], C["ones_b"], C["ones_f"], C["neghalf"]
    l4 = kb.tile([1, 256], F32, "l4")
    kb.dma("sp", l4.ap, E["lam4"], [], [l4])
    lp = kb.tile([1, 128], F32, "lp")
    lsum = kb.tile([1, 4], F32, "lsum")
    kb.tt(lp[:, 0:64], l4[:, 0:64], l4[:, 64:128], ALU.mult, [l4], [lp])
    kb.tt(lp[:, 64:128], l4[:, 128:192], l4[:, 192:256], ALU.mult, [l4], [lp])
    kb.v(lambda e: e.reduce_sum(out=lsum[:, 0:1], in_=lp[:, 0:64], axis=mybir.AxisListType.X), [lp], [lsum])
    kb.v(lambda e: e.reduce_sum(out=lsum[:, 1:2], in_=lp[:, 64:128], axis=mybir.AxisListType.X), [lp], [lsum])
    kb.act(lsum[:, 2:4], lsum[:, 0:2], AF.Exp, [lsum], [lsum])
    kb.tt(lsum[:, 0:1], lsum[:, 3:4], lsum[:, 2:3], ALU.subtract, [lsum], [lsum])
    kb.ts(lsum[:, 1:2], lsum[:, 0:1], -LAM_INIT, None, ALU.add, None, [lsum], [lsum])
    kb.mm(b[0][:, 0:1], ones_f[0:1, :], lsum[:, 1:2], True, True, [ones_f, lsum], [b[0]])
    kb.cp(C["neglam"].ap, b[0][:, 0:1], [b[0]], [C["neglam"]])
    shc = kb.tile([128, 8, 2], BF16, "shc1")
    kb.cp(shc.ap, modT[:, 0:8, :], [modT], [shc])
    A1 = kb.tile([128, 8, 2], F32, "A1_1")
    kb.ts(A1.ap, modT[:, 8:16, :], 1.0, None, ALU.add, None, [modT], [A1])
    W = [kb.tile([128, 8, 3072], BF16, f"W1_{st}") for st in range(2)]
    bcol = kb.tile([128, 16, 2], F32, "bcol1")
    brow = [kb.tile([1, 1024], BF16, f"brow1_{st}") for st in range(2)]
    mark = kb.off
    wtmp = kb.tile([128, 8, 1024], F32, "wtmp1")
    Wun = kb.tile([128, 8, 1024], BF16, "Wun1")
    for third in range(3):
        cs_ = slice(third * 1024, (third + 1) * 1024)
        kb.dma("sp", wtmp.ap, E["win1"][:, :, cs_], [], [wtmp])
        for k in range(8):
            kb.cp(Wun[:, k, :], wtmp[:, k, :], [wtmp], [Wun], eng=("act" if k % 2 else "dve"))
        for st in range(2):
            for k in range(8):
                if k % 2:
                    kb.act(W[st][:, k, cs_], wtmp[:, k, :], AF.Identity, [wtmp, A1], [W[st]], scale=A1[:, k, st:st + 1])
                else:
                    kb.ts(W[st][:, k, cs_], wtmp[:, k, :], A1[:, k, st:st + 1], None, ALU.mult, None, [wtmp, A1], [W[st]])
        if third < 2:
            for m in range(8):
                mi = third * 8 + m
                for k in range(8):
                    kb.mm(b[0][:, mi * 2:mi * 2 + 2], Wun[:, k, m * 128:(m + 1) * 128], shc[:, k, :], k == 0, k == 7, [Wun, shc], [b[0]], sig=(k == 7))
        else:
            for st in range(2):
                for half in range(2):
                    for k in range(8):
                        kb.mm(b[1 + half][0:1, :], shc[:, k, st:st + 1], Wun[:, k, half * 512:(half + 1) * 512], k == 0, k == 7,
                              [Wun, shc], [b[1 + half]], sig=(k == 7))
                    kb.cp(brow[st][:, half * 512:(half + 1) * 512], b[1 + half][0:1, :], [b[1 + half]], [brow[st]])
    kb.cp(bcol.ap, b[0][:, 0:32].rearrange("p (a b) -> p a b", a=16), [b[0]], [bcol])
    kb.s.barrier()
    kb.off = mark
    gam = bc_row(kb, E["lnffn0"][0, :], "gamA1")
    bet = bc_row(kb, E["lnffn0"][1, :], "betA1")
    ys = Rot([kb.tile([128, 1024], F32, f"yA{i}") for i in range(3)])
    x2s = Rot([kb.tile([128, 1024], F32, f"x2_{i}") for i in range(2)])
    xbs = Rot([kb.tile([128, 1024], BF16, f"xbA1_{i}") for i in range(2)])
    st12 = kb.tile([128, 12], F32, "st12A")
    mv = kb.tile([128, 2], F32, "mvA")
    rs1 = kb.tile([128, 2], F32, "rs1A")
    xTs = Rot([kb.tile([128, 8, 512], BF16, f"xTA{i}") for i in range(2)])
    cosTs = Rot([kb.tile([128, 512], F32, f"cosT{i}") for i in range(2)])
    sinTs = Rot([kb.tile([128, 512], F32, f"sinT{i}") for i in range(2)])
    qbs = Rot([kb.tile([128, 512], BF16, f"qb{i}") for i in range(3)])
    tAs = Rot([kb.tile([128, 512], F32, f"tA1_{i}") for i in range(2)])
    tBs = Rot([kb.tile([128, 512], F32, f"tB1_{i}") for i in range(2)])
    outs = Rot([kb.tile([128, 512], BF16, f"qk{i}") for i in range(3)])
    vaugs = Rot([kb.tile([128, 8, 128], BF16, f"vaug1_{i}") for i in range(2)])
    psw_f = kb.tile([128, 128], F32, "psw_f")
    kb.dma("sp", psw_f.ap, E["psw"], [], [psw_f])
    psw = kb.tile([128, 128], BF16, "psw")
    kb.cp(psw.ap, psw_f.ap, [psw_f], [psw])
    V1v = E["V1"].rearrange("h p kt d -> p h kt d")

    def loadA(i):
        y = ys.next()
        kb.dma("sp", y.ap, E["FACC"][i * 128:(i + 1) * 128, :], [], [y])
        return y

    def stageL(gi):
        ntl = 4 if gi < 16 else 2
        xT = xTs.next()

        def one(tl):
            i = gi * 4 + tl
            if i == 0:
                ybox[0] = loadA(0)
            y = ybox[0]
            if i + 1 < 66:
                ybox[0] = loadA(i + 1)
            x2 = x2s.next()
            layer_norm_tile(kb, y, gam, bet, x2, st12, mv, rs1, neghalf)
            if i < 64:
                kb.dma("sp", E["X2"][i * 128:(i + 1) * 128, :], x2.ap, [x2], [])
            xb = xbs.next()
            kb.cp(xb.ap, x2.ap, [x2], [xb], eng="act")
            pb = kb.bank_bf(0)
            for k in range(8):
                kb.tr(pb[:, k * 128:(k + 1) * 128], xb[:, k * 128:(k + 1) * 128], ident_b.ap, [xb, ident_b], [b[0]], sig=(k == 7))
            kb.cp(xT[:, :, tl * 128:(tl + 1) * 128], pb.rearrange("p (a b) -> p a b", a=8), [b[0]], [xT], eng=("act" if tl % 2 else "dve"))
        return xT, [(lambda tl=tl: one(tl)) for tl in range(ntl)]

    def stageP(gi, xT, Lsteps):
        ntl = 4 if gi < 16 else 2
        Wd = ntl * 128
        st = 0 if gi < 16 else 1
        col0 = gi * 512
        cosT, sinT = cosTs.next(), sinTs.next()
        kb.dma("sp", cosT[:, 0:Wd], E["cos1"][:, col0:col0 + Wd], [], [cosT])
        kb.dma("sp", sinT[:, 0:Wd], E["sin1"][:, col0:col0 + Wd], [], [sinT])

        def finish(mi, qb_):
            br = b[5 + mi % 2]
            kb.mm(br[:, 0:Wd], psw.ap, qb_[:, 0:Wd], True, True, [psw, qb_], [br])
            tA_ = tAs.next()
            kb.tt(tA_[:, 0:Wd], qb_[:, 0:Wd], cosT[:, 0:Wd], ALU.mult, [qb_, cosT], [tA_], eng="pool")
            tB_ = tBs.next()
            kb.tt(tB_[:, 0:Wd], br[:, 0:Wd], sinT[:, 0:Wd], ALU.mult, [br, sinT], [tB_])
            o = outs.next()
            kb.tt(o[:, 0:Wd], tA_[:, 0:Wd], tB_[:, 0:Wd], ALU.add, [tA_, tB_], [o])
            dst_d = E["QT"] if mi < 8 else E["KT"]
            kb.dma("sp", dst_d[mi % 8][:, col0:col0 + Wd], o[:, 0:Wd], [o], [])

        pend = None
        pbanks = (b[1], b[2], b[7])
        for mi in range(16):
            bk = pbanks[mi % 3]
            for k in range(8):
                kb.mm(bk[:, 0:Wd], W[st][:, k, mi * 128:(mi + 1) * 128], xT[:, k, 0:Wd], k == 0, k == 7, [W[st], xT], [bk], sig=(k == 7))
            qb_ = qbs.next()
            kb.act(qb_[:, 0:Wd], bk[:, 0:Wd], AF.Identity, [bk, bcol], [qb_], bias=bcol[:, mi, st:st + 1])
            if pend is not None:
                finish(*pend)
            pend = (mi, qb_)
            if mi % 4 == 3 and Lsteps:
                Lsteps.pop(0)()
        finish(*pend)
        for tl in range(ntl):
            i = gi * 4 + tl
            ts_ = slice(tl * 128, (tl + 1) * 128)
            va = vaugs.next()
            for half in range(2):
                bk = b[3 + half]
                for k in range(8):
                    kb.mm(bk.ap, xT[:, k, ts_], W[st][:, k, 2048 + half * 512:2048 + (half + 1) * 512], k == 0, False, [xT, W[st]], [bk], sig=False)
                kb.mm(bk.ap, ones_b[0:1, 0:128], brow[st][:, half * 512:(half + 1) * 512], False, True, [ones_b, brow[st]], [bk])
                kb.cp(va[:, half * 4:(half + 1) * 4, :], bk.ap.rearrange("p (a b) -> p a b", a=4), [bk], [va], eng=("act" if half else "dve"))
            kb.dma("sp", V1v[:, :, i, :], va.ap, [va], [])
        while Lsteps:
            Lsteps.pop(0)()

    ybox = [None]
    xT_cur, steps = stageL(0)
    for f_ in steps:
        f_()
    for gi in range(17):
        if gi + 1 < 17:
            xT_next, steps = stageL(gi + 1)
        else:
            xT_next, steps = None, []
        stageP(gi, xT_cur, steps)
        xT_cur = xT_next


PHASES = ["mod", "A0", "B0", "C0", "D0", "A1", "B1", "C1", "D1", "E"]


def build(dbg=False, stop_after=None, only=None):
    kb = KB(dbg, stop_after)
    E = {}
    inp = kb.inp
    E["x"] = inp("x", [NLAT, D])
    E["ctx"] = inp("ctx", [NCTX, D])
    E["ccT"] = inp("ccT", [128, 8, 2])
    E["ident"] = inp("ident", [128, 128])
    E["mconst"] = inp("mconst", [128, 4])
    E["bdiag"] = inp("bdiag", [128, 128])
    E["drow"] = inp("drow", [16, 128])
    E["cs0"] = inp("cs0", [64, NALL])
    E["cos1"] = inp("cos1", [128, NALL])
    E["sin1"] = inp("sin1", [128, NALL])
    for l in range(2):
        E[f"wmod{l}"] = inp(f"wmod{l}", [12, 128, 8, 512])
        E[f"bmod{l}"] = inp(f"bmod{l}", [1, 6144])
        E[f"bmodT{l}"] = inp(f"bmodT{l}", [128, 48])
        E[f"wout{l}"] = inp(f"wout{l}", [128, 8, 1024])
        E[f"router{l}"] = inp(f"router{l}", [128, 8, 16])
        E[f"lnmix{l}"] = inp(f"lnmix{l}", [2, 1024])
        E[f"lnffn{l}"] = inp(f"lnffn{l}", [2, 1024])
        for nm in ("gate", "up", "down"):
            E[f"w{nm}{l}"] = inp(f"w{nm}{l}", [16, 1024, 1024])
    E["win0"] = inp("win0", [128, 8, 1472])
    E["wuqx"] = inp("wuqx", [128, 2, 1024])
    E["wukx"] = inp("wukx", [128, 1024])
    E["wuv"] = inp("wuv", [128, 512])
    E["qnorm"] = inp("qnorm", [128, 2])
    E["kvnorm"] = inp("kvnorm", [128, 1])
    E["gln"] = inp("gln", [128, 8])
    E["wsT"] = inp("wsT", [128, 512])
    E["gbs"] = inp("gbs", [1, 512])
    E["win1"] = inp("win1", [128, 8, 3072])
    E["lam4"] = inp("lam4", [1, 256])
    E["subln"] = inp("subln", [128, 1])
    E["psw"] = inp("psw", [128, 128])
    sc = kb.scratch
    E["MOD"] = sc("MOD", [2, 2, 6144], F32)
    E["AT0"] = sc("AT0", [512, NALL], BF16)
    E["QT"] = sc("QT", [8, 128, NALL], BF16)
    E["KT"] = sc("KT", [8, 128, NALL], BF16)
    E["V0"] = sc("V0", [8, 128, 66, 128], BF16)
    E["V1"] = sc("V1", [8, 128, 66, 128], BF16)
    E["OT"] = sc("OT", [1024, NALL], BF16)
    E["FACC"] = sc("FACC", [NPAD, D], F32)
    E["HF"] = sc("HF", [NPAD, D], BF16)
    E["X2"] = sc("X2", [NLAT, D], F32)
    E["AFF"] = sc("AFF", [NPAD, 16], F32)
    E["KEYS"] = sc("KEYS", [128, 64], F32)
    E["out"] = kb.nc.dram_tensor("out", [NLAT, D], F32, kind="ExternalOutput").ap()
    C = setup_consts(kb, E)
    fns = {
        "mod": lambda: phase_mod(kb, E, C),
        "A0": lambda: phase_A0(kb, E, C),
        "B0": lambda: phase_attn(kb, E, C, 0),
        "C0": lambda: phase_C(kb, E, C, 0),
        "D0": lambda: phase_D(kb, E, C, 0),
        "A1": lambda: phase_A1(kb, E, C),
        "B1": lambda: phase_attn(kb, E, C, 1),
        "C1": lambda: phase_C(kb, E, C, 1),
        "D1": lambda: phase_D(kb, E, C, 1),
        "E": lambda: phase_E(kb, E, C),
    }
    for ph in PHASES:
        if only is None or ph in only:
            fns[ph]()
            kb.phase()
        if ph == stop_after:
            break
    kb.s.barrier()
    return kb


def _rope_tables():
    t = np.arange(NLAT)
    row = (t // 64).astype(np.float32)
    col = (t % 64).astype(np.float32)

    def ang(dim):
        nf = dim // 4
        inv = (np.float32(10000.0) ** (-np.arange(nf, dtype=np.float32) / np.float32(nf))).astype(np.float32)
        return np.concatenate([row[:, None] * inv, col[:, None] * inv], -1).astype(np.float32)

    a0 = ang(32)
    c0, s0 = np.cos(a0).T, np.sin(a0).T
    cs0 = np.zeros((64, NALL), np.float32)
    cs0[0:32, NLAT:] = 1.0
    cs0[0:16, :NLAT] = c0
    cs0[16:32, :NLAT] = c0
    cs0[32:48, :NLAT] = -s0
    cs0[48:64, :NLAT] = s0
    a1 = ang(64)
    c1, s1 = np.cos(a1).T, np.sin(a1).T
    cos1 = np.ones((128, NALL), np.float32)
    sin1 = np.zeros((128, NALL), np.float32)
    for blk in range(4):
        cos1[blk * 32:(blk + 1) * 32, :NLAT] = c1
        sin1[blk * 32:(blk + 1) * 32, :NLAT] = (-s1 if blk % 2 == 0 else s1)
    return cs0, cos1, sin1


def _pk(w):
    K = w.shape[0] // 128
    return np.ascontiguousarray(w.reshape(K, 128, -1).transpose(1, 0, 2))


def prep_shared(I):
    f = lambda a: np.ascontiguousarray(np.asarray(a, dtype=np.float32))
    S = {}
    S["ident"] = np.eye(128, dtype=np.float32)
    p = np.arange(128)
    mc = np.zeros((128, 4), np.float32)
    mc[:, 0] = 128 * (p % 8)
    mc[:, 1] = NALL + p
    S["mconst"] = mc
    S["bdiag"] = (p[:, None] // 8 == p[None, :] // 8).astype(np.float32)
    S["drow"] = np.tile((NALL + np.arange(128, dtype=np.float32))[None, :], (16, 1))
    S["cs0"], S["cos1"], S["sin1"] = _rope_tables()
    for l in range(2):
        wm = f(I[f"w_mod_{l}"])
        S[f"wmod{l}"] = np.ascontiguousarray(wm.reshape(8, 128, 12, 512).transpose(2, 1, 0, 3))
        bm = f(I[f"b_mod_{l}"])
        S[f"bmod{l}"] = bm.reshape(1, 6144)
        S[f"bmodT{l}"] = np.ascontiguousarray(bm.reshape(48, 128).T)
        S[f"wout{l}"] = _pk(f(I[f"w_out_{l}"]))
        S[f"router{l}"] = _pk(f(I[f"router_{l}"]))
        S[f"lnmix{l}"] = np.stack([f(I[f"ln_mix_g_{l}"]), f(I[f"ln_mix_b_{l}"])])
        S[f"lnffn{l}"] = np.stack([f(I[f"ln_ffn_g_{l}"]), f(I[f"ln_ffn_b_{l}"])])
        for nm in ("gate", "up", "down"):
            S[f"w{nm}{l}"] = f(I[f"w_{nm}_{l}"])
    w = f(I["w_in_0"])
    kr = w[:, 1408:1440]
    krE, krO = kr[:, 0::2], kr[:, 1::2]
    S["win0"] = _pk(np.concatenate([w[:, 0:512], w[:, 1024:1280], w[:, 1280:1408], krE, krO, krO, krE, w[:, 512:1024]], 1))
    wuq = f(I["mla_w_uq_0"])
    blocks = []
    for h in range(8):
        nope = wuq[:, h * 96:h * 96 + 64]
        rp = wuq[:, h * 96 + 64:h * 96 + 96]
        rE, rO = rp[:, 0::2], rp[:, 1::2]
        blocks.append(np.concatenate([rE, rO, rO, rE, nope], 1))
    S["wuqx"] = _pk(np.concatenate(blocks, 1))
    wukv = f(I["mla_w_ukv_0"])
    S["wukx"] = np.ascontiguousarray(np.concatenate(
        [np.concatenate([np.zeros((128, 64), np.float32), wukv[:, h * 128:h * 128 + 64]], 1) for h in range(8)], 1))
    S["wuv"] = np.ascontiguousarray(np.concatenate([wukv[:, h * 128 + 64:h * 128 + 128] for h in range(8)], 1))
    S["qnorm"] = np.ascontiguousarray(f(I["mla_q_norm_0"]).reshape(2, 128).T)
    S["kvnorm"] = f(I["mla_kv_norm_0"]).reshape(128, 1)
    S["gln"] = np.ascontiguousarray(np.concatenate([f(I["gmlp_ln_g_0"]).reshape(4, 128).T, f(I["gmlp_ln_b_0"]).reshape(4, 128).T], 1))
    S["wsT"] = np.ascontiguousarray(f(I["gmlp_ws_0"]).transpose(2, 0, 1).reshape(128, 512))
    S["gbs"] = f(I["gmlp_bs_0"]).reshape(1, 512)
    w1 = f(I["w_in_1"])
    cols = []
    for part in range(2):
        for j in range(16):
            blk = w1[:, part * 1024 + j * 64:part * 1024 + (j + 1) * 64]
            cols += [blk[:, 0::2], blk[:, 1::2]]
    cols.append(w1[:, 2048:3072])
    S["win1"] = _pk(np.concatenate(cols, 1))
    S["lam4"] = np.concatenate([f(I["lambda_q1_1"]), f(I["lambda_k1_1"]), f(I["lambda_q2_1"]), f(I["lambda_k2_1"])]).reshape(1, 256)
    S["subln"] = f(I["subln_g_1"]).reshape(128, 1)
    S["psw"] = np.eye(128, dtype=np.float32)[:, np.arange(128) ^ 32]
    return S


def prep_core(I, S, bidx):
    m = dict(S)
    m["x"] = np.ascontiguousarray(np.asarray(I["x"][bidx], dtype=np.float32))
    m["ctx"] = np.ascontiguousarray(np.asarray(I["ctx"][bidx], dtype=np.float32))
    cc = np.stack([np.asarray(I["c"][bidx], np.float32), np.asarray(I["c_ctx"], np.float32)], -1)
    m["ccT"] = np.ascontiguousarray(cc.reshape(8, 128, 2).transpose(1, 0, 2))
    return m


_KB_CACHE = {}


def kernel(**inputs):
    if "kb" not in _KB_CACHE:
        _KB_CACHE["kb"] = build()
    kb = _KB_CACHE["kb"]
    S = prep_shared(inputs)
    in_maps = [prep_core(inputs, S, bidx) for bidx in range(8)]
    res = run_bass_kernel_spmd(kb.nc, in_maps, core_ids=list(range(8)))
    return np.stack([np.asarray(r["out"], dtype=np.float32) for r in res.results], 0)
```

```python
import math
import numpy as np
import concourse.bass as bass
import concourse.mybir as mybir
from concourse.bass_utils import run_bass_kernel_spmd

F32 = mybir.dt.float32
BF16 = mybir.dt.bfloat16
I32 = mybir.dt.int32
U32 = mybir.dt.uint32
ALU = mybir.AluOpType
AF = mybir.ActivationFunctionType

D = 1024
NLAT = 8192
NCTX = 256
NALL = NLAT + NCTX
NPAD = NALL + 128
ALPHA = 4 ** 0.25
LAM_INIT = 0.8 - 0.6 * math.exp(-0.3)
CAP = 192
ECAP = 1024
CCAP = 32
AW = 51500


class Tok:
    __slots__ = ("w", "r", "name")

    def __init__(self, name=""):
        self.w = None
        self.r = {}
        self.name = name


class Tile:
    def __init__(self, ap, name=""):
        self.ap = ap
        self.tok = Tok(name)

    def __getitem__(self, k):
        return self.ap[k]


def _toks(xs):
    return [x.tok if isinstance(x, Tile) else x for x in xs]


class Sched:
    EPOCH = 24000
    NDMA = 56
    NSP = 36

    def __init__(self, nc):
        self.nc = nc
        self.eng = {"pe": nc.tensor, "act": nc.scalar, "dve": nc.vector, "pool": nc.gpsimd, "sp": nc.sync}
        self.cnt = {e: 0 for e in self.eng}
        self.sems = {e: [] for e in self.eng}
        self.known = {e: {} for e in self.eng}
        self.pending = {e: [] for e in self.eng}
        self.dma_sems = [nc.alloc_semaphore(f"dq{i}") for i in range(self.NDMA)]
        self.dma_val = [0] * self.NDMA
        self.dma_rng = {"sp": (0, self.NSP), "pool": (self.NSP, self.NDMA)}
        self.dma_nxt = {"sp": 0, "pool": self.NSP}

    def _sem(self, e, seq):
        k = (seq - 1) // self.EPOCH
        while len(self.sems[e]) <= k:
            self.sems[e].append(self.nc.alloc_semaphore(f"c_{e}_{len(self.sems[e])}"))
        return self.sems[e][k], (seq - 1) % self.EPOCH + 1

    def _wait(self, e, ev):
        if ev is None:
            return
        kind, src, val = ev
        if kind == "eng":
            if src == e and e == "pe":
                return
            key = ("eng", src)
            if self.known[e].get(key, 0) >= val:
                return
            if src == e and val <= self.cnt[e] - 2:
                return
            sem, v = self._sem(src, val)
            self.eng[e].wait_ge(sem, v)
            self.known[e][key] = val
        else:
            key = ("dma", src)
            if self.known[e].get(key, 0) >= val:
                return
            self.eng[e].wait_ge(self.dma_sems[src], val)
            self.known[e][key] = val

    def _deps(self, e, reads, writes):
        for t in reads:
            self._wait(e, t.w)
        for t in writes:
            self._wait(e, t.w)
            for ev in list(t.r.values()):
                self._wait(e, ev)

    def op(self, e, fn, reads=(), writes=(), sig=True):
        reads = _toks(reads)
        writes = _toks(writes)
        self._deps(e, reads, writes)
        ins = fn(self.eng[e])
        if not sig:
            self.pending[e].append((reads, writes))
            return ins
        self.cnt[e] += 1
        seq = self.cnt[e]
        sem, v = self._sem(e, seq)
        ins.then_inc(sem, 1)
        me = ("eng", e, seq)
        groups = self.pending[e] + [(reads, writes)]
        self.pending[e] = []
        for rs, ws in groups:
            for t in rs:
                t.r[("eng", e)] = me
            for t in ws:
                t.w = me
                t.r = {}
        return ins

    def dma(self, q, fn, reads=(), writes=()):
        reads = _toks(reads)
        writes = _toks(writes)
        self._deps(q, reads, writes)
        lo_, hi_ = self.dma_rng[q]
        k = self.dma_nxt[q]
        self.dma_nxt[q] = lo_ + (k + 1 - lo_) % (hi_ - lo_)
        if self.dma_val[k] > 0:
            self._wait(q, ("dma", k, self.dma_val[k]))
        ins = fn(self.eng[q])
        self.dma_val[k] += 16
        ins.then_inc(self.dma_sems[k], 16)
        me = ("dma", k, self.dma_val[k])
        for t in reads:
            t.r[("dma", k)] = me
        for t in writes:
            t.w = me
            t.r = {}
        return ins

    def barrier(self, engines=None):
        engines = engines or list(self.eng)
        for e in self.eng:
            assert not self.pending[e]
        for e in engines:
            for f in self.eng:
                if f != e and self.cnt[f] > 0:
                    self._wait(e, ("eng", f, self.cnt[f]))
            for k in range(self.NDMA):
                if self.dma_val[k] > 0:
                    self._wait(e, ("dma", k, self.dma_val[k]))


def _dsize(dt):
    return 2 if dt == BF16 else 4


class KB:
    def __init__(self, dbg=False, stop_after=None):
        self.dbg = dbg
        self.stop_after = stop_after
        nc = self.nc = bass.Bass("TRN2", target_bir_lowering=False)
        self.s = Sched(nc)
        self.arena = nc.alloc_sbuf_tensor("arena", [128, AW], F32)
        self.off = 0
        self.persist = 0
        self.psum = nc.alloc_psum_tensor("psum_all", [128, 4096], F32)
        self.banks = [Tile(self.psum[:, i * 512:(i + 1) * 512], f"bank{i}") for i in range(8)]
        self.ext = {}
        self.outs = {}

    def tile(self, shape, dt=F32, name=""):
        P = shape[0]
        n = 1
        for d in shape[1:]:
            n *= d
        words = (n * _dsize(dt) + 3) // 4
        words = (words + 7) // 8 * 8
        assert self.off + words <= AW, f"arena overflow {name} {self.off}+{words}"
        ap = self.arena[0:P, self.off:self.off + words]
        self.off += words
        if dt != F32:
            ap = ap.bitcast(dt)
        ap = ap[:, 0:n]
        if len(shape) == 3:
            ap = ap.rearrange("p (a b) -> p a b", a=shape[1])
        elif len(shape) == 4:
            ap = ap.rearrange("p (a b c) -> p a b c", a=shape[1], b=shape[2])
        return Tile(ap, name)

    def phase(self):
        self.s.barrier()
        self.off = self.persist
        for b in self.banks:
            b.tok = Tok(b.tok.name)

    def keep(self):
        self.persist = self.off

    def bank_bf(self, i):
        return self.banks[i].ap.bitcast(BF16)

    def bank2(self, i):
        return self.psum[:, i * 512:(i + 2) * 512]

    def inp(self, name, shape, dt=F32):
        t = self.nc.dram_tensor(name, list(shape), dt, kind="ExternalInput")
        self.ext[name] = (tuple(shape), dt)
        return t.ap()

    def scratch(self, name, shape, dt):
        if self.dbg:
            t = self.nc.dram_tensor(name, list(shape), dt, kind="ExternalOutput")
            self.outs[name] = (tuple(shape), dt)
        else:
            t = self.nc.dram_tensor(name, list(shape), dt)
        return t.ap()

    def dma(self, q, out, in_, reads=(), writes=(), **kw):
        return self.s.dma(q, lambda e: e.dma_start(out=out, in_=in_, **kw), reads, writes)

    def mm(self, out, lhsT, rhs, start, stop, reads, writes, sig=True):
        return self.s.op("pe", lambda e: e.matmul(out, lhsT=lhsT, rhs=rhs, start=start, stop=stop), reads, writes, sig)

    def tr(self, out, in_, ident, reads, writes, sig=True):
        return self.s.op("pe", lambda e: e.transpose(out=out, in_=in_, identity=ident), reads, writes, sig)

    def act(self, out, in_, func, reads, writes, bias=0.0, scale=1.0, accum=None):
        if accum is None:
            return self.s.op("act", lambda e: e.activation(out=out, in_=in_, func=func, bias=bias, scale=scale), reads, writes)
        return self.s.op("act", lambda e: e.activation(out=out, in_=in_, func=func, bias=bias, scale=scale, accum_out=accum), reads, writes)

    def v(self, fn, reads, writes):
        return self.s.op("dve", fn, reads, writes)

    def g(self, fn, reads, writes):
        return self.s.op("pool", fn, reads, writes)

    def tt(self, out, a, b, op, reads, writes, eng="dve"):
        return self.s.op(eng, lambda e: e.tensor_tensor(out=out, in0=a, in1=b, op=op), reads, writes)

    def ts(self, out, a, s1, s2, op0, op1, reads, writes, eng="dve", accum=None):
        if accum is not None:
            return self.s.op(eng, lambda e: e.tensor_scalar(out=out, in0=a, scalar1=s1, scalar2=s2, op0=op0, op1=op1, accum_out=accum), reads, writes)
        if s2 is None:
            return self.s.op(eng, lambda e: e.tensor_scalar(out=out, in0=a, scalar1=s1, scalar2=None, op0=op0), reads, writes)
        return self.s.op(eng, lambda e: e.tensor_scalar(out=out, in0=a, scalar1=s1, scalar2=s2, op0=op0, op1=op1), reads, writes)

    def stt(self, out, a, sc, b, op0, op1, reads, writes, eng="dve"):
        return self.s.op(eng, lambda e: e.scalar_tensor_tensor(out=out, in0=a, scalar=sc, in1=b, op0=op0, op1=op1), reads, writes)

    def rsq(self, out, in_, scale, eps, tmp, reads, writes):
        self.act(tmp, in_, AF.Sqrt, reads, writes, bias=eps, scale=scale)
        self.s.op("dve", lambda e: e.reciprocal(out=out, in_=tmp), _toks(writes), _toks(writes))

    def cp(self, out, in_, reads, writes, eng="dve"):
        if eng == "act":
            return self.s.op("act", lambda e: e.copy(out=out, in_=in_), reads, writes)
        return self.s.op(eng, lambda e: e.tensor_copy(out=out, in_=in_), reads, writes)

    def rsqrt(self, out, in_, scale, eps, reads, writes, tmp, neghalf):
        self.ts(tmp.ap if isinstance(tmp, Tile) else tmp, in_, scale, eps, ALU.mult, ALU.add, reads, [tmp])
        self.tt(out, tmp.ap if isinstance(tmp, Tile) else tmp, neghalf, ALU.pow, [tmp], writes, eng="pool")


class Rot:
    def __init__(self, tiles):
        self.t = tiles
        self.i = -1

    def next(self):
        self.i = (self.i + 1) % len(self.t)
        return self.t[self.i]


MCH0 = [(0, 128), (128, 256), (256, 384), (384, 512), (512, 640), (640, 768), (768, 896), (896, 960)]


def rows_of(E, L, i):
    if L == 0:
        if i < 64:
            return E["x"][i * 128:(i + 1) * 128, :]
        return E["ctx"][(i - 64) * 128:(i - 63) * 128, :]
    return E["X2"][i * 128:(i + 1) * 128, :]


def setup_consts(kb, E):
    C = {}
    C["ident_f"] = kb.tile([128, 128], F32, "ident_f")
    C["ident_b"] = kb.tile([128, 128], BF16, "ident_b")
    C["ones_b"] = kb.tile([128, 128], BF16, "ones_b")
    C["ones_f"] = kb.tile([128, 128], F32, "ones_f")
    C["neghalf"] = kb.tile([128, 512], F32, "neghalf")
    C["mc"] = kb.tile([128, 4], F32, "mc")
    C["bd"] = kb.tile([128, 128], F32, "bd")
    C["affT"] = kb.tile([128, 1024], F32, "affT")
    C["affc"] = kb.tile([16, 256], F32, "affc")
    C["modT"] = [kb.tile([128, 48, 2], F32, f"modT{l}") for l in range(2)]
    C["neglam"] = kb.tile([128, 1], F32, "neglam")
    kb.dma("sp", C["ident_f"].ap, E["ident"], [], [C["ident_f"]])
    kb.dma("sp", C["mc"].ap, E["mconst"], [], [C["mc"]])
    kb.dma("sp", C["bd"].ap, E["bdiag"], [], [C["bd"]])
    kb.cp(C["ident_b"].ap, C["ident_f"].ap, [C["ident_f"]], [C["ident_b"]])
    kb.v(lambda e: e.memset(C["ones_b"].ap, 1.0), [], [C["ones_b"]])
    kb.v(lambda e: e.memset(C["ones_f"].ap, 1.0), [], [C["ones_f"]])
    kb.v(lambda e: e.memset(C["neghalf"].ap, -0.5), [], [C["neghalf"]])
    kb.keep()
    return C


def phase_mod(kb, E, C):
    b = kb.banks
    cc = kb.tile([128, 8, 2], F32, "cc")
    sc = kb.tile([128, 8, 2], F32, "sc")
    kb.dma("sp", cc.ap, E["ccT"], [], [cc])
    kb.act(sc.ap, cc.ap, AF.Silu, [cc], [sc])
    wts = Rot([kb.tile([128, 8, 512], F32, f"wmod{i}") for i in range(2)])
    for l in range(2):
        brow = kb.tile([2, 6144], F32, "brow")
        bT = kb.tile([128, 48], F32, "bT")
        modsb = kb.tile([2, 6144], F32, "modsb")
        kb.dma("sp", brow[0:1, :], E[f"bmod{l}"], [], [brow])
        kb.dma("sp", brow[1:2, :], E[f"bmod{l}"], [], [brow])
        kb.dma("sp", bT.ap, E[f"bmodT{l}"], [], [bT])
        for j in range(12):
            wt = wts.next()
            kb.dma("sp", wt.ap, E[f"wmod{l}"][j], [], [wt])
            bk = b[j % 2]
            for k in range(8):
                kb.mm(bk[0:2, :], sc[:, k, :], wt[:, k, :], k == 0, k == 7, [sc, wt], [bk], sig=(k == 7))
            kb.tt(modsb[:, j * 512:(j + 1) * 512], bk[0:2, :], brow[:, j * 512:(j + 1) * 512], ALU.add, [bk, brow], [modsb])
            for q in range(4):
                c48 = j * 4 + q
                if c48 >= 16:
                    continue
                bk2 = b[2 + c48 % 2]
                for k in range(8):
                    kb.mm(bk2[:, 0:2], wt[:, k, q * 128:(q + 1) * 128], sc[:, k, :], k == 0, k == 7, [sc, wt], [bk2], sig=(k == 7))
                kb.ts(C["modT"][l][:, c48, :], bk2[:, 0:2], bT[:, c48:c48 + 1], None, ALU.add, None, [bk2, bT], [C["modT"][l]])
        kb.dma("sp", E["MOD"][l], modsb.ap, [modsb], [])


def bc_load(kb, E, l, st, ch, name, plus1=False):
    t = kb.tile([128, 1024], F32, name)
    src = E["MOD"][l][st, ch * 1024:(ch + 1) * 1024].partition_broadcast(128)
    kb.dma("sp", t.ap, src, [], [t])
    if plus1:
        kb.ts(t.ap, t.ap, 1.0, None, ALU.add, None, [t], [t], eng="pool")
    return t


def bc_row(kb, src_row, name):
    t = kb.tile([128, 1024], F32, name)
    kb.dma("sp", t.ap, src_row.partition_broadcast(128), [], [t])
    return t


def phase_A0(kb, E, C):
    b = kb.banks
    modT = C["modT"][0]
    ident_b, ones_b, ones_f, neghalf = C["ident_b"], C["ones_b"], C["ones_f"], C["neghalf"]
    W = [kb.tile([128, 8, 1472], BF16, f"W{st}") for st in range(2)]
    bcol = kb.tile([128, 8, 2], F32, "bcol")
    brow = [kb.tile([1, 512], BF16, f"brow{st}") for st in range(2)]
    Wq = kb.tile([128, 2, 1024], BF16, "Wq")
    Wk = kb.tile([128, 1024], BF16, "Wk")
    Wv = kb.tile([128, 512], BF16, "Wv")
    wsT = kb.tile([128, 512], BF16, "wsT")
    gln = kb.tile([128, 8], F32, "gln")
    Rt = kb.tile([128, 512], F32, "Rt")
    mark = kb.off
    wtmp = kb.tile([128, 8, 1472], F32, "wtmp")
    kb.dma("sp", wtmp.ap, E["win0"], [], [wtmp])
    Wun = kb.tile([128, 8, 1472], BF16, "Wun")
    for k in range(8):
        kb.cp(Wun[:, k, :], wtmp[:, k, :], [wtmp], [Wun], eng=("act" if k % 2 else "dve"))
    shc = kb.tile([128, 8, 2], BF16, "shc")
    kb.cp(shc.ap, modT[:, 0:8, :], [modT], [shc])
    A1 = kb.tile([128, 8, 2], F32, "A1")
    kb.ts(A1.ap, modT[:, 8:16, :], 1.0, None, ALU.add, None, [modT], [A1])
    for st in range(2):
        for k in range(8):
            if k % 2:
                kb.act(W[st][:, k, :], wtmp[:, k, :], AF.Identity, [wtmp, A1], [W[st]], scale=A1[:, k, st:st + 1])
            else:
                kb.ts(W[st][:, k, :], wtmp[:, k, :], A1[:, k, st:st + 1], None, ALU.mult, None, [wtmp, A1], [W[st]])
    for mi, (a, bb) in enumerate(MCH0):
        for k in range(8):
            kb.mm(b[0][0:bb - a, mi * 2:mi * 2 + 2], Wun[:, k, a:bb], shc[:, k, :], k == 0, k == 7, [Wun, shc], [b[0]], sig=(k == 7))
    kb.cp(bcol[:, 0:7, :], b[0][:, 0:14].rearrange("p (a b) -> p a b", a=7), [b[0]], [bcol])
    kb.cp(bcol[0:64, 7, :], b[0][0:64, 14:16], [b[0]], [bcol])
    for st in range(2):
        for k in range(8):
            kb.mm(b[1][0:1, :], shc[:, k, st:st + 1], Wun[:, k, 960:1472], k == 0, k == 7, [Wun, shc], [b[1]], sig=(k == 7))
        kb.cp(brow[st].ap, b[1][0:1, :], [b[1]], [brow[st]])
    qn = kb.tile([128, 2], F32, "qn")
    kvn = kb.tile([128, 1], F32, "kvn")
    kb.dma("sp", qn.ap, E["qnorm"], [], [qn])
    kb.dma("sp", kvn.ap, E["kvnorm"], [], [kvn])
    wq_f = kb.tile([128, 2, 1024], F32, "wq_f")
    kb.dma("sp", wq_f.ap, E["wuqx"], [], [wq_f])
    for r in range(2):
        kb.ts(Wq[:, r, :], wq_f[:, r, :], qn[:, r:r + 1], None, ALU.mult, None, [wq_f, qn], [Wq])
    wk_f = kb.tile([128, 1024], F32, "wk_f")
    kb.dma("sp", wk_f.ap, E["wukx"], [], [wk_f])
    kb.ts(Wk.ap, wk_f.ap, kvn[:, 0:1], None, ALU.mult, None, [wk_f, kvn], [Wk])
    wv_f = kb.tile([128, 512], F32, "wv_f")
    kb.dma("sp", wv_f.ap, E["wuv"], [], [wv_f])
    kb.ts(Wv.ap, wv_f.ap, kvn[:, 0:1], None, ALU.mult, None, [wv_f, kvn], [Wv])
    wsT_f = kb.tile([128, 512], F32, "wsT_f")
    kb.dma("sp", wsT_f.ap, E["wsT"], [], [wsT_f])
    kb.cp(wsT.ap, wsT_f.ap, [wsT_f], [wsT])
    kb.dma("sp", gln.ap, E["gln"], [], [gln])
    bs_bc = kb.tile([128, 512], F32, "bs_bc")
    kb.dma("sp", bs_bc.ap, E["gbs"][0, :].partition_broadcast(128), [], [bs_bc])
    kb.mm(b[2].ap, ones_f.ap, wsT_f.ap, True, True, [ones_f, wsT_f], [b[2]])
    for g in range(4):
        gs = slice(g * 128, (g + 1) * 128)
        kb.stt(Rt[:, gs], b[2][:, gs], gln[:, 4 + g:5 + g], bs_bc[:, gs], ALU.mult, ALU.add, [b[2], gln, bs_bc], [Rt])
    kb.s.barrier()
    kb.off = mark
    xTs = Rot([kb.tile([128, 8, 512], BF16, f"xT{i}") for i in range(2)])
    xbs = Rot([kb.tile([128, 1024], BF16, f"xb{i}") for i in range(8)])
    guT = kb.tile([128, 4, 512], BF16, "guT")
    tq = kb.tile([128, 2, 512], F32, "tq")
    tkv = kb.tile([128, 512], F32, "tkv")
    sq = kb.tile([128, 3, 512], BF16, "sq")
    krx = kb.tile([64, 512], F32, "krx")
    css = Rot([kb.tile([64, 512], F32, f"cs{i}") for i in range(2)])
    tmpA = kb.tile([128, 512], F32, "tmpA")
    rstd_q = kb.tile([128, 512], F32, "rstd_q")
    rstd_kv = kb.tile([128, 512], F32, "rstd_kv")
    cqn = kb.tile([128, 2, 512], BF16, "cqn")
    ckvn = kb.tile([128, 512], BF16, "ckvn")
    t1 = kb.tile([64, 512], F32, "t1")
    t2 = kb.tile([32, 512], F32, "t2")
    krr = kb.tile([32, 512], BF16, "krr")
    kts = Rot([kb.tile([128, 512], BF16, f"kt{i}") for i in range(2)])
    qts = Rot([kb.tile([128, 512], BF16, f"qt{i}") for i in range(2)])
    vaugs = Rot([kb.tile([128, 8, 128], BF16, f"vaug{i}") for i in range(2)])
    for t in kts.t + qts.t:
        kb.v(lambda e: e.memset(t.ap, 0.0), [], [t])
    for t in vaugs.t:
        kb.v(lambda e: e.memset(t.ap, 1.0), [], [t])
    gv = kb.tile([128, 512], F32, "gv")
    st6 = kb.tile([128, 6], F32, "st6")
    mv = kb.tile([128, 2], F32, "mv")
    rs1 = kb.tile([128, 2], F32, "rs1")
    vhat = kb.tile([128, 512], BF16, "vhat")
    tmpg = kb.tile([128, 512], F32, "tmpg")
    aTs = Rot([kb.tile([128, 4, 128], BF16, f"aT{i}") for i in range(2)])
    AT0v = E["AT0"].rearrange("(g c) t -> c g t", c=128)
    V0v = E["V0"].rearrange("h p kt d -> p h kt d")

    def loadX(gi_):
        res = []
        for tl_ in range(4 if gi_ < 16 else 2):
            xb_ = xbs.next()
            kb.dma("pool", xb_.ap, rows_of(E, 0, gi_ * 4 + tl_), [], [xb_])
            res.append(xb_)
        return res

    def stageL(gi):
        ntl = 4 if gi < 16 else 2
        Wd = ntl * 128
        st = 0 if gi < 16 else 1
        col0 = gi * 512
        xT = xTs.next()
        if gi == 0:
            xbox[0] = loadX(0)
        xb_cur = xbox[0]
        if gi + 1 < 17:
            xbox[0] = loadX(gi + 1)
        for tl in range(ntl):
            i = gi * 4 + tl
            xb = xb_cur[tl]
            pb = kb.bank_bf(0)
            for k in range(8):
                kb.tr(pb[:, k * 128:(k + 1) * 128], xb[:, k * 128:(k + 1) * 128], ident_b.ap, [xb, ident_b], [b[0]], sig=(k == 7))
            kb.cp(xT[:, :, tl * 128:(tl + 1) * 128], pb.rearrange("p (a b) -> p a b", a=8), [b[0]], [xT], eng=("act" if tl % 2 else "dve"))
        return xT

    def stageP(gi, xT):
        ntl = 4 if gi < 16 else 2
        Wd = ntl * 128
        st = 0 if gi < 16 else 1
        col0 = gi * 512
        cs = css.next()
        kb.dma("sp", cs[:, 0:Wd], E["cs0"][:, col0:col0 + Wd], [], [cs])
        for mi, (a, bb) in enumerate(MCH0):
            M_ = bb - a
            bk = b[1 + mi % 2]
            for k in range(8):
                kb.mm(bk[0:M_, 0:Wd], W[st][:, k, a:bb], xT[:, k, 0:Wd], k == 0, k == 7, [W[st], xT], [bk], sig=(k == 7))
            bias = bcol[0:M_, mi, st:st + 1]
            if mi < 4:
                kb.act(guT[:, mi, 0:Wd], bk[:, 0:Wd], AF.Gelu, [bk, bcol], [guT], bias=bias)
            elif mi < 6:
                r = mi - 4
                kb.act(tq[:, r, 0:Wd], bk[:, 0:Wd], AF.Identity, [bk, bcol], [tq], bias=bias)
                kb.act(sq[:, r, 0:Wd], bk[:, 0:Wd], AF.Square, [bk, bcol], [sq], bias=bias)
            elif mi == 6:
                kb.act(tkv[:, 0:Wd], bk[:, 0:Wd], AF.Identity, [bk, bcol], [tkv], bias=bias)
                kb.act(sq[:, 2, 0:Wd], bk[:, 0:Wd], AF.Square, [bk, bcol], [sq], bias=bias)
            else:
                kb.act(krx[:, 0:Wd], bk[0:64, 0:Wd], AF.Identity, [bk, bcol], [krx], bias=bias)
        kb.mm(b[5][:, 0:Wd], ones_b.ap, sq[:, 0, 0:Wd], True, False, [ones_b, sq], [b[5]], sig=False)
        kb.mm(b[5][:, 0:Wd], ones_b.ap, sq[:, 1, 0:Wd], False, True, [ones_b, sq], [b[5]])
        kb.rsq(rstd_q[:, 0:Wd], b[5][:, 0:Wd], 1.0 / 256, 1e-6, tmpA[:, 0:Wd], [b[5]], [tmpA, rstd_q])
        kb.mm(b[5][:, 0:Wd], ones_b.ap, sq[:, 2, 0:Wd], True, True, [ones_b, sq], [b[5]])
        kb.rsq(rstd_kv[:, 0:Wd], b[5][:, 0:Wd], 1.0 / 128, 1e-6, tmpA[:, 0:Wd], [b[5]], [tmpA, rstd_kv])
        for r in range(2):
            kb.tt(cqn[:, r, 0:Wd], tq[:, r, 0:Wd], rstd_q[:, 0:Wd], ALU.mult, [tq, rstd_q], [cqn])
        kb.tt(ckvn[:, 0:Wd], tkv[:, 0:Wd], rstd_kv[:, 0:Wd], ALU.mult, [tkv, rstd_kv], [ckvn])
        kb.tt(t1[0:32, 0:Wd], krx[0:32, 0:Wd], cs[0:32, 0:Wd], ALU.mult, [krx, cs], [t1])
        kb.tt(t2[0:32, 0:Wd], krx[32:64, 0:Wd], cs[32:64, 0:Wd], ALU.mult, [krx, cs], [t2])
        kb.tt(krr[:, 0:Wd], t1[0:32, 0:Wd], t2[0:32, 0:Wd], ALU.add, [t1, t2], [krr])
        for h in range(8):
            bk = b[6 + h % 2]
            kb.mm(bk[:, 0:Wd], Wk[:, h * 128:(h + 1) * 128], ckvn[:, 0:Wd], True, True, [Wk, ckvn], [bk])
            kt = kts.next()
            kb.cp(kt[64:128, 0:Wd], bk[64:128, 0:Wd], [bk], [kt], eng="act")
            kb.cp(kt[0:32, 0:Wd], krr[:, 0:Wd], [krr], [kt], eng="pool")
            kb.dma("sp", E["KT"][h][:, col0:col0 + Wd], kt[:, 0:Wd], [kt], [])
        for h in range(8):
            bk = b[6 + h % 2]
            for r in range(2):
                kb.mm(bk[:, 0:Wd], Wq[:, r, h * 128:(h + 1) * 128], cqn[:, r, 0:Wd], r == 0, r == 1, [Wq, cqn], [bk], sig=(r == 1))
            qt = qts.next()
            kb.cp(qt[64:128, 0:Wd], bk[64:128, 0:Wd], [bk], [qt], eng="act")
            kb.tt(t1[0:32, 0:Wd], bk[0:32, 0:Wd], cs[0:32, 0:Wd], ALU.mult, [bk, cs], [t1])
            kb.tt(t2[0:32, 0:Wd], bk[32:64, 0:Wd], cs[32:64, 0:Wd], ALU.mult, [bk, cs], [t2])
            kb.tt(qt[0:32, 0:Wd], t1[0:32, 0:Wd], t2[0:32, 0:Wd], ALU.add, [t1, t2], [qt])
            kb.dma("sp", E["QT"][h][:, col0:col0 + Wd], qt[:, 0:Wd], [qt], [])
        for tl in range(ntl):
            i = gi * 4 + tl
            ts_ = slice(tl * 128, (tl + 1) * 128)
            kb.mm(b[3].ap, ckvn[:, ts_], Wv.ap, True, True, [ckvn, Wv], [b[3]])
            va = vaugs.next()
            kb.cp(va[:, :, 0:64], b[3].ap.rearrange("p (a b) -> p a b", a=8), [b[3]], [va], eng="act")
            kb.dma("sp", V0v[:, :, i, :], va.ap, [va], [])
            for k in range(8):
                kb.mm(b[4].ap, xT[:, k, ts_], W[st][:, k, 960:1472], k == 0, False, [xT, W[st]], [b[4]], sig=False)
            kb.mm(b[4].ap, ones_b[0:1, 0:128], brow[st].ap, False, True, [ones_b, brow[st]], [b[4]])
            kb.act(gv.ap, b[4].ap, AF.Gelu, [b[4]], [gv])
            kb.v(lambda e: e.bn_stats(out=st6.ap, in_=gv.ap), [gv], [st6])
            kb.v(lambda e: e.bn_aggr(out=mv.ap, in_=st6.ap), [st6], [mv])
            kb.ts(rs1[:, 0:1], mv[:, 1:2], 1e-5, None, ALU.add, None, [mv], [rs1])
            kb.tt(rs1[:, 1:2], rs1[:, 0:1], neghalf[:, 0:1], ALU.pow, [rs1, neghalf], [rs1], eng="pool")
            kb.ts(vhat.ap, gv.ap, mv[:, 0:1], rs1[:, 1:2], ALU.subtract, ALU.mult, [gv, mv, rs1], [vhat])
            for g in range(4):
                gs = slice(g * 128, (g + 1) * 128)
                kb.mm(b[5][:, gs], vhat[:, gs], wsT[:, gs], True, True, [vhat, wsT], [b[5]], sig=(g == 3))
            for g in range(4):
                gs = slice(g * 128, (g + 1) * 128)
                kb.stt(tmpg[:, gs], b[5][:, gs], gln[:, g:g + 1], Rt[:, gs], ALU.mult, ALU.add, [b[5], gln, Rt], [tmpg])
            aT = aTs.next()
            kb.tt(aT.ap, tmpg.ap.rearrange("p (a b) -> p a b", a=4), guT[:, :, ts_], ALU.mult, [tmpg, guT], [aT])
            kb.dma("sp", AT0v[:, :, i * 128:(i + 1) * 128], aT.ap, [aT], [])

    xbox = [None]
    xT_prev = None
    for gi in range(17):
        xT_cur = stageL(gi)
        if xT_prev is not None:
            stageP(gi - 1, xT_prev)
        xT_prev = xT_cur
    stageP(16, xT_prev)


def phase_attn(kb, E, C, L):
    b = kb.banks
    ones_b, ones_f = C["ones_b"], C["ones_f"]
    scale = (96 ** -0.5) if L == 0 else 0.125
    nqb = 17 if L == 0 else 16
    Vd = E["V0"] if L == 0 else E["V1"]
    OT = E["OT"]
    KTb = [kb.tile([128, NALL], BF16, f"KTb{i}") for i in range(2)]
    Vb = [kb.tile([128, 66, 128], BF16, f"Vb{i}") for i in range(2)]
    qts = Rot([kb.tile([128, 512], BF16, f"aq{i}") for i in range(2)])
    if L == 0:
        Ps = Rot([kb.tile([128, 512], BF16, f"P{j}") for j in range(3)])
        rl = [kb.tile([128, 512], F32, f"rl{i}") for i in range(2)]
        ots = Rot([kb.tile([64, 512], BF16, f"ot{i}") for i in range(3)])
    else:
        Ps = Rot([kb.tile([128, 2, 512], BF16, f"P12_{j}") for j in range(4)])
        accs = Rot([kb.tile([128, 512], F32, f"acc{j}") for j in range(2)])
        rls = [kb.tile([128, 512], F32, f"rl{i}") for i in range(2)]
        tAs = [kb.tile([128, 512], F32, f"tA{i}") for i in range(2)]
        ox = kb.tile([128, 512], F32, "ox")
        osb = [kb.tile([128, 512], F32, f"osb{i}") for i in range(2)]
        l2s = kb.tile([128, 512], F32, "l2s")
        sqx = kb.tile([128, 512], BF16, "sqx")
        tmpR = kb.tile([128, 512], F32, "tmpR")
        rstd = kb.tile([128, 512], F32, "rstdo")
        ots = Rot([kb.tile([128, 512], BF16, f"ot{i}") for i in range(2)])
        subg = kb.tile([128, 1], F32, "subg")
        kb.dma("sp", subg.ap, E["subln"], [], [subg])
        kb.ts(subg.ap, subg.ap, 1.0 - LAM_INIT, None, ALU.mult, None, [subg], [subg])
        neglam = C["neglam"]

    def load_head(h, bi):
        kb.dma("sp", KTb[bi].ap, E["KT"][h], [], [KTb[bi]])
        kb.dma("sp", Vb[bi].ap, Vd[h], [], [Vb[bi]])

    def loadQ(h_, qb_):
        Wd_ = 512 if qb_ < 16 else 256
        qt_ = qts.next()
        kb.dma("sp", qt_[:, 0:Wd_], E["QT"][h_][:, qb_ * 512:qb_ * 512 + Wd_], [], [qt_])
        return qt_

    pending_epi = []
    load_head(0, 0)
    for h in range(8):
        bi = h % 2
        if h + 1 < 8:
            load_head(h + 1, (h + 1) % 2)
        K_ = KTb[bi]
        V_ = Vb[bi]
        for qb in range(nqb):
            Wd = 512 if qb < 16 else 256
            col0 = qb * 512
            ktl = list(range(66)) if qb < 16 else [64, 65]
            n = len(ktl)
            if h == 0 and qb == 0:
                qt_next = loadQ(0, 0)
            qt = qt_next
            if qb + 1 < nqb:
                qt_next = loadQ(h, qb + 1)
            elif h + 1 < 8:
                qt_next = loadQ(h + 1, 0)
            if L == 0:
                O = b[4 + qb % 2]

                def S_(j):
                    kt = ktl[j]
                    sb = b[j % 3]
                    kb.mm(sb[:, 0:Wd], K_[:, kt * 128:(kt + 1) * 128], qt[:, 0:Wd], True, True, [K_, qt], [sb])
                S_(0)
                if n > 1:
                    S_(1)
                for j in range(n):
                    if j + 2 < n:
                        S_(j + 2)
                    sb = b[j % 3]
                    P = Ps.next()
                    kb.act(P[:, 0:Wd], sb[:, 0:Wd], AF.Exp, [sb], [P], scale=scale)
                    kb.mm(O[:, 0:Wd], V_[:, ktl[j], :], P[:, 0:Wd], j == 0, j == n - 1, [V_, P], [O])
                r = rl[qb % 2]
                kb.v(lambda e: e.reciprocal(out=r[64:128, 0:Wd], in_=O[64:128, 0:Wd]), [O], [r])
                ot = ots.next()
                kb.tt(ot[0:64, 0:Wd], O[0:64, 0:Wd], r[64:128, 0:Wd], ALU.mult, [O, r], [ot])
                kb.dma("sp", OT[h * 64:(h + 1) * 64, col0:col0 + Wd], ot[:, 0:Wd], [ot], [])
            else:
                O12 = [b[4], b[5]]
                L2 = b[6]
                acc = accs.next()

                def S_(j):
                    kt = ktl[j]
                    for i in range(2):
                        sb = b[(j % 2) * 2 + i]
                        ps = slice(i * 64, (i + 1) * 64)
                        kb.mm(sb.ap, K_[ps, kt * 128:(kt + 1) * 128], qt[ps, :], True, True, [K_, qt], [sb])
                S_(0)
                for j in range(n):
                    if j + 1 < n:
                        S_(j + 1)
                    s0 = (j % 2) * 2
                    P = Ps.next()
                    kb.act(P.ap.rearrange("p a b -> p (a b)"), kb.bank2(s0), AF.Exp, [b[s0], b[s0 + 1]], [P], scale=scale)
                    for i in range(2):
                        kb.mm(O12[i].ap, V_[:, ktl[j], :], P[:, i, :], j == 0, j == n - 1, [V_, P], [O12[i]])
                    kb.mm(L2.ap, ones_b.ap, P[:, 1, :], j == 0, j == n - 1, [ones_b, P], [L2])
                    if j == 0:
                        kb.cp(acc.ap, P[:, 0, :], [P], [acc])
                    else:
                        kb.tt(acc.ap, acc.ap, P[:, 0, :], ALU.add, [acc, P], [acc])
                    if j % 6 == 5 and pending_epi:
                        pending_epi.pop(0)()
                while pending_epi:
                    pending_epi.pop(0)()
                kb.cp(osb[0].ap, O12[0].ap, [O12[0]], [osb[0]], eng="act")
                kb.cp(osb[1].ap, O12[1].ap, [O12[1]], [osb[1]])
                kb.cp(l2s.ap, L2.ap, [L2], [l2s])
                def mk_epi(acc=acc, h=h, col0=col0, Wd=Wd):
                    def e0():
                        kb.mm(b[7].ap, ones_f.ap, acc.ap, True, True, [ones_f, acc], [b[7]])
                        kb.v(lambda e: e.reciprocal(out=rls[0].ap, in_=b[7].ap), [b[7]], [rls[0]])

                    def e1():
                        kb.v(lambda e: e.reciprocal(out=rls[1].ap, in_=l2s.ap), [l2s], [rls[1]])

                    def e2():
                        for i in range(2):
                            kb.tt(tAs[i].ap, osb[i].ap, rls[i].ap, ALU.mult, [osb[i], rls[i]], [tAs[i]], eng="pool")

                    def e3():
                        kb.stt(ox.ap, tAs[1].ap, neglam[:, 0:1], tAs[0].ap, ALU.mult, ALU.add, [tAs[0], tAs[1], neglam], [ox])
                        kb.tt(sqx.ap, ox.ap, ox.ap, ALU.mult, [ox], [sqx], eng="pool")

                    def e4():
                        kb.mm(b[7].ap, ones_b.ap, sqx.ap, True, True, [ones_b, sqx], [b[7]])
                        kb.act(tmpR.ap, b[7].ap, AF.Sqrt, [b[7]], [tmpR], bias=1e-6, scale=1.0 / 128)

                    def e5():
                        kb.v(lambda e: e.reciprocal(out=rstd.ap, in_=tmpR.ap), [tmpR], [rstd])

                    def e6():
                        ot = ots.next()
                        kb.stt(ot.ap, ox.ap, subg[:, 0:1], rstd.ap, ALU.mult, ALU.mult, [ox, subg, rstd], [ot])
                        kb.dma("sp", OT[h * 128:(h + 1) * 128, col0:col0 + Wd], ot.ap, [ot], [])
                    return [e0, e1, e2, e3, e4, e5, e6]
                while pending_epi:
                    pending_epi.pop(0)()
                pending_epi.extend(mk_epi())
                if h == 7 and qb == nqb - 1:
                    while pending_epi:
                        pending_epi.pop(0)()


def layer_norm_tile(kb, y, gam, bet, out, st12, mv, rs1, neghalf):
    kb.v(lambda e: e.bn_stats(out=st12[:, 0:6], in_=y[:, 0:512]), [y], [st12])
    kb.v(lambda e: e.bn_stats(out=st12[:, 6:12], in_=y[:, 512:1024]), [y], [st12])
    kb.v(lambda e: e.bn_aggr(out=mv.ap, in_=st12.ap), [st12], [mv])
    kb.ts(rs1[:, 0:1], mv[:, 1:2], 1e-5, None, ALU.add, None, [mv], [rs1])
    kb.tt(rs1[:, 1:2], rs1[:, 0:1], neghalf[:, 0:1], ALU.pow, [rs1, neghalf], [rs1], eng="pool")
    kb.stt(rs1[:, 0:1], mv[:, 0:1], -1.0, rs1[:, 1:2], ALU.mult, ALU.mult, [mv, rs1], [rs1])
    kb.act(y.ap, y.ap, AF.Identity, [y, rs1], [y], bias=rs1[:, 0:1], scale=rs1[:, 1:2])
    kb.tt(y.ap, y.ap, gam.ap, ALU.mult, [y, gam], [y], eng="pool")
    kb.tt(out.ap, y.ap, bet.ap, ALU.add, [y, bet], [out])


def lockstep(gens):
    gens = list(gens)
    while gens:
        for g in list(gens):
            try:
                next(g)
            except StopIteration:
                gens.remove(g)


def phase_C(kb, E, C, L):
    b = kb.banks
    ntiles = 66 if L == 0 else 64
    nst = 2 if L == 0 else 1
    ident_b, ident_f, neghalf = C["ident_b"], C["ident_f"], C["neghalf"]
    wout = kb.tile([128, 8, 1024], BF16, "wout")
    kb.dma("pool", wout.ap, E[f"wout{L}"], [], [wout])
    router_f = kb.tile([128, 8, 16], F32, "router_f")
    kb.dma("sp", router_f.ap, E[f"router{L}"], [], [router_f])
    router_b = kb.tile([128, 8, 16], BF16, "router_b")
    kb.cp(router_b.ap, router_f.ap, [router_f], [router_b])
    gate = [bc_load(kb, E, L, st, 2, f"gate{st}", True) for st in range(nst)]
    G1 = kb.tile([128, 1024], F32, "G1")
    B1 = kb.tile([128, 1024], F32, "B1")
    G2 = [kb.tile([128, 1024], F32, f"G2_{st}") for st in range(nst)]
    B2 = [kb.tile([128, 1024], F32, f"B2_{st}") for st in range(nst)]
    mark = kb.off
    gam = bc_row(kb, E[f"lnmix{L}"][0, :], "gam")
    bet = bc_row(kb, E[f"lnmix{L}"][1, :], "bet")
    kb.ts(G1.ap, gam.ap, ALPHA, None, ALU.mult, None, [gam], [G1])
    kb.ts(B1.ap, bet.ap, ALPHA, None, ALU.mult, None, [bet], [B1])
    for st in range(nst):
        Af = bc_load(kb, E, L, st, 4, f"Af{st}", True)
        shf = bc_load(kb, E, L, st, 3, f"shf{st}")
        kb.tt(G2[st].ap, gam.ap, Af.ap, ALU.mult, [gam, Af], [G2[st]])
        kb.tt(B2[st].ap, bet.ap, Af.ap, ALU.mult, [bet, Af], [B2[st]])
        kb.tt(B2[st].ap, B2[st].ap, shf.ap, ALU.add, [B2[st], shf], [B2[st]])
    kb.s.barrier()
    kb.off = mark
    affx = [kb.tile([128, 16, 8], F32, f"affx{s}") for s in range(8)]
    for t in affx:
        kb.v(lambda e: e.memset(t.ap, 0.0), [], [t])
    catTs = Rot([kb.tile([128, 8, 128], BF16, f"catT{i}") for i in range(4)])
    xts = Rot([kb.tile([128, 1024], F32, f"xt{i}") for i in range(4)])

    def mk(shape, dt, nm):
        return [Rot([kb.tile(shape, dt, f"{nm}{sl}_{i}") for i in range(2)]) for sl in range(2)]
    tmps, ysC, x1as, t2s = mk([128, 1024], F32, "tmpC"), mk([128, 1024], F32, "yC"), mk([128, 1024], F32, "x1a"), mk([128, 1024], F32, "t2C")
    hfs = mk([128, 1024], BF16, "hf")
    hfTs = mk([128, 8, 128], BF16, "hfT")
    st12s, mvs, rs1s, sms, exs = (mk([128, 12], F32, "st12"), mk([128, 2], F32, "mvC"), mk([128, 2], F32, "rs1C"),
                                  mk([128, 4], F32, "sm"), mk([128, 16], F32, "ex"))
    affc_t = kb.tile([128, 16], F32, "affc_t")
    affrows = mk([128, 16], F32, "affrow")
    zrow = kb.tile([128, 16], F32, "zrow")
    kb.v(lambda e: e.memset(zrow.ap, 0.0), [], [zrow])
    kb.dma("sp", E["AFF"][NALL:NPAD, :], zrow.ap, [zrow], [])
    AT0v = E["AT0"].rearrange("(g c) t -> c g t", c=128)
    OTv = E["OT"].rearrange("(k p) t -> p k t", p=128)

    def loadsC(i):
        cols = slice(i * 128, (i + 1) * 128)
        catT = catTs.next()
        if L == 0:
            kb.dma("sp", catT[:, 0:4, :], AT0v[:, :, cols], [], [catT])
            kb.dma("sp", catT[:, 4:8, :], OTv[:, 0:4, cols], [], [catT])
        else:
            kb.dma("sp", catT.ap, OTv[:, :, cols], [], [catT])
        xt = xts.next()
        kb.dma("sp", xt.ap, rows_of(E, L, i), [], [xt])
        return catT, xt

    def tileA(i, sl, ld, out):
        st = 0 if i < 64 else 1
        catT, xt = ld
        tmp, y, st12, mv, rs1 = tmps[sl].next(), ysC[sl].next(), st12s[sl].next(), mvs[sl].next(), rs1s[sl].next()
        mb = (b[0], b[1]) if sl == 0 else (b[6], b[7])
        for half in range(2):
            hs = slice(half * 512, (half + 1) * 512)
            for k in range(8):
                kb.mm(mb[half].ap, catT[:, k, :], wout[:, k, hs], k == 0, k == 7, [catT, wout], [mb[half]], sig=(k == 7))
            yield
            kb.tt(tmp[:, hs], mb[half].ap, gate[st][:, hs], ALU.mult, [mb[half], gate[st]], [tmp])
            yield
        kb.stt(y.ap, xt.ap, ALPHA, tmp.ap, ALU.mult, ALU.add, [xt, tmp], [y])
        yield
        kb.v(lambda e: e.bn_stats(out=st12[:, 0:6], in_=y[:, 0:512]), [y], [st12])
        yield
        kb.v(lambda e: e.bn_stats(out=st12[:, 6:12], in_=y[:, 512:1024]), [y], [st12])
        yield
        kb.v(lambda e: e.bn_aggr(out=mv.ap, in_=st12.ap), [st12], [mv])
        kb.ts(rs1[:, 0:1], mv[:, 1:2], 1e-5, None, ALU.add, None, [mv], [rs1])
        yield
        kb.tt(rs1[:, 1:2], rs1[:, 0:1], neghalf[:, 0:1], ALU.pow, [rs1, neghalf], [rs1], eng="pool")
        yield
        kb.stt(rs1[:, 0:1], mv[:, 0:1], -1.0, rs1[:, 1:2], ALU.mult, ALU.mult, [mv, rs1], [rs1])
        yield
        kb.act(y.ap, y.ap, AF.Identity, [y, rs1], [y], bias=rs1[:, 0:1], scale=rs1[:, 1:2])
        yield
        x1a, t2, hf = x1as[sl].next(), t2s[sl].next(), hfs[sl].next()
        kb.tt(tmp.ap, y.ap, G1.ap, ALU.mult, [y, G1], [tmp], eng="pool")
        kb.tt(t2.ap, y.ap, G2[st].ap, ALU.mult, [y, G2[st]], [t2])
        yield
        kb.tt(hf.ap, t2.ap, B2[st].ap, ALU.add, [t2, B2[st]], [hf])
        kb.dma("sp", E["HF"][i * 128:(i + 1) * 128, :], hf.ap, [hf], [])
        yield
        kb.tt(x1a.ap, tmp.ap, B1.ap, ALU.add, [tmp, B1], [x1a])
        kb.dma("sp", E["FACC"][i * 128:(i + 1) * 128, :], x1a.ap, [x1a], [])
        out.append(hf)
        yield

    def tileB(i, sl, hf):
        hfT, sm, ex = hfTs[sl].next(), sms[sl].next(), exs[sl].next()
        tb = 2 + sl
        pb = kb.bank_bf(tb)
        for k in range(8):
            kb.tr(pb[:, k * 128:(k + 1) * 128], hf[:, k * 128:(k + 1) * 128], ident_b.ap, [hf, ident_b], [b[tb]], sig=(k == 7))
        yield
        kb.cp(hfT.ap, pb.rearrange("p (a b) -> p a b", a=8), [b[tb]], [hfT], eng="act")
        yield
        lg = b[tb][:, 512 - 16:512]
        for k in range(8):
            kb.mm(lg, hfT[:, k, :], router_b[:, k, :], k == 0, k == 7, [hfT, router_b], [b[tb]], sig=(k == 7))
        yield
        kb.v(lambda e: e.reduce_max(out=sm[:, 0:1], in_=lg, axis=mybir.AxisListType.X), [b[tb]], [sm])
        yield
        kb.ts(sm[:, 1:2], sm[:, 0:1], -1.0, None, ALU.mult, None, [sm], [sm])
        yield
        kb.act(ex.ap, lg, AF.Exp, [b[tb], sm], [ex, sm], bias=sm[:, 1:2], accum=sm[:, 2:3])
        yield
        kb.v(lambda e: e.reciprocal(out=sm[:, 3:4], in_=sm[:, 2:3]), [sm], [sm])
        yield
        if i < 64:
            s_, j = i % 8, i // 8
            ax = affx[s_]
            ar = affrows[sl].next()
            kb.ts(ar.ap, ex.ap, sm[:, 3:4], None, ALU.mult, None, [ex, sm], [ar])
            kb.dma("sp", E["AFF"][i * 128:(i + 1) * 128, :], ar.ap, [ar], [])
            kb.cp(ax[:, :, s_], ar.ap, [ar], [ax])
            yield
            bk = b[4 + j // 4]
            kb.mm(bk[:, (j % 4) * 128:(j % 4 + 1) * 128], ax.ap.rearrange("p a b -> p (a b)"), ident_f.ap, s_ == 0, s_ == 7,
                  [ax, ident_f], [bk])
        else:
            kb.ts(affc_t.ap, ex.ap, sm[:, 3:4], None, ALU.mult, None, [ex, sm], [affc_t])
            yield
            kb.tr(b[2][0:16, (i - 64) * 128:(i - 63) * 128], affc_t.ap, ident_f.ap, [affc_t, ident_f], [b[2]])
            if i == 65:
                kb.cp(C["affc"].ap, b[2][0:16, 0:256], [b[2]], [C["affc"]])
        yield

    npairs = ntiles // 2
    lds = [loadsC(0), loadsC(1)]
    prev = None
    for p in range(npairs):
        cur_ld = lds
        if p + 1 < npairs:
            lds = [loadsC(2 * p + 2), loadsC(2 * p + 3)]
        outs = [[], []]
        lockstep([tileA(2 * p, 0, cur_ld[0], outs[0]), tileA(2 * p + 1, 1, cur_ld[1], outs[1])])
        if prev is not None:
            lockstep([tileB(2 * (p - 1), 0, prev[0][0]), tileB(2 * (p - 1) + 1, 1, prev[1][0])])
        prev = outs
    lockstep([tileB(2 * (npairs - 1), 0, prev[0][0]), tileB(2 * (npairs - 1) + 1, 1, prev[1][0])])
    kb.cp(C["affT"][:, 0:512], b[4].ap, [b[4]], [C["affT"]])
    kb.cp(C["affT"][:, 512:1024], b[5].ap, [b[5]], [C["affT"]])


def phase_D(kb, E, C, L):
    b = kb.banks
    has_ctx = (L == 0)
    nst = 2 if L == 0 else 1
    ident_b, ident_f, mc, bd = C["ident_b"], C["ident_f"], C["mc"], C["bd"]
    affT = C["affT"]
    gatef = [bc_load(kb, E, L, st, 5, f"gatef{st}", True) for st in range(nst)]
    Wt = [[kb.tile([128, 8, 1024], BF16, f"w{nm}{i}") for nm in ("gate", "up", "down")] for i in range(2)]

    def load_w(e_, bi):
        for wi, nm in enumerate(("gate", "up", "down")):
            kb.dma("pool", Wt[bi][wi].ap, E[f"w{nm}{L}"][e_].rearrange("(k p) n -> p k n", p=128), [], [Wt[bi][wi]])

    load_w(0, 0)
    work = kb.tile([128, 1024], F32, "work")
    kb.cp(work.ap, affT.ap, [affT], [work])
    vals = kb.tile([128, CAP], F32, "vals")
    idxu = kb.tile([128, CAP], U32, "idxu")
    for it in range(CAP // 8):
        sl = slice(it * 8, (it + 1) * 8)
        kb.v(lambda e: e.max(out=vals[:, sl], in_=work.ap), [work], [vals])
        kb.v(lambda e: e.max_index(out=idxu[:, sl], in_max=vals[:, sl], in_values=work.ap), [work, vals], [idxu])
        kb.v(lambda e: e.match_replace(out=work.ap, in_to_replace=vals[:, sl], in_values=work.ap, imm_value=-1.0), [vals, work], [work])
    lo = kb.tile([128, 1], F32, "lo")
    hi = kb.tile([128, 1], F32, "hi")
    mid = kb.tile([128, 1], F32, "mid")
    cnt = kb.tile([128, 1], F32, "cnt")
    d1 = kb.tile([128, 1], F32, "d1")
    ge = kb.tile([128, 1], F32, "ge")
    junk = kb.tile([128, CAP], F32, "junk")
    kb.v(lambda e: e.memset(lo.ap, 0.0), [], [lo])
    kb.v(lambda e: e.memset(hi.ap, 1.0), [], [hi])
    for it in range(32):
        kb.tt(mid.ap, lo.ap, hi.ap, ALU.add, [lo, hi], [mid])
        kb.ts(mid.ap, mid.ap, 0.5, None, ALU.mult, None, [mid], [mid])
        kb.ts(junk.ap, vals.ap, mid[:, 0:1], 0.0, ALU.is_ge, ALU.add, [vals, mid], [junk, cnt], accum=cnt.ap)
        kb.mm(b[0][:, 0:1], bd.ap, cnt.ap, True, True, [bd, cnt], [b[0]])
        kb.ts(ge.ap, b[0][:, 0:1], ECAP - 0.5, None, ALU.is_ge, None, [b[0]], [ge])
        kb.tt(d1.ap, mid.ap, lo.ap, ALU.subtract, [mid, lo], [d1])
        kb.stt(lo.ap, d1.ap, ge[:, 0:1], lo.ap, ALU.mult, ALU.add, [d1, ge, lo], [lo])
        kb.tt(d1.ap, hi.ap, mid.ap, ALU.subtract, [hi, mid], [d1])
        kb.stt(hi.ap, d1.ap, ge[:, 0:1], mid.ap, ALU.mult, ALU.add, [d1, ge, mid], [hi])
    valid = kb.tile([128, CAP], F32, "valid")
    gvv = kb.tile([128, CAP], F32, "gvv")
    kb.ts(valid.ap, vals.ap, lo[:, 0:1], None, ALU.is_ge, None, [vals, lo], [valid])
    kb.tt(gvv.ap, vals.ap, valid.ap, ALU.mult, [vals, valid], [gvv])
    jbi = kb.tile([128, CAP], I32, "jbi")
    kb.v(lambda e: e.tensor_single_scalar(out=jbi.ap, in_=idxu.ap.bitcast(I32), scalar=7, op=ALU.arith_shift_right), [idxu], [jbi])
    idxf = kb.tile([128, CAP], F32, "idxf")
    jbf = kb.tile([128, CAP], F32, "jbf")
    tokf = kb.tile([128, CAP], F32, "tokf")
    kb.cp(idxf.ap, idxu.ap, [idxu], [idxf])
    kb.cp(jbf.ap, jbi.ap, [jbi], [jbf])
    kb.stt(tokf.ap, jbf.ap, 896.0, idxf.ap, ALU.mult, ALU.add, [jbf, idxf], [tokf])
    kb.ts(tokf.ap, tokf.ap, mc[:, 0:1], None, ALU.add, None, [tokf, mc], [tokf])
    mains = []
    for ai, arr in enumerate((tokf, gvv, valid)):
        bk = b[1 + ai]
        kb.tr(bk[:, 0:128], arr[:, 0:128], ident_f.ap, [arr, ident_f], [bk])
        m_ = kb.tile([128, 128], F32, f"main{ai}")
        kb.cp(m_.ap, bk[:, 0:128], [bk], [m_])
        mains.append(m_)
    IDXM = kb.tile([128, 128], I32, "IDXM")
    tmpi = kb.tile([128, 128], F32, "tmpi")
    kb.ts(tmpi.ap, mains[0].ap, mc[:, 1:2], None, ALU.subtract, None, [mains[0], mc], [tmpi])
    kb.tt(tmpi.ap, tmpi.ap, mains[2].ap, ALU.mult, [tmpi, mains[2]], [tmpi])
    kb.ts(tmpi.ap, tmpi.ap, mc[:, 1:2], None, ALU.add, None, [tmpi, mc], [tmpi])
    kb.cp(IDXM.ap, tmpi.ap, [tmpi], [IDXM])
    GM = mains[1]
    keyt = kb.tile([128, 64], F32, "keyt")
    kb.stt(keyt.ap, tokf[:, 128:192], 1.0, valid[:, 128:192], ALU.add, ALU.mult, [tokf, valid], [keyt])
    kdram = Tok("keys")
    kb.dma("sp", E["KEYS"], keyt.ap, [keyt], [kdram])
    kc = kb.tile([16, 512], F32, "kc")
    kb.dma("sp", kc.ap, E["KEYS"].rearrange("(e s) r -> e (s r)", s=8), [kdram], [kc])
    tkey = kb.tile([16, 128], F32, "tkey")
    for it in range(16):
        sl = slice(it * 8, (it + 1) * 8)
        kb.v(lambda e: e.max(out=tkey[:, sl], in_=kc.ap), [kc], [tkey])
        kb.v(lambda e: e.match_replace(out=kc.ap, in_to_replace=tkey[:, sl], in_values=kc.ap, imm_value=-1.0), [tkey, kc], [kc])
    drow = kb.tile([16, 128], F32, "drow")
    kb.dma("sp", drow.ap, E["drow"], [], [drow])
    tval = kb.tile([16, 128], F32, "tval")
    kb.ts(tval.ap, tkey.ap, 0.5, None, ALU.is_ge, None, [tkey], [tval])
    tix = kb.tile([16, 128], F32, "tix")
    kb.stt(tix.ap, tkey.ap, -1.0, drow.ap, ALU.add, ALU.subtract, [tkey, drow], [tix])
    kb.tt(tix.ap, tix.ap, tval.ap, ALU.mult, [tix, tval], [tix])
    kb.tt(tix.ap, tix.ap, drow.ap, ALU.add, [tix, drow], [tix])
    kb.tr(b[4][:, 0:16], tix.ap, ident_f[0:16, 0:16], [tix, ident_f], [b[4]])
    IDXT = kb.tile([128, 16], I32, "IDXT")
    kb.cp(IDXT.ap, b[4][:, 0:16], [b[4]], [IDXT])
    gas = [kb.tile([128, 16], F32, f"ga{e_}") for e_ in range(16)]
    if has_ctx:
        cwork = kb.tile([16, 256], F32, "cwork")
        kb.cp(cwork.ap, C["affc"].ap, [C["affc"]], [cwork])
        cvals = kb.tile([16, CCAP], F32, "cvals")
        cidx = kb.tile([16, CCAP], U32, "cidx")
        for it in range(CCAP // 8):
            sl = slice(it * 8, (it + 1) * 8)
            kb.v(lambda e: e.max(out=cvals[:, sl], in_=cwork.ap), [cwork], [cvals])
            kb.v(lambda e: e.max_index(out=cidx[:, sl], in_max=cvals[:, sl], in_values=cwork.ap), [cwork, cvals], [cidx])
            kb.v(lambda e: e.match_replace(out=cwork.ap, in_to_replace=cvals[:, sl], in_values=cwork.ap, imm_value=-1.0), [cvals, cwork], [cwork])
        cidf = kb.tile([16, CCAP], F32, "cidf")
        kb.cp(cidf.ap, cidx.ap, [cidx], [cidf])
        kb.ts(cidf.ap, cidf.ap, float(NLAT), None, ALU.add, None, [cidf], [cidf])
        kb.tr(b[4][0:32, 0:16], cidf.ap, ident_f[0:16, 0:16], [cidf, ident_f], [b[4]])
        kb.tr(b[4][0:32, 16:32], cvals.ap, ident_f[0:16, 0:16], [cvals, ident_f], [b[4]])
        CIDX = kb.tile([32, 16], I32, "CIDX")
        CG = kb.tile([32, 16], F32, "CG")
        kb.cp(CIDX.ap, b[4][0:32, 0:16], [b[4]], [CIDX])
        kb.cp(CG.ap, b[4][0:32, 16:32], [b[4]], [CG])
    xss = Rot([kb.tile([128, 1024], BF16, f"xs{i}") for i in range(6)])
    xsTs = Rot([kb.tile([128, 8, 512], BF16, f"xsT{i}") for i in range(2)])
    hidTs = Rot([kb.tile([128, 8, 512], BF16, f"hidT{i}") for i in range(2)])
    sgs = Rot([kb.tile([128, 512], F32, f"sg{i}") for i in range(2)])
    ysbs = Rot([kb.tile([128, 1024], F32, f"ysb{i}") for i in range(3)])

    work_items = []
    for e_ in range(16):
        chunks = [(IDXM[:, e_ * 8 + s_:e_ * 8 + s_ + 1], GM[:, e_ * 8 + s_:e_ * 8 + s_ + 1], 128, 0, IDXM, GM) for s_ in range(8)]
        chunks += [(IDXT[:, e_:e_ + 1], gas[e_][:, e_:e_ + 1], 128, 0, IDXT, gas[e_])]
        blocks = [chunks[0:4], chunks[4:8], chunks[8:9]]
        if has_ctx:
            blocks.append([(CIDX[:, e_:e_ + 1], CG[:, e_:e_ + 1], 32, 1, CIDX, CG)])
        for bi_, blk in enumerate(blocks):
            work_items.append((e_, bi_, blk))

    def gathers(blk):
        res = []
        R = blk[0][2]
        for (icol, gcol, R_, st, it_, gt_) in blk:
            xs = xss.next()
            kb.s.dma("pool", lambda q: q.indirect_dma_start(out=xs[0:R, :], out_offset=None, in_=E["HF"],
                                                             in_offset=bass.IndirectOffsetOnAxis(ap=icol, axis=0)),
                     _toks([it_]), _toks([xs]))
            if it_ is IDXT:
                kb.s.dma("pool", lambda q: q.indirect_dma_start(out=gt_.ap, out_offset=None, in_=E["AFF"],
                                                                 in_offset=bass.IndirectOffsetOnAxis(ap=icol, axis=0)),
                         _toks([it_]), _toks([gt_]))
            res.append(xs)
        return res

    prev_sc = []
    cur_sc = []
    g_next = gathers(work_items[0][2])
    for wi_, (e_, bi_, blk) in enumerate(work_items):
        bi = e_ % 2
        if bi_ == 0:
            if e_ + 1 < 16:
                load_w(e_ + 1, (e_ + 1) % 2)
            prev_sc = cur_sc
            cur_sc = []
        wg, wu, wd = Wt[bi]
        R = blk[0][2]
        Wd = R * len(blk)
        xs_list = g_next
        xsT = xsTs.next()
        hidT = hidTs.next()
        for cl, xs in enumerate(xs_list):
            pb = kb.bank_bf(0)
            for k in range(8):
                kb.tr(pb[:, k * 128:k * 128 + R], xs[0:R, k * 128:(k + 1) * 128], ident_b[0:R, 0:R], [xs, ident_b], [b[0]], sig=(k == 7))
            kb.cp(xsT[:, :, cl * R:(cl + 1) * R], pb.rearrange("p (a b) -> p a b", a=8)[:, :, 0:R], [b[0]], [xsT],
                  eng=("act" if cl % 2 else "dve"))
        if wi_ + 1 < len(work_items):
            g_next = gathers(work_items[wi_ + 1][2])
        for ffc in range(8):
            fs = slice(ffc * 128, (ffc + 1) * 128)
            G_ = b[1 + ffc % 2]
            U_ = b[3 + ffc % 2]
            for k in range(8):
                kb.mm(G_[:, 0:Wd], wg[:, k, fs], xsT[:, k, 0:Wd], k == 0, k == 7, [wg, xsT], [G_], sig=(k == 7))
            for k in range(8):
                kb.mm(U_[:, 0:Wd], wu[:, k, fs], xsT[:, k, 0:Wd], k == 0, k == 7, [wu, xsT], [U_], sig=(k == 7))
            sg = sgs.next()
            kb.act(sg[:, 0:Wd], G_[:, 0:Wd], AF.Silu, [G_], [sg])
            kb.tt(hidT[:, ffc, 0:Wd], sg[:, 0:Wd], U_[:, 0:Wd], ALU.mult, [sg, U_], [hidT])
        for cl, (icol, gcol, R_, st, it_, gt_) in enumerate(blk):
            ysb = ysbs.next()
            for half in range(2):
                hs = slice(half * 512, (half + 1) * 512)
                Y_ = b[5 + half]
                for ffc in range(8):
                    kb.mm(Y_[0:R, :], hidT[:, ffc, cl * R:(cl + 1) * R], wd[:, ffc, hs], ffc == 0, ffc == 7, [hidT, wd], [Y_], sig=(ffc == 7))
                kb.stt(ysb[0:R, hs], Y_[0:R, :], gcol, gatef[st][0:R, hs], ALU.mult, ALU.mult, [Y_, gt_, gatef[st]], [ysb])
            tk = Tok("sc")
            kb.s.dma("pool", lambda q: q.indirect_dma_start(out=E["FACC"], out_offset=bass.IndirectOffsetOnAxis(ap=icol, axis=0),
                                                             in_=ysb[0:R, :], in_offset=None, compute_op=ALU.add),
                     _toks([ysb, it_]) + prev_sc, [tk])
            cur_sc.append(tk)


def phase_E(kb, E, C):
    neghalf = C["neghalf"]
    gam = bc_row(kb, E["lnffn1"][0, :], "gamE")
    bet = bc_row(kb, E["lnffn1"][1, :], "betE")
    ys = Rot([kb.tile([128, 1024], F32, f"yE{i}") for i in range(4)])
    outs = Rot([kb.tile([128, 1024], F32, f"oE{i}") for i in range(4)])
    st12 = kb.tile([128, 12], F32, "st12E")
    mv = kb.tile([128, 2], F32, "mvE")
    rs1 = kb.tile([128, 2], F32, "rs1E")
    def loadE(i):
        y = ys.next()
        kb.dma("sp", y.ap, E["FACC"][i * 128:(i + 1) * 128, :], [], [y])
        return y

    st12s = [kb.tile([128, 12], F32, f"st12E{i}") for i in range(2)]
    mvs = [kb.tile([128, 2], F32, f"mvE{i}") for i in range(2)]
    rs1s = [kb.tile([128, 2], F32, f"rs1E{i}") for i in range(2)]

    def tileE(i, sl, y):
        o = outs.next()
        st12_, mv_, rs1_ = st12s[sl], mvs[sl], rs1s[sl]
        kb.v(lambda e: e.bn_stats(out=st12_[:, 0:6], in_=y[:, 0:512]), [y], [st12_])
        yield
        kb.v(lambda e: e.bn_stats(out=st12_[:, 6:12], in_=y[:, 512:1024]), [y], [st12_])
        yield
        kb.v(lambda e: e.bn_aggr(out=mv_.ap, in_=st12_.ap), [st12_], [mv_])
        kb.ts(rs1_[:, 0:1], mv_[:, 1:2], 1e-5, None, ALU.add, None, [mv_], [rs1_])
        yield
        kb.tt(rs1_[:, 1:2], rs1_[:, 0:1], neghalf[:, 0:1], ALU.pow, [rs1_, neghalf], [rs1_], eng="pool")
        yield
        kb.stt(rs1_[:, 0:1], mv_[:, 0:1], -1.0, rs1_[:, 1:2], ALU.mult, ALU.mult, [mv_, rs1_], [rs1_])
        yield
        kb.act(y.ap, y.ap, AF.Identity, [y, rs1_], [y], bias=rs1_[:, 0:1], scale=rs1_[:, 1:2])
        yield
        kb.tt(y.ap, y.ap, gam.ap, ALU.mult, [y, gam], [y], eng="pool")
        yield
        kb.tt(o.ap, y.ap, bet.ap, ALU.add, [y, bet], [o])
        kb.dma("sp", E["out"][i * 128:(i + 1) * 128, :], o.ap, [o], [])
        yield

    lds = [loadE(0), loadE(1)]
    for p in range(32):
        cur = lds
        if p + 1 < 32:
            lds = [loadE(2 * p + 2), loadE(2 * p + 3)]
        lockstep([tileE(2 * p, 0, cur[0]), tileE(2 * p + 1, 1, cur[1])])


def phase_A1(kb, E, C):
    b = kb.banks
    modT = C["modT"][1]
    ident_b, ones_b, ones_f, neghalf = C["ident_b"], C["ones_b"], C["ones_f"], C["neghalf"]
    l4 = kb.tile([1, 256], F32, "l4")
    kb.dma("sp", l4.ap, E["lam4"], [], [l4])
    lp = kb.tile([1, 128], F32, "lp")
    lsum = kb.tile([1, 4], F32, "lsum")
    kb.tt(lp[:, 0:64], l4[:, 0:64], l4[:, 64:128], ALU.mult, [l4], [lp])
    kb.tt(lp[:, 64:128], l4[:, 128:192], l4[:, 192:256], ALU.mult, [l4], [lp])
    kb.v(lambda e: e.reduce_sum(out=lsum[:, 0:1], in_=lp[:, 0:64], axis=mybir.AxisListType.X), [lp], [lsum])
    kb.v(lambda e: e.reduce_sum(out=lsum[:, 1:2], in_=lp[:, 64:128], axis=mybir.AxisListType.X), [lp], [lsum])
    kb.act(lsum[:, 2:4], lsum[:, 0:2], AF.Exp, [lsum], [lsum])
    kb.tt(lsum[:, 0:1], lsum[:, 3:4], lsum[:, 2:3], ALU.subtract, [lsum], [lsum])
    kb.ts(lsum[:, 1:2], lsum[:, 0:1], -LAM_INIT, None, ALU.add, None, [lsum], [lsum])
    kb.mm(b[0][:, 0:1], ones_f[0:1, :], lsum[:, 1:2], True, True, [ones_f, lsum], [b[0]])
    kb.cp(C["neglam"].ap, b[0][:, 0:1], [b[0]], [C["neglam"]])
    shc = kb.tile([128, 8, 2], BF16, "shc1")
    kb.cp(shc.ap, modT[:, 0:8, :], [modT], [shc])
    A1 = kb.tile([128, 8, 2], F32, "A1_1")
    kb.ts(A1.ap, modT[:, 8:16, :], 1.0, None, ALU.add, None, [modT], [A1])
    W = [kb.tile([128, 8, 3072], BF16, f"W1_{st}") for st in range(2)]
    bcol = kb.tile([128, 16, 2], F32, "bcol1")
    brow = [kb.tile([1, 1024], BF16, f"brow1_{st}") for st in range(2)]
    mark = kb.off
    wtmp = kb.tile([128, 8, 1024], F32, "wtmp1")
    Wun = kb.tile([128, 8, 1024], BF16, "Wun1")
    for third in range(3):
        cs_ = slice(third * 1024, (third + 1) * 1024)
        kb.dma("sp", wtmp.ap, E["win1"][:, :, cs_], [], [wtmp])
        for k in range(8):
            kb.cp(Wun[:, k, :], wtmp[:, k, :], [wtmp], [Wun], eng=("act" if k % 2 else "dve"))
        for st in range(2):
            for k in range(8):
                if k % 2:
                    kb.act(W[st][:, k, cs_], wtmp[:, k, :], AF.Identity, [wtmp, A1], [W[st]], scale=A1[:, k, st:st + 1])
                else:
                    kb.ts(W[st][:, k, cs_], wtmp[:, k, :], A1[:, k, st:st + 1], None, ALU.mult, None, [wtmp, A1], [W[st]])
        if third < 2:
            for m in range(8):
                mi = third * 8 + m
                for k in range(8):
                    kb.mm(b[0][:, mi * 2:mi * 2 + 2], Wun[:, k, m * 128:(m + 1) * 128], shc[:, k, :], k == 0, k == 7, [Wun, shc], [b[0]], sig=(k == 7))
        else:
            for st in range(2):
                for half in range(2):
                    for k in range(8):
                        kb.mm(b[1 + half][0:1, :], shc[:, k, st:st + 1], Wun[:, k, half * 512:(half + 1) * 512], k == 0, k == 7,
                              [Wun, shc], [b[1 + half]], sig=(k == 7))
                    kb.cp(brow[st][:, half * 512:(half + 1) * 512], b[1 + half][0:1, :], [b[1 + half]], [brow[st]])
    kb.cp(bcol.ap, b[0][:, 0:32].rearrange("p (a b) -> p a b", a=16), [b[0]], [bcol])
    kb.s.barrier()
    kb.off = mark
    gam = bc_row(kb, E["lnffn0"][0, :], "gamA1")
    bet = bc_row(kb, E["lnffn0"][1, :], "betA1")
    ys = Rot([kb.tile([128, 1024], F32, f"yA{i}") for i in range(3)])
    x2s = Rot([kb.tile([128, 1024], F32, f"x2_{i}") for i in range(2)])
    xbs = Rot([kb.tile([128, 1024], BF16, f"xbA1_{i}") for i in range(2)])
    st12 = kb.tile([128, 12], F32, "st12A")
    mv = kb.tile([128, 2], F32, "mvA")
    rs1 = kb.tile([128, 2], F32, "rs1A")
    xTs = Rot([kb.tile([128, 8, 512], BF16, f"xTA{i}") for i in range(2)])
    cosTs = Rot([kb.tile([128, 512], F32, f"cosT{i}") for i in range(2)])
    sinTs = Rot([kb.tile([128, 512], F32, f"sinT{i}") for i in range(2)])
    qbs = Rot([kb.tile([128, 512], BF16, f"qb{i}") for i in range(3)])
    tAs = Rot([kb.tile([128, 512], F32, f"tA1_{i}") for i in range(2)])
    tBs = Rot([kb.tile([128, 512], F32, f"tB1_{i}") for i in range(2)])
    outs = Rot([kb.tile([128, 512], BF16, f"qk{i}") for i in range(3)])
    vaugs = Rot([kb.tile([128, 8, 128], BF16, f"vaug1_{i}") for i in range(2)])
    psw_f = kb.tile([128, 128], F32, "psw_f")
    kb.dma("sp", psw_f.ap, E["psw"], [], [psw_f])
    psw = kb.tile([128, 128], BF16, "psw")
    kb.cp(psw.ap, psw_f.ap, [psw_f], [psw])
    V1v = E["V1"].rearrange("h p kt d -> p h kt d")

    def loadA(i):
        y = ys.next()
        kb.dma("sp", y.ap, E["FACC"][i * 128:(i + 1) * 128, :], [], [y])
        return y

    def stageL(gi):
        ntl = 4 if gi < 16 else 2
        xT = xTs.next()

        def one(tl):
            i = gi * 4 + tl
            if i == 0:
                ybox[0] = loadA(0)
            y = ybox[0]
            if i + 1 < 66:
                ybox[0] = loadA(i + 1)
            x2 = x2s.next()
            layer_norm_tile(kb, y, gam, bet, x2, st12, mv, rs1, neghalf)
            if i < 64:
                kb.dma("sp", E["X2"][i * 128:(i + 1) * 128, :], x2.ap, [x2], [])
            xb = xbs.next()
            kb.cp(xb.ap, x2.ap, [x2], [xb], eng="act")
            pb = kb.bank_bf(0)
            for k in range(8):
                kb.tr(pb[:, k * 128:(k + 1) * 128], xb[:, k * 128:(k + 1) * 128], ident_b.ap, [xb, ident_b], [b[0]], sig=(k == 7))
            kb.cp(xT[:, :, tl * 128:(tl + 1) * 128], pb.rearrange("p (a b) -> p a b", a=8), [b[0]], [xT], eng=("act" if tl % 2 else "dve"))
        return xT, [(lambda tl=tl: one(tl)) for tl in range(ntl)]

    def stageP(gi, xT, Lsteps):
        ntl = 4 if gi < 16 else 2
        Wd = ntl * 128
        st = 0 if gi < 16 else 1
        col0 = gi * 512
        cosT, sinT = cosTs.next(), sinTs.next()
        kb.dma("sp", cosT[:, 0:Wd], E["cos1"][:, col0:col0 + Wd], [], [cosT])
        kb.dma("sp", sinT[:, 0:Wd], E["sin1"][:, col0:col0 + Wd], [], [sinT])

        def finish(mi, qb_):
            br = b[5 + mi % 2]
            kb.mm(br[:, 0:Wd], psw.ap, qb_[:, 0:Wd], True, True, [psw, qb_], [br])
            tA_ = tAs.next()
            kb.tt(tA_[:, 0:Wd], qb_[:, 0:Wd], cosT[:, 0:Wd], ALU.mult, [qb_, cosT], [tA_], eng="pool")
            tB_ = tBs.next()
            kb.tt(tB_[:, 0:Wd], br[:, 0:Wd], sinT[:, 0:Wd], ALU.mult, [br, sinT], [tB_])
            o = outs.next()
            kb.tt(o[:, 0:Wd], tA_[:, 0:Wd], tB_[:, 0:Wd], ALU.add, [tA_, tB_], [o])
            dst_d = E["QT"] if mi < 8 else E["KT"]
            kb.dma("sp", dst_d[mi % 8][:, col0:col0 + Wd], o[:, 0:Wd], [o], [])

        pend = None
        pbanks = (b[1], b[2], b[7])
        for mi in range(16):
            bk = pbanks[mi % 3]
            for k in range(8):
                kb.mm(bk[:, 0:Wd], W[st][:, k, mi * 128:(mi + 1) * 128], xT[:, k, 0:Wd], k == 0, k == 7, [W[st], xT], [bk], sig=(k == 7))
            qb_ = qbs.next()
            kb.act(qb_[:, 0:Wd], bk[:, 0:Wd], AF.Identity, [bk, bcol], [qb_], bias=bcol[:, mi, st:st + 1])
            if pend is not None:
                finish(*pend)
            pend = (mi, qb_)
            if mi % 4 == 3 and Lsteps:
                Lsteps.pop(0)()
        finish(*pend)
        for tl in range(ntl):
            i = gi * 4 + tl
            ts_ = slice(tl * 128, (tl + 1) * 128)
            va = vaugs.next()
            for half in range(2):
                bk = b[3 + half]
                for k in range(8):
                    kb.mm(bk.ap, xT[:, k, ts_], W[st][:, k, 2048 + half * 512:2048 + (half + 1) * 512], k == 0, False, [xT, W[st]], [bk], sig=False)
                kb.mm(bk.ap, ones_b[0:1, 0:128], brow[st][:, half * 512:(half + 1) * 512], False, True, [ones_b, brow[st]], [bk])
                kb.cp(va[:, half * 4:(half + 1) * 4, :], bk.ap.rearrange("p (a b) -> p a b", a=4), [bk], [va], eng=("act" if half else "dve"))
            kb.dma("sp", V1v[:, :, i, :], va.ap, [va], [])
        while Lsteps:
            Lsteps.pop(0)()

    ybox = [None]
    xT_cur, steps = stageL(0)
    for f_ in steps:
        f_()
    for gi in range(17):
        if gi + 1 < 17:
            xT_next, steps = stageL(gi + 1)
        else:
            xT_next, steps = None, []
        stageP(gi, xT_cur, steps)
        xT_cur = xT_next


PHASES = ["mod", "A0", "B0", "C0", "D0", "A1", "B1", "C1", "D1", "E"]


def build(dbg=False, stop_after=None, only=None):
    kb = KB(dbg, stop_after)
    E = {}
    inp = kb.inp
    E["x"] = inp("x", [NLAT, D])
    E["ctx"] = inp("ctx", [NCTX, D])
    E["ccT"] = inp("ccT", [128, 8, 2])
    E["ident"] = inp("ident", [128, 128])
    E["mconst"] = inp("mconst", [128, 4])
    E["bdiag"] = inp("bdiag", [128, 128])
    E["drow"] = inp("drow", [16, 128])
    E["cs0"] = inp("cs0", [64, NALL])
    E["cos1"] = inp("cos1", [128, NALL])
    E["sin1"] = inp("sin1", [128, NALL])
    for l in range(2):
        E[f"wmod{l}"] = inp(f"wmod{l}", [12, 128, 8, 512])
        E[f"bmod{l}"] = inp(f"bmod{l}", [1, 6144])
        E[f"bmodT{l}"] = inp(f"bmodT{l}", [128, 48])
        E[f"wout{l}"] = inp(f"wout{l}", [128, 8, 1024])
        E[f"router{l}"] = inp(f"router{l}", [128, 8, 16])
        E[f"lnmix{l}"] = inp(f"lnmix{l}", [2, 1024])
        E[f"lnffn{l}"] = inp(f"lnffn{l}", [2, 1024])
        for nm in ("gate", "up", "down"):
            E[f"w{nm}{l}"] = inp(f"w{nm}{l}", [16, 1024, 1024])
    E["win0"] = inp("win0", [128, 8, 1472])
    E["wuqx"] = inp("wuqx", [128, 2, 1024])
    E["wukx"] = inp("wukx", [128, 1024])
    E["wuv"] = inp("wuv", [128, 512])
    E["qnorm"] = inp("qnorm", [128, 2])
    E["kvnorm"] = inp("kvnorm", [128, 1])
    E["gln"] = inp("gln", [128, 8])
    E["wsT"] = inp("wsT", [128, 512])
    E["gbs"] = inp("gbs", [1, 512])
    E["win1"] = inp("win1", [128, 8, 3072])
    E["lam4"] = inp("lam4", [1, 256])
    E["subln"] = inp("subln", [128, 1])
    E["psw"] = inp("psw", [128, 128])
    sc = kb.scratch
    E["MOD"] = sc("MOD", [2, 2, 6144], F32)
    E["AT0"] = sc("AT0", [512, NALL], BF16)
    E["QT"] = sc("QT", [8, 128, NALL], BF16)
    E["KT"] = sc("KT", [8, 128, NALL], BF16)
    E["V0"] = sc("V0", [8, 128, 66, 128], BF16)
    E["V1"] = sc("V1", [8, 128, 66, 128], BF16)
    E["OT"] = sc("OT", [1024, NALL], BF16)
    E["FACC"] = sc("FACC", [NPAD, D], F32)
    E["HF"] = sc("HF", [NPAD, D], BF16)
    E["X2"] = sc("X2", [NLAT, D], F32)
    E["AFF"] = sc("AFF", [NPAD, 16], F32)
    E["KEYS"] = sc("KEYS", [128, 64], F32)
    E["out"] = kb.nc.dram_tensor("out", [NLAT, D], F32, kind="ExternalOutput").ap()
    C = setup_consts(kb, E)
    fns = {
        "mod": lambda: phase_mod(kb, E, C),
        "A0": lambda: phase_A0(kb, E, C),
        "B0": lambda: phase_attn(kb, E, C, 0),
        "C0": lambda: phase_C(kb, E, C, 0),
        "D0": lambda: phase_D(kb, E, C, 0),
        "A1": lambda: phase_A1(kb, E, C),
        "B1": lambda: phase_attn(kb, E, C, 1),
        "C1": lambda: phase_C(kb, E, C, 1),
        "D1": lambda: phase_D(kb, E, C, 1),
        "E": lambda: phase_E(kb, E, C),
    }
    for ph in PHASES:
        if only is None or ph in only:
            fns[ph]()
            kb.phase()
        if ph == stop_after:
            break
    kb.s.barrier()
    return kb


def _rope_tables():
    t = np.arange(NLAT)
    row = (t // 64).astype(np.float32)
    col = (t % 64).astype(np.float32)

    def ang(dim):
        nf = dim // 4
        inv = (np.float32(10000.0) ** (-np.arange(nf, dtype=np.float32) / np.float32(nf))).astype(np.float32)
        return np.concatenate([row[:, None] * inv, col[:, None] * inv], -1).astype(np.float32)

    a0 = ang(32)
    c0, s0 = np.cos(a0).T, np.sin(a0).T
    cs0 = np.zeros((64, NALL), np.float32)
    cs0[0:32, NLAT:] = 1.0
    cs0[0:16, :NLAT] = c0
    cs0[16:32, :NLAT] = c0
    cs0[32:48, :NLAT] = -s0
    cs0[48:64, :NLAT] = s0
    a1 = ang(64)
    c1, s1 = np.cos(a1).T, np.sin(a1).T
    cos1 = np.ones((128, NALL), np.float32)
    sin1 = np.zeros((128, NALL), np.float32)
    for blk in range(4):
        cos1[blk * 32:(blk + 1) * 32, :NLAT] = c1
        sin1[blk * 32:(blk + 1) * 32, :NLAT] = (-s1 if blk % 2 == 0 else s1)
    return cs0, cos1, sin1


def _pk(w):
    K = w.shape[0] // 128
    return np.ascontiguousarray(w.reshape(K, 128, -1).transpose(1, 0, 2))


def prep_shared(I):
    f = lambda a: np.ascontiguousarray(np.asarray(a, dtype=np.float32))
    S = {}
    S["ident"] = np.eye(128, dtype=np.float32)
    p = np.arange(128)
    mc = np.zeros((128, 4), np.float32)
    mc[:, 0] = 128 * (p % 8)
    mc[:, 1] = NALL + p
    S["mconst"] = mc
    S["bdiag"] = (p[:, None] // 8 == p[None, :] // 8).astype(np.float32)
    S["drow"] = np.tile((NALL + np.arange(128, dtype=np.float32))[None, :], (16, 1))
    S["cs0"], S["cos1"], S["sin1"] = _rope_tables()
    for l in range(2):
        wm = f(I[f"w_mod_{l}"])
        S[f"wmod{l}"] = np.ascontiguousarray(wm.reshape(8, 128, 12, 512).transpose(2, 1, 0, 3))
        bm = f(I[f"b_mod_{l}"])
        S[f"bmod{l}"] = bm.reshape(1, 6144)
        S[f"bmodT{l}"] = np.ascontiguousarray(bm.reshape(48, 128).T)
        S[f"wout{l}"] = _pk(f(I[f"w_out_{l}"]))
        S[f"router{l}"] = _pk(f(I[f"router_{l}"]))
        S[f"lnmix{l}"] = np.stack([f(I[f"ln_mix_g_{l}"]), f(I[f"ln_mix_b_{l}"])])
        S[f"lnffn{l}"] = np.stack([f(I[f"ln_ffn_g_{l}"]), f(I[f"ln_ffn_b_{l}"])])
        for nm in ("gate", "up", "down"):
            S[f"w{nm}{l}"] = f(I[f"w_{nm}_{l}"])
    w = f(I["w_in_0"])
    kr = w[:, 1408:1440]
    krE, krO = kr[:, 0::2], kr[:, 1::2]
    S["win0"] = _pk(np.concatenate([w[:, 0:512], w[:, 1024:1280], w[:, 1280:1408], krE, krO, krO, krE, w[:, 512:1024]], 1))
    wuq = f(I["mla_w_uq_0"])
    blocks = []
    for h in range(8):
        nope = wuq[:, h * 96:h * 96 + 64]
        rp = wuq[:, h * 96 + 64:h * 96 + 96]
        rE, rO = rp[:, 0::2], rp[:, 1::2]
        blocks.append(np.concatenate([rE, rO, rO, rE, nope], 1))
    S["wuqx"] = _pk(np.concatenate(blocks, 1))
    wukv = f(I["mla_w_ukv_0"])
    S["wukx"] = np.ascontiguousarray(np.concatenate(
        [np.concatenate([np.zeros((128, 64), np.float32), wukv[:, h * 128:h * 128 + 64]], 1) for h in range(8)], 1))
    S["wuv"] = np.ascontiguousarray(np.concatenate([wukv[:, h * 128 + 64:h * 128 + 128] for h in range(8)], 1))
    S["qnorm"] = np.ascontiguousarray(f(I["mla_q_norm_0"]).reshape(2, 128).T)
    S["kvnorm"] = f(I["mla_kv_norm_0"]).reshape(128, 1)
    S["gln"] = np.ascontiguousarray(np.concatenate([f(I["gmlp_ln_g_0"]).reshape(4, 128).T, f(I["gmlp_ln_b_0"]).reshape(4, 128).T], 1))
    S["wsT"] = np.ascontiguousarray(f(I["gmlp_ws_0"]).transpose(2, 0, 1).reshape(128, 512))
    S["gbs"] = f(I["gmlp_bs_0"]).reshape(1, 512)
    w1 = f(I["w_in_1"])
    cols = []
    for part in range(2):
        for j in range(16):
            blk = w1[:, part * 1024 + j * 64:part * 1024 + (j + 1) * 64]
            cols += [blk[:, 0::2], blk[:, 1::2]]
    cols.append(w1[:, 2048:3072])
    S["win1"] = _pk(np.concatenate(cols, 1))
    S["lam4"] = np.concatenate([f(I["lambda_q1_1"]), f(I["lambda_k1_1"]), f(I["lambda_q2_1"]), f(I["lambda_k2_1"])]).reshape(1, 256)
    S["subln"] = f(I["subln_g_1"]).reshape(128, 1)
    S["psw"] = np.eye(128, dtype=np.float32)[:, np.arange(128) ^ 32]
    return S


def prep_core(I, S, bidx):
    m = dict(S)
    m["x"] = np.ascontiguousarray(np.asarray(I["x"][bidx], dtype=np.float32))
    m["ctx"] = np.ascontiguousarray(np.asarray(I["ctx"][bidx], dtype=np.float32))
    cc = np.stack([np.asarray(I["c"][bidx], np.float32), np.asarray(I["c_ctx"], np.float32)], -1)
    m["ccT"] = np.ascontiguousarray(cc.reshape(8, 128, 2).transpose(1, 0, 2))
    return m


_KB_CACHE = {}


def kernel(**inputs):
    if "kb" not in _KB_CACHE:
        _KB_CACHE["kb"] = build()
    kb = _KB_CACHE["kb"]
    S = prep_shared(inputs)
    in_maps = [prep_core(inputs, S, bidx) for bidx in range(8)]
    res = run_bass_kernel_spmd(kb.nc, in_maps, core_ids=list(range(8)))
    return np.stack([np.asarray(r["out"], dtype=np.float32) for r in res.results], 0)
```
